# Optimizing a Trainium2 kernel written in Bass

```python
import jax, jax.numpy as jnp
from jax import lax
import numpy as np

D_MODEL = 1024
BATCH = 16
SEQ = 2048
DEPTH = 2

CHUNK = 64
MEM_LEN = 256
HEAD_GROUP = 64
A_WIDTH = 384
B_WIDTH = 320
C_WIDTH = 320
MIX_WIDTH = A_WIDTH + B_WIDTH + C_WIDTH
N_MIX_GROUPS = MIX_WIDTH // HEAD_GROUP
C_GROUPS = C_WIDTH // HEAD_GROUP
A_KERNEL = 31
B_KERNEL = 3
GMLP_BLOCK = 128
PROJ_WIDTH = 2 * A_WIDTH + 3 * B_WIDTH + 2 * C_WIDTH
X_HEADS = 4
X_HEAD_DIM = D_MODEL // X_HEADS
D_FF = 2816
N_EXPERTS = 8
TOP_K = 2
D_EXPERT = 3584
N_DENSE = (DEPTH + 1) // 2
N_MOE = DEPTH // 2
EPS = 1e-6

kernel_name = "hybrid_conv_gmlp_moe_encoder"


def rmsnorm(x, g):
    xf = x.astype(jnp.float32)
    y = xf * lax.rsqrt(jnp.mean(xf * xf, axis=-1, keepdims=True) + EPS)
    return (y * g.astype(jnp.float32)).astype(x.dtype)


def layernorm(x, g, b):
    xf = x.astype(jnp.float32)
    mu = jnp.mean(xf, axis=-1, keepdims=True)
    xc = xf - mu
    var = jnp.mean(xc * xc, axis=-1, keepdims=True)
    y = xc * lax.rsqrt(var + EPS) * g.astype(jnp.float32) + b.astype(jnp.float32)
    return y.astype(x.dtype)


def causal_depthwise_conv(x, w):
    k, c = w.shape
    return lax.conv_general_dilated(
        x, w[:, None, :], window_strides=(1,), padding=[(k - 1, 0)],
        dimension_numbers=("NWC", "WIO", "NWC"), feature_group_count=c)


def chunk_spatial_gate(u, v, ws, bias):
    bsz, s, c = v.shape
    n_blocks = s // GMLP_BLOCK
    chunk_id = jnp.arange(GMLP_BLOCK) // CHUNK
    mask = chunk_id[:, None] >= chunk_id[None, :]
    ws = jnp.where(mask[None], ws, jnp.zeros((), ws.dtype))
    vb = v.reshape(bsz, n_blocks, GMLP_BLOCK, C_GROUPS, HEAD_GROUP)
    mixed = jnp.einsum("gij,bnjgc->bnigc", ws, vb) + bias.T[None, None, :, :, None]
    return u * mixed.reshape(bsz, s, c)


def hybrid_mixer(xn, w_in, conv_a_w, conv_a_b, ln_a_g, ln_a_b, conv_b_w,
                 ln_c_g, ln_c_b, gmlp_ws, gmlp_b, mix_out_g, w_mix_out):
    z = xn @ w_in
    splits = [A_WIDTH, 2 * A_WIDTH, 2 * A_WIDTH + B_WIDTH,
              2 * A_WIDTH + 2 * B_WIDTH, 2 * A_WIDTH + 3 * B_WIDTH]
    a_val, a_gate, b_gate, c_gate, b_in, c_z = jnp.split(z, splits, axis=-1)
    ya = a_val * jax.nn.sigmoid(a_gate)
    ya = causal_depthwise_conv(ya, conv_a_w) + conv_a_b
    ya = jax.nn.silu(layernorm(ya, ln_a_g, ln_a_b))
    yb = b_gate * causal_depthwise_conv(c_gate * b_in, conv_b_w)
    c_z = jax.nn.gelu(c_z)
    c_u, c_v = jnp.split(c_z, 2, axis=-1)
    c_v = layernorm(c_v, ln_c_g, ln_c_b)
    yc = chunk_spatial_gate(c_u, c_v, gmlp_ws, gmlp_b)
    y = jnp.concatenate([ya, yb, yc], axis=-1)
    bsz, s, _ = y.shape
    y = rmsnorm(y.reshape(bsz, s, N_MIX_GROUPS, HEAD_GROUP),
                mix_out_g.reshape(N_MIX_GROUPS, HEAD_GROUP)).reshape(bsz, s, MIX_WIDTH)
    return y @ w_mix_out


def memory_cross_attention(xn, memn, w_xq, w_xkv, w_xo):
    bsz, s, _ = xn.shape
    m = memn.shape[1]
    q = (xn @ w_xq).reshape(bsz, s, X_HEADS, X_HEAD_DIM)
    k, v = jnp.split(memn @ w_xkv, 2, axis=-1)
    k = k.reshape(bsz, m, X_HEADS, X_HEAD_DIM)
    v = v.reshape(bsz, m, X_HEADS, X_HEAD_DIM)
    scores = jnp.einsum("bshd,bmhd->bhsm", q, k).astype(jnp.float32) * (X_HEAD_DIM ** -0.5)
    p = jax.nn.softmax(scores, axis=-1).astype(v.dtype)
    o = jnp.einsum("bhsm,bmhd->bshd", p, v).reshape(bsz, s, D_MODEL)
    return o @ w_xo


def swiglu(x, w_gate, w_up, w_down):
    return (jax.nn.silu(x @ w_gate) * (x @ w_up)) @ w_down


def moe_swiglu(xn, w_router, w_gate, w_up, w_down):
    bsz, s, d = xn.shape
    xt = xn.reshape(-1, d)
    logits = (xt @ w_router).astype(jnp.float32)
    top_vals, top_idx = lax.top_k(logits, TOP_K)
    top_w = jax.nn.softmax(top_vals, axis=-1)
    combine = jnp.sum(jax.nn.one_hot(top_idx, N_EXPERTS, dtype=jnp.float32) * top_w[..., None], axis=1)
    combine = combine.astype(xn.dtype)
    y = jnp.zeros_like(xt)
    for e in range(N_EXPERTS):
        y = y + combine[:, e:e + 1] * swiglu(xt, w_gate[e], w_up[e], w_down[e])
    return y.reshape(bsz, s, d)


def setup_inputs(seed: int = 0) -> dict:
    key = jax.random.key(seed)
    ks = jax.random.split(key, 32)
    f32 = jnp.float32

    def nrm(k, shape, scale):
        return jax.random.normal(k, shape, f32) * scale

    def gain(k, shape):
        return 1.0 + 0.02 * jax.random.normal(k, shape, f32)

    return {
        "x": nrm(ks[0], (BATCH, SEQ, D_MODEL), 1.0),
        "mem": nrm(ks[1], (BATCH, MEM_LEN, D_MODEL), 1.0),
        "norm_mix_g": gain(ks[2], (DEPTH, D_MODEL)),
        "w_in": nrm(ks[3], (DEPTH, D_MODEL, PROJ_WIDTH), D_MODEL ** -0.5),
        "conv_a_w": nrm(ks[4], (DEPTH, A_KERNEL, A_WIDTH), A_KERNEL ** -0.5),
        "conv_a_b": nrm(ks[5], (DEPTH, A_WIDTH), 0.02),
        "ln_a_g": gain(ks[6], (DEPTH, A_WIDTH)),
        "ln_a_b": nrm(ks[7], (DEPTH, A_WIDTH), 0.02),
        "conv_b_w": nrm(ks[8], (DEPTH, B_KERNEL, B_WIDTH), B_KERNEL ** -0.5),
        "ln_c_g": gain(ks[9], (DEPTH, C_WIDTH)),
        "ln_c_b": nrm(ks[10], (DEPTH, C_WIDTH), 0.02),
        "gmlp_ws": nrm(ks[11], (DEPTH, C_GROUPS, GMLP_BLOCK, GMLP_BLOCK), GMLP_BLOCK ** -0.5),
        "gmlp_b": 1.0 + nrm(ks[12], (DEPTH, C_GROUPS, GMLP_BLOCK), 0.1),
        "mix_out_g": gain(ks[13], (DEPTH, MIX_WIDTH)),
        "w_mix_out": nrm(ks[14], (DEPTH, MIX_WIDTH, D_MODEL), MIX_WIDTH ** -0.5),
        "norm_x_g": gain(ks[15], (DEPTH, D_MODEL)),
        "norm_mem_g": gain(ks[16], (DEPTH, D_MODEL)),
        "w_xq": nrm(ks[17], (DEPTH, D_MODEL, D_MODEL), D_MODEL ** -0.5),
        "w_xkv": nrm(ks[18], (DEPTH, D_MODEL, 2 * D_MODEL), D_MODEL ** -0.5),
        "w_xo": nrm(ks[19], (DEPTH, D_MODEL, D_MODEL), D_MODEL ** -0.5),
        "norm_ffn_g": gain(ks[20], (DEPTH, D_MODEL)),
        "ffn_w_gate": nrm(ks[21], (N_DENSE, D_MODEL, D_FF), D_MODEL ** -0.5),
        "ffn_w_up": nrm(ks[22], (N_DENSE, D_MODEL, D_FF), D_MODEL ** -0.5),
        "ffn_w_down": nrm(ks[23], (N_DENSE, D_FF, D_MODEL), D_FF ** -0.5),
        "moe_router": nrm(ks[24], (N_MOE, D_MODEL, N_EXPERTS), D_MODEL ** -0.5),
        "moe_w_gate": nrm(ks[25], (N_MOE, N_EXPERTS, D_MODEL, D_EXPERT), D_MODEL ** -0.5),
        "moe_w_up": nrm(ks[26], (N_MOE, N_EXPERTS, D_MODEL, D_EXPERT), D_MODEL ** -0.5),
        "moe_w_down": nrm(ks[27], (N_MOE, N_EXPERTS, D_EXPERT, D_MODEL), D_EXPERT ** -0.5),
        "norm_final_g": gain(ks[28], (D_MODEL,)),
    }


def reference(x, mem, norm_mix_g, w_in, conv_a_w, conv_a_b, ln_a_g, ln_a_b, conv_b_w,
              ln_c_g, ln_c_b, gmlp_ws, gmlp_b, mix_out_g, w_mix_out, norm_x_g, norm_mem_g,
              w_xq, w_xkv, w_xo, norm_ffn_g, ffn_w_gate, ffn_w_up, ffn_w_down,
              moe_router, moe_w_gate, moe_w_up, moe_w_down, norm_final_g):
    h = x
    for layer in range(DEPTH):
        h = h + hybrid_mixer(rmsnorm(h, norm_mix_g[layer]), w_in[layer], conv_a_w[layer],
                             conv_a_b[layer], ln_a_g[layer], ln_a_b[layer], conv_b_w[layer],
                             ln_c_g[layer], ln_c_b[layer], gmlp_ws[layer], gmlp_b[layer],
                             mix_out_g[layer], w_mix_out[layer])
        h = h + memory_cross_attention(rmsnorm(h, norm_x_g[layer]), rmsnorm(mem, norm_mem_g[layer]),
                                       w_xq[layer], w_xkv[layer], w_xo[layer])
        hn = rmsnorm(h, norm_ffn_g[layer])
        i = layer // 2
        if layer % 2 == 0:
            h = h + swiglu(hn, ffn_w_gate[i], ffn_w_up[i], ffn_w_down[i])
        else:
            h = h + moe_swiglu(hn, moe_router[i], moe_w_gate[i], moe_w_up[i], moe_w_down[i])
    return rmsnorm(h, norm_final_g)
```

```python
import numpy as np
import concourse.bass as bass
import concourse.mybir as mybir
from concourse.bass_utils import run_bass_kernel_spmd

F32 = mybir.dt.float32
BF16 = mybir.dt.bfloat16
AF = mybir.ActivationFunctionType
ALU = mybir.AluOpType
AX = mybir.AxisListType

D = 1024
KC = 8
A_W, B_W, C_W = 384, 320, 320
PROJ = 2368
A_K = 31
EPS = 1e-6
NT = 512
N_EXP = 8


class Cfg:
    def __init__(self, S=2048, NB=2, MEM=256, DFF=2816, DEXP=3584, layers=(0, 1), ncores=8,
                 nslot=7, nf32=12, nb16x=8):
        self.S, self.NB, self.MEM, self.DFF, self.DEXP = S, NB, MEM, DFF, DEXP
        self.layers = tuple(layers)
        self.ncores = ncores
        self.nslot, self.nf32, self.nb16x = nslot, nf32, nb16x


def _colmajor(v):
    v = np.asarray(v, np.float32).reshape(-1)
    n = v.shape[0]
    c = (n + 127) // 128
    buf = np.zeros((c * 128,), np.float32)
    buf[:n] = v
    return np.ascontiguousarray(buf.reshape(c, 128).T)


PCOLS = {}


def _param_layout():
    off = 0
    def add(name, n):
        nonlocal off
        PCOLS[name] = (off, n)
        off += n
    add("norm_mix_g", 8); add("norm_x_g", 8); add("norm_mem_g", 8); add("norm_ffn_g", 8)
    add("mix_out_g", 8)
    add("conv_a_w", 3 * A_K); add("conv_a_b", 3); add("ln_a_g", 3); add("ln_a_b", 3)
    add("conv_b_w", 9)
    add("router", 64)
    add("ln_c_g", C_W); add("ln_c_b", C_W)
    add("gmlp_bias", 3 * 128)
    return off


NPCOL = _param_layout()


def _build_params(inp, layer):
    P = np.zeros((128, NPCOL), np.float32)
    def put(name, arr):
        o, n = PCOLS[name]
        assert arr.shape == (128, n), (name, arr.shape, n)
        P[:, o:o + n] = arr
    put("norm_mix_g", _colmajor(inp["norm_mix_g"][layer]))
    put("norm_x_g", _colmajor(inp["norm_x_g"][layer]))
    put("norm_mem_g", _colmajor(inp["norm_mem_g"][layer]))
    put("norm_ffn_g", _colmajor(inp["norm_ffn_g"][layer]))
    put("mix_out_g", _colmajor(inp["mix_out_g"][layer]))
    caw = np.asarray(inp["conv_a_w"][layer], np.float32)
    put("conv_a_w", np.concatenate([caw[:, c * 128:(c + 1) * 128].T for c in range(3)], axis=1))
    put("conv_a_b", _colmajor(inp["conv_a_b"][layer]))
    put("ln_a_g", _colmajor(inp["ln_a_g"][layer]))
    put("ln_a_b", _colmajor(inp["ln_a_b"][layer]))
    cbw = np.asarray(inp["conv_b_w"][layer], np.float32)
    cb = np.zeros((128, 9), np.float32)
    for j in range(3):
        r = min(128, B_W - j * 128)
        cb[:r, j * 3:(j + 1) * 3] = cbw[:, j * 128:j * 128 + r].T
    put("conv_b_w", cb)
    if layer % 2 == 1:
        wr = np.asarray(inp["moe_router"][layer // 2], np.float32)
        put("router", np.ascontiguousarray(wr.reshape(8, 128, 8).transpose(1, 0, 2)).reshape(128, 64))
    put("ln_c_g", np.broadcast_to(np.asarray(inp["ln_c_g"][layer], np.float32)[None, :], (128, C_W)))
    put("ln_c_b", np.broadcast_to(np.asarray(inp["ln_c_b"][layer], np.float32)[None, :], (128, C_W)))
    gb = np.asarray(inp["gmlp_b"][layer], np.float32)
    gbt = np.zeros((128, 3, 128), np.float32)
    gbt[64:128, 0, :] = gb[0][None, :]
    gbt[0:64, 1, :] = gb[1][None, :]
    gbt[64:128, 1, :] = gb[2][None, :]
    gbt[0:64, 2, :] = gb[3][None, :]
    gbt[64:128, 2, :] = gb[4][None, :]
    put("gmlp_bias", gbt.reshape(128, 384))
    return P


def _build_consts():
    ident = np.eye(128, dtype=np.float32)
    return ident


class Sched:
    ENGS = ("pe", "act", "dve", "pool", "sp")

    def __init__(self):
        self.ops = {e: [] for e in self.ENGS}
        self.cnt = {}
        self.seen = {e: {} for e in self.ENGS}
        self.lastw = {}
        self.readers = {}
        self.n_ops = 0

    def _need(self, eng, deps):
        waits = {}
        for (sem, val) in deps:
            if val <= self.seen[eng].get(sem, 0):
                continue
            if val > waits.get(sem, 0):
                waits[sem] = val
        for sem, val in waits.items():
            self.seen[eng][sem] = val
        return list(waits.items())

    def op(self, eng, fn, reads=(), writes=(), inc=True, sem=None, amt=1):
        if sem is None:
            sem = eng
        deps = []
        for k in reads:
            w = self.lastw.get(k)
            if w is not None:
                deps.append(w)
        for k in writes:
            w = self.lastw.get(k)
            if w is not None:
                deps.append(w)
            deps.extend(self.readers.get(k, ()))
        if eng == "pe":
            deps = [d for d in deps if d[0] != "pe"]
        waits = self._need(eng, deps)
        cur = self.cnt.get(sem, 0)
        val = cur + amt
        if inc:
            self.cnt[sem] = val
        for k in reads:
            self.readers.setdefault(k, []).append((sem, val))
        for k in writes:
            self.lastw[k] = (sem, val)
            self.readers[k] = []
        self.ops[eng].append((waits, fn, sem if inc else None, amt))
        self.n_ops += 1

    def check_deadlock(self):
        cnt = {}
        pos = {e: 0 for e in self.ENGS}
        progress = True
        while progress:
            progress = False
            for e in self.ENGS:
                q = self.ops[e]
                while pos[e] < len(q):
                    waits, fn, incsem, amt = q[pos[e]]
                    if any(cnt.get(s_, 0) < v for (s_, v) in waits):
                        break
                    if incsem is not None:
                        cnt[incsem] = cnt.get(incsem, 0) + amt
                    pos[e] += 1
                    progress = True
        stuck = {e: (pos[e], len(self.ops[e])) for e in self.ENGS if pos[e] < len(self.ops[e])}
        if stuck:
            msg = []
            for e, (p_, n_) in stuck.items():
                waits = self.ops[e][p_][0]
                msg.append(f"{e} stuck at {p_}/{n_} waiting {[(s_, v, cnt.get(s_, 0)) for (s_, v) in waits]}")
            raise RuntimeError("DEADLOCK in schedule: " + "; ".join(msg))

    def barrier_wait(self, eng, sems):
        deps = [(s, self.cnt.get(s, 0)) for s in sems]
        waits = self._need(eng, deps)
        if waits:
            self.ops[eng].append((waits, None, None, 0))


class Pool:
    def __init__(self, items):
        self.free_list = list(items)

    def get(self):
        assert self.free_list, "pool exhausted"
        return self.free_list.pop(0)

    def put(self, t):
        self.free_list.append(t)


class Tile:
    def __init__(self, ap, key):
        self.ap = ap
        self.key = key


def build_nc(cfg):
    nc = bass.Bass("TRN2", target_bir_lowering=False)
    S, NB, MEM = cfg.S, cfg.NB, cfg.MEM
    NTILES = S // NT
    L = len(cfg.layers)
    has_dense = any(l % 2 == 0 for l in cfg.layers)
    has_moe = any(l % 2 == 1 for l in cfg.layers)
    DFF, DEXP = cfg.DFF, cfg.DEXP

    def dram(name, shape, kind="ExternalInput"):
        return nc.dram_tensor(name, list(shape), F32, kind=kind).ap()

    x_d = dram("x", [NB, S, D])
    mem_d = dram("mem", [NB, MEM, D])
    par_d = dram("params", [L, 128, NPCOL])
    wst_d = dram("wsT", [L, 128, 5 * 128])
    fin_d = dram("fin_g", [128, 8])
    ident_d = dram("ident", [128, 128])
    w_in_d = dram("w_in", [L, D, PROJ])
    w_mo_d = dram("w_mix_out", [L, D, D])
    w_xq_d = dram("w_xq", [L, D, D])
    w_xkv_d = dram("w_xkv", [L, D, 2 * D])
    w_xo_d = dram("w_xo", [L, D, D])
    if has_dense:
        fg_d = dram("ffn_w_gate", [1, D, DFF])
        fu_d = dram("ffn_w_up", [1, D, DFF])
        fd_d = dram("ffn_w_down", [1, DFF, D])
    if has_moe:
        mg_d = dram("moe_w_gate", [1, N_EXP, D, DEXP])
        mu_d = dram("moe_w_up", [1, N_EXP, D, DEXP])
        md_d = dram("moe_w_down", [1, N_EXP, DEXP, D])
    out_d = dram("out", [NB, S, D], kind="ExternalOutput")

    from contextlib import ExitStack
    es = ExitStack()
    with es:
        def sb(name, shape, dt):
            return es.enter_context(nc.sbuf_tensor(name, list(shape), dt))

        def ps(name):
            return es.enter_context(nc.psum_tensor(name, [128, NT], F32))

        hT = sb("hT", [128, KC, S], F32)
        NB16 = KC * NTILES + cfg.nb16x
        b16 = sb("b16", [128, NB16, NT], BF16)
        f32p = sb("f32p", [128, cfg.nf32, NT], F32)
        ring = sb("ring", [128, cfg.nslot, 4096], BF16)
        abuf = sb("abuf", [128, 3, NT + A_K - 1], BF16)
        NDG = 8
        dg = sb("dg", [128, NDG, 128], BF16)
        ident_b = sb("ident_b", [128, 128], BF16)
        bhist = sb("bhist", [128, 3, 2], F32)
        par = sb("par", [128, L, NPCOL], F32)
        fing = sb("fing", [128, 8], F32)
        wsT = sb("wsTs", [128, 5, 128], BF16)
        ident = sb("identf", [128, 128], F32)
        ones_b = sb("ones_b", [128, 128], BF16)
        bdiag = sb("bdiag", [128, 128], BF16)
        ones_f = sb("ones_f", [128, 128], F32)
        comb_tm = sb("comb_tm", [128, (S // 128) * 8], F32)
        lbc = sb("lbc", [128, 2, 128], F32)
        gw = sb("gw", [128, 64], F32)
        small = sb("small", [128, 64], F32)
        stat6 = sb("stat6", [128, 8], F32)
        stat6v = sb("stat6v", [128, 4, 6], F32)
        banks = [ps(f"bank{i}") for i in range(8)]

        sems = {}
        def sem(name):
            if name not in sems:
                sems[name] = es.enter_context(nc.semaphore(name))
            return sems[name]

        for e in Sched.ENGS:
            sem(e)

        sch = Sched()
        bpool = Pool([Tile(b16[:, i, :], ("b", i)) for i in range(NB16)])
        fpool = Pool([Tile(f32p[:, i, :], ("f", i)) for i in range(cfg.nf32)])
        ppool = Pool([Tile(banks[i][:, :], ("ps", i)) for i in range(8)])

        def ACT(out, in_, func, reads, writes, **kw):
            sch.op("act", lambda e: e.activation(out=out, in_=in_, func=func, **kw), reads, writes)

        def TT(out, in0, in1, op, reads, writes):
            sch.op("dve", lambda e: e.tensor_tensor(out=out, in0=in0, in1=in1, op=op), reads, writes)

        def TS(out, in0, s1, s2, op0, op1, reads, writes, **kw):
            if s2 is None:
                sch.op("dve", lambda e: e.tensor_scalar(out=out, in0=in0, scalar1=s1, scalar2=None, op0=op0, **kw),
                       reads, writes)
            else:
                sch.op("dve", lambda e: e.tensor_scalar(out=out, in0=in0, scalar1=s1, scalar2=s2, op0=op0, op1=op1, **kw),
                       reads, writes)

        def STT(out, in0, scalar, in1, op0, op1, reads, writes, **kw):
            sch.op("dve", lambda e: e.scalar_tensor_tensor(out=out, in0=in0, scalar=scalar, in1=in1, op0=op0, op1=op1, **kw),
                   reads, writes)

        def RECIP(out, in_, reads, writes):
            sch.op("dve", lambda e: e.reciprocal(out=out, in_=in_), reads, writes)

        def MM(out, lhsT, rhs, start, stop, reads, writes, inc_all=False):
            sch.op("pe", lambda e: e.matmul(out, lhsT, rhs, start=start, stop=stop), reads, writes, inc=(stop or inc_all))

        def TR(out, in_, reads, writes, inc=True):
            sch.op("pe", lambda e: e.transpose(out, in_, ident[:, :]), list(reads) + ["ident"], writes, inc=inc)

        def DMA(eng, out, in_, semname, reads, writes, **kw):
            sem(semname)
            sch.op(eng, lambda e: e.dma_start(out=out, in_=in_, **kw), reads, writes, sem=semname, amt=16)

        def hkey(c, t):
            return ("h", c, t)

        def pcol(l, name, c0=0, n=None):
            o, nn = PCOLS[name]
            if n is None:
                n = nn - c0
            return par[:, l, o + c0:o + c0 + n]

        class Ring:
            def __init__(self):
                self.n = cfg.nslot
                self.next = 0
                self.loads = [0] * self.n

            def load(self, src_ap, view):
                i = self.next
                self.next = (self.next + 1) % self.n
                a, b = src_ap.shape[1], src_ap.shape[2]
                if view == "k":
                    dst = ring[:, i, :].rearrange("p (k n) -> p k n", k=8)[:, 0:a, 0:b]
                    full = ring[:, i, :].rearrange("p (k n) -> p k n", k=8)
                else:
                    dst = ring[:, i, :].rearrange("p (k n) -> p k n", k=4)[:, 0:a, 0:b]
                    full = ring[:, i, :].rearrange("p (k n) -> p k n", k=4)
                key = ("w", i)
                DMA("pool", dst, src_ap, f"w{i}", [], [key])
                return Tile(full, key)

        wring = Ring()

        def wblock(W2d, c0, ncols):
            src = W2d[:, c0:c0 + ncols].rearrange("(k p) n -> p k n", p=128)
            return wring.load(src, "k")

        def wblock_down(W2d, f0, nf):
            src = W2d[f0:f0 + nf, :].rearrange("(k p) n -> p k n", p=128)
            return wring.load(src, "d")

        DMA("sp", ident[:, :], ident_d[:, :], "cst", [], ["ident"])
        DMA("sp", fing[:, :], fin_d[:, :], "cst", [], ["fing"])
        for li in range(L):
            DMA("sp", par[:, li, :], par_d[li], "cst", [], [("par", li)])
        for k in ["ident", "fing"] + [("par", li) for li in range(L)]:
            sch.lastw[k] = ("cst", sch.cnt["cst"])
        sch.op("dve", lambda e: e.tensor_copy(out=ident_b[:, :], in_=ident[:, :]), ["ident"], ["ident_b"])
        sch.op("dve", lambda e: e.memset(ones_b[:, :], 1.0), [], ["ones_b"])
        sch.op("dve", lambda e: e.memset(ones_f[:, :], 1.0), [], ["ones_f"])
        sch.op("dve", lambda e: e.memset(bdiag[:, :], 0.0), [], ["bdiag"])
        sch.op("dve", lambda e: e.memset(bdiag[0:64, 0:64], 1.0), [], ["bdiag"])
        sch.op("dve", lambda e: e.memset(bdiag[64:128, 64:128], 1.0), [], ["bdiag"])

        def rmsnorm_T(src, gcol, gkey, dst, n, keep_rstd=False):
            pst = ppool.get()
            for c in range(KC):
                sq = bpool.get()
                ACT(sq.ap[:, :n], src[c][0], AF.Square, [src[c][1]], [sq.key])
                MM(pst.ap[:, :n], ones_b[:, :], sq.ap[:, :n], c == 0, c == KC - 1, [sq.key, "ones_b"], [pst.key], inc_all=True)
                bpool.put(sq)
            rstd = fpool.get()
            ACT(rstd.ap[:, :n], pst.ap[:, :n], AF.Ln, [pst.key], [rstd.key], scale=1.0 / D, bias=EPS)
            ACT(rstd.ap[:, :n], rstd.ap[:, :n], AF.Exp, [rstd.key], [rstd.key], scale=-0.5)
            ppool.put(pst)
            if dst is not None:
                for c in range(KC):
                    STT(dst[c][0], src[c][0], gcol[:, c:c + 1], rstd.ap[:, :n], ALU.mult, ALU.mult,
                        [src[c][1], rstd.key, gkey], [dst[c][1]])
            if keep_rstd:
                return rstd
            fpool.put(rstd)
            return None

        def load_T(src2d, nrows, dst_fn, blks=None):
            for blk in (range(nrows // 128) if blks is None else blks):
                for half in range(2):
                    xt = fpool.get()
                    DMA("sp", xt.ap, src2d[blk * 128:(blk + 1) * 128, half * 512:(half + 1) * 512], "ld%d" % xt.key[1],
                        [], [xt.key])
                    pst = ppool.get()
                    for j in range(4):
                        TR(pst.ap[:, j * 128:(j + 1) * 128], xt.ap[:, j * 128:(j + 1) * 128], [xt.key], [pst.key], inc=(j == 3))
                    fpool.put(xt)
                    dap, dkeys = dst_fn(half, blk)
                    ACT(dap, pst.ap.rearrange("p (j n) -> p j n", j=4), AF.Identity, [pst.key], dkeys)
                    ppool.put(pst)

        def hsrc(t):
            return [(hT[:, c, t * NT:(t + 1) * NT], hkey(c, t)) for c in range(KC)]

        def groupnorm(y, li, chunk):
            sq = bpool.get()
            ACT(sq.ap, y.ap, AF.Square, [y.key], [sq.key])
            pst = ppool.get()
            MM(pst.ap, bdiag[:, :], sq.ap, True, True, [sq.key, "bdiag"], [pst.key])
            bpool.put(sq)
            rstd = fpool.get()
            ACT(rstd.ap, pst.ap, AF.Ln, [pst.key], [rstd.key], scale=1.0 / 64, bias=EPS)
            ACT(rstd.ap, rstd.ap, AF.Exp, [rstd.key], [rstd.key], scale=-0.5)
            ppool.put(pst)
            yn = bpool.get()
            STT(yn.ap, y.ap, pcol(li, "mix_out_g", chunk, 1), rstd.ap, ALU.mult, ALU.mult,
                [y.key, rstd.key, ("par", li)], [yn.key])
            fpool.put(rstd)
            return yn

        def gelu(out_ap, out_key, src_ap, src_key, shape_fn, extra_reads=()):
            t = fpool.get()
            tap = shape_fn(t.ap)
            ACT(out_ap, src_ap, AF.Identity, [src_key], [out_key])
            ACT(tap, src_ap, AF.Square, [src_key], [t.key])
            TS(tap, tap, 0.044715, 1.0, ALU.mult, ALU.add, [t.key], [t.key])
            TT(tap, tap, out_ap, ALU.mult, [t.key, out_key], [t.key])
            ACT(tap, tap, AF.Sigmoid, [t.key], [t.key], scale=1.5957691216057308)
            TT(out_ap, tap, out_ap, ALU.mult, [t.key, out_key] + list(extra_reads), [out_key])
            fpool.put(t)

        def proj(wt, col0, m, xn, prow=0):
            pst = ppool.get()
            for k in range(KC):
                MM(pst.ap[prow:prow + m, :], wt.ap[:, k, col0:col0 + m], xn[k].ap, k == 0, k == KC - 1,
                   [wt.key, xn[k].key], [pst.key])
            return pst

        def resid_add(pst, c, t):
            hs = hT[:, c, t * NT:(t + 1) * NT]
            TT(hs, hs, pst.ap, ALU.add, [hkey(c, t), pst.key], [hkey(c, t)])
            ppool.put(pst)

        def full_barrier():
            allsems = list(sch.cnt.keys())
            for e in Sched.ENGS:
                sch.barrier_wait(e, allsems)

        def mixer_phase(li, l):
            from collections import deque
            W = w_in_d[li]
            wb = [wblock(W, 512 * i, min(512, PROJ - 512 * i)) for i in range(5)]
            wo = [wblock(w_mo_d[li], 512 * i, 512) for i in range(2)]
            DMA("pool", wsT[:, :, :], wst_d[li].rearrange("p (g i) -> p g i", g=5), "wst", [], ["wsT"])
            sch.op("dve", lambda e: e.memset(wsT[64:128, :, 0:64], 0.0), [], ["wsT"])
            sch.op("dve", lambda e: e.memset(abuf[:, :, 0:A_K - 1], 0.0), [], [("abuf", 0), ("abuf", 1), ("abuf", 2)])
            sch.op("dve", lambda e: e.memset(bhist[:, :, :], 0.0), [], ["bhist"])
            pk = ("par", li)

            def wcol(col):
                return wb[col // 512], col % 512

            pending = deque()

            def run_unit(gen):
                olds = list(pending)
                pending.clear()
                try:
                    next(gen)
                    newp = gen
                except StopIteration:
                    newp = None
                for g in olds:
                    try:
                        next(g)
                        pending.append(g)
                    except StopIteration:
                        pass
                if newp is not None:
                    pending.append(newp)

            def drain():
                while pending:
                    g = pending.popleft()
                    try:
                        next(g)
                        pending.append(g)
                    except StopIteration:
                        pass

            def gn_stages(y, chunk, ymix, free_y=True):
                sq = bpool.get()
                ACT(sq.ap, y.ap, AF.Square, [y.key], [sq.key])
                yield
                pst = ppool.get()
                MM(pst.ap, bdiag[:, :], sq.ap, True, True, [sq.key, "bdiag"], [pst.key])
                bpool.put(sq)
                rstd = fpool.get()
                ACT(rstd.ap, pst.ap, AF.Ln, [pst.key], [rstd.key], scale=1.0 / 64, bias=EPS)
                ppool.put(pst)
                ACT(rstd.ap, rstd.ap, AF.Exp, [rstd.key], [rstd.key], scale=-0.5)
                yn = bpool.get()
                STT(yn.ap, y.ap, pcol(li, "mix_out_g", chunk, 1), rstd.ap, ALU.mult, ALU.mult,
                    [y.key, rstd.key, ("par", li)], [yn.key])
                fpool.put(rstd)
                if free_y:
                    fpool.put(y)
                ymix[chunk] = yn

            class TS_:
                pass

            def new_tile_state(t):
                st = TS_()
                st.t = t
                st.xn = None
                st.ymix = [None] * 8
                st.acc = [None] * 3
                st.ybs = [None] * 3
                st.ps1 = st.ps2 = None
                st.astats = 0
                st.y5 = None
                st.vnb = [None] * 4
                st.nproj = 0
                return st

            def u_rmsnorm(st):
                st.xn = [bpool.get() for _ in range(KC)]
                rmsnorm_T(hsrc(st.t), pcol(li, "norm_mix_g"), pk, [(x.ap, x.key) for x in st.xn], NT)
                return
                yield

            def u_A(st, c):
                xn = st.xn
                wt, co = wcol(c * 128)
                pv = proj(wt, co, 128, xn)
                wt, co = wcol(A_W + c * 128)
                pg = proj(wt, co, 128, xn)
                sg = fpool.get()
                ACT(sg.ap, pg.ap, AF.Sigmoid, [pg.key], [sg.key])
                ppool.put(pg)
                ak = ("abuf", c)
                TT(abuf[:, c, A_K - 1:A_K - 1 + NT], pv.ap, sg.ap, ALU.mult, [pv.key, sg.key], [ak])
                ppool.put(pv)
                fpool.put(sg)
                yield
                a = fpool.get()
                cw = pcol(li, "conv_a_w", c * A_K, A_K)
                psc = ppool.get()
                for k in range(A_K):
                    slot = dgcnt[0] % NDG
                    dgcnt[0] += 1
                    TS(dg[:, slot, :], ident_b[:, :], cw[:, k:k + 1], None, ALU.mult, None, ["ident_b", pk], [("dg", slot)])
                    sch.op("pe", lambda e, psc=psc, slot=slot, c=c, k=k: e.matmul(
                        psc.ap, dg[:, slot, :], abuf[:, c, k:k + NT], start=(k == 0), stop=(k == A_K - 1)),
                        [("dg", slot), ak], [psc.key])
                ACT(a.ap, psc.ap, AF.Identity, [psc.key, pk], [a.key], bias=pcol(li, "conv_a_b", c, 1))
                ppool.put(psc)
                ACT(abuf[:, c, 0:A_K - 1], abuf[:, c, NT:NT + A_K - 1], AF.Identity, [ak], [ak])
                yb_ = bpool.get()
                ys_ = bpool.get()
                ACT(yb_.ap, a.ap, AF.Identity, [a.key], [yb_.key])
                ACT(ys_.ap, a.ap, AF.Square, [a.key], [ys_.key])
                st.acc[c] = a
                st.ybs[c] = (yb_, ys_)
                st.astats += 1

            def u_Afin(st):
                assert st.astats == 3, "A conv not complete before A-fin"
                ps1, ps2 = ppool.get(), ppool.get()
                for c in range(3):
                    MM(ps1.ap, ones_b[:, :], st.ybs[c][0].ap, c == 0, c == 2, [st.ybs[c][0].key, "ones_b"], [ps1.key])
                for c in range(3):
                    MM(ps2.ap, ones_b[:, :], st.ybs[c][1].ap, c == 0, c == 2, [st.ybs[c][1].key, "ones_b"], [ps2.key])
                for c in range(3):
                    bpool.put(st.ybs[c][0])
                    bpool.put(st.ybs[c][1])
                mean, msq = fpool.get(), fpool.get()
                ACT(mean.ap, ps1.ap, AF.Identity, [ps1.key], [mean.key], scale=1.0 / A_W)
                ACT(msq.ap, ps1.ap, AF.Square, [ps1.key], [msq.key], scale=1.0 / A_W)
                ppool.put(ps1)
                STT(msq.ap, ps2.ap, 1.0 / A_W, msq.ap, ALU.mult, ALU.subtract, [ps2.key, msq.key], [msq.key])
                ppool.put(ps2)
                yield
                ACT(msq.ap, msq.ap, AF.Ln, [msq.key], [msq.key], bias=EPS)
                ACT(msq.ap, msq.ap, AF.Exp, [msq.key], [msq.key], scale=-0.5)
                yield
                for c in range(3):
                    a = st.acc[c]
                    TT(a.ap, a.ap, mean.ap, ALU.subtract, [a.key, mean.key], [a.key])
                    TT(a.ap, a.ap, msq.ap, ALU.mult, [a.key, msq.key], [a.key])
                fpool.put(mean)
                fpool.put(msq)
                yield
                for c in range(3):
                    a = st.acc[c]
                    ACT(a.ap, a.ap, AF.Silu, [a.key, pk], [a.key], scale=pcol(li, "ln_a_g", c, 1), bias=pcol(li, "ln_a_b", c, 1))
                gens = [gn_stages(st.acc[c], c, st.ymix) for c in range(3)]
                for g in gens:
                    next(g)
                yield
                for g in gens:
                    try:
                        next(g)
                    except StopIteration:
                        pass

            def u_B(st, j):
                xn = st.xn
                r = 128 if j < 2 else 64
                wt, co = wcol(2 * A_W + j * 128)
                pbg = proj(wt, co, r, xn)
                wt, co = wcol(2 * A_W + B_W + j * 128)
                pcg = proj(wt, co, r, xn)
                wt, co = wcol(2 * A_W + 2 * B_W + j * 128)
                pbi = proj(wt, co, r, xn)
                yield
                cg = fpool.get()
                ACT(cg.ap[0:r, :], pcg.ap[0:r, :], AF.Identity, [pcg.key], [cg.key])
                ppool.put(pcg)
                p = fpool.get()
                TT(p.ap[0:r, :], pbi.ap[0:r, :], cg.ap[0:r, :], ALU.mult, [pbi.key, cg.key], [p.key])
                ppool.put(pbi)
                a = cg
                cw = pcol(li, "conv_b_w", j * 3, 3)
                hk = ("bh", j)
                TS(a.ap[0:r, :], p.ap[0:r, :], cw[0:r, 2:3], None, ALU.mult, None, [p.key, pk], [a.key])
                STT(a.ap[0:r, 1:NT], p.ap[0:r, 0:NT - 1], cw[0:r, 1:2], a.ap[0:r, 1:NT], ALU.mult, ALU.add, [p.key, pk, a.key], [a.key])
                STT(a.ap[0:r, 0:1], bhist[0:r, j, 1:2], cw[0:r, 1:2], a.ap[0:r, 0:1], ALU.mult, ALU.add, [hk, "bhist", pk, a.key], [a.key])
                STT(a.ap[0:r, 2:NT], p.ap[0:r, 0:NT - 2], cw[0:r, 0:1], a.ap[0:r, 2:NT], ALU.mult, ALU.add, [p.key, pk, a.key], [a.key])
                STT(a.ap[0:r, 0:2], bhist[0:r, j, 0:2], cw[0:r, 0:1], a.ap[0:r, 0:2], ALU.mult, ALU.add, [hk, "bhist", pk, a.key], [a.key])
                ACT(bhist[0:r, j, 0:2], p.ap[0:r, NT - 2:NT], AF.Identity, [p.key], [hk])
                if j < 2:
                    y = p
                else:
                    st.y5 = fpool.get()
                    y = st.y5
                TT(y.ap[0:r, :], pbg.ap[0:r, :], a.ap[0:r, :], ALU.mult, [pbg.key, a.key], [y.key])
                ppool.put(pbg)
                fpool.put(a)
                if j < 2:
                    yield from gn_stages(y, 3 + j, st.ymix)
                else:
                    fpool.put(p)

            def gelu_stages(out_ap, out_key, src_ap, bank, shape_fn):
                t = fpool.get()
                tap = shape_fn(t.ap)
                ACT(out_ap, src_ap, AF.Identity, [bank.key], [out_key])
                ACT(tap, src_ap, AF.Square, [bank.key], [t.key])
                ppool.put(bank)
                yield
                TS(tap, tap, 0.044715, 1.0, ALU.mult, ALU.add, [t.key], [t.key])
                TT(tap, tap, out_ap, ALU.mult, [t.key, out_key], [t.key])
                yield
                ACT(tap, tap, AF.Sigmoid, [t.key], [t.key], scale=1.5957691216057308)
                yield
                TT(out_ap, tap, out_ap, ALU.mult, [t.key, out_key], [out_key])
                fpool.put(t)

            def u_V(st, blk):
                xn = st.xn
                wt4 = wb[4]
                pst = ppool.get()
                for k in range(KC):
                    MM(pst.ap[:, 0:C_W], xn[k].ap[:, blk * 128:(blk + 1) * 128], wt4.ap[:, k, 0:C_W], k == 0, k == KC - 1,
                       [wt4.key, xn[k].key], [pst.key])
                v = fpool.get()
                yield from gelu_stages(v.ap[:, 0:C_W], v.key, pst.ap[:, 0:C_W], pst, lambda ap: ap[:, 0:C_W])
                sk = ("small_v", blk)
                sm = small[:, 4 * blk:4 * blk + 4]
                s6 = stat6v[:, blk, :]
                sch.op("dve", lambda e, v=v, s6=s6: e.bn_stats(out=s6, in_=v.ap[:, 0:C_W]), [v.key], [sk])
                sch.op("dve", lambda e, sm=sm, s6=s6: e.bn_aggr(out=sm[:, 0:2], in_=s6), [sk], [sk])
                yield
                ACT(sm[:, 2:3], sm[:, 1:2], AF.Sqrt, [sk], [sk], bias=EPS)
                yield
                RECIP(sm[:, 2:3], sm[:, 2:3], [sk], [sk])
                TS(v.ap[:, 0:C_W], v.ap[:, 0:C_W], sm[:, 0:1], sm[:, 2:3], ALU.subtract, ALU.mult, [v.key, sk], [v.key])
                TT(v.ap[:, 0:C_W], v.ap[:, 0:C_W], pcol(li, "ln_c_g"), ALU.mult, [v.key, pk], [v.key])
                vb = bpool.get()
                TT(vb.ap[:, 0:C_W], v.ap[:, 0:C_W], pcol(li, "ln_c_b"), ALU.add, [v.key, pk], [vb.key])
                fpool.put(v)
                st.vnb[blk] = vb

            units_c = [(5, [(0, 64)]), (6, [(1, 0), (2, 64)]), (7, [(3, 0), (4, 64)])]
            ucol = {5: 2 * A_W + 3 * B_W, 6: 2 * A_W + 3 * B_W + 64, 7: 2 * A_W + 3 * B_W + 192}

            def u_U(st, ui, last_proj):
                xn = st.xn
                chunk, groups = units_c[ui]
                p0 = 64 if chunk == 5 else 0
                m = 64 if chunk == 5 else 128
                wt, co = wcol(ucol[chunk])
                pu = proj(wt, co, m, xn, prow=p0)
                if last_proj:
                    for x in xn:
                        bpool.put(x)
                u = fpool.get()
                yield from gelu_stages(u.ap[p0:128, :], u.key, pu.ap[p0:128, :], pu, lambda ap, p0=p0: ap[p0:128, :])
                yield
                vnb = st.vnb
                assert all(v is not None for v in vnb)
                pm = ppool.get()
                ng = len(groups) * (NT // 128)
                i = 0
                for blk in range(NT // 128):
                    for (g, prow) in groups:
                        i += 1
                        sch.op("pe", lambda e, pm=pm, prow=prow, blk=blk, g=g, vb=vnb[blk]: e.matmul(
                            pm.ap[prow:prow + 64, blk * 128:(blk + 1) * 128], vb.ap[:, g * 64:(g + 1) * 64],
                            wsT[:, g, :], start=True, stop=True), [vnb[blk].key, "wsT"], [pm.key], inc=(i == ng))
                if ui == 2:
                    for vb in vnb:
                        bpool.put(vb)
                y = st.y5 if chunk == 5 else fpool.get()
                bias = pcol(li, "gmlp_bias", ui * 128, 128)
                tmp = fpool.get()
                for blk in range(NT // 128):
                    TT(tmp.ap[p0:128, blk * 128:(blk + 1) * 128], pm.ap[p0:128, blk * 128:(blk + 1) * 128], bias[p0:128, :],
                       ALU.add, [pm.key, pk], [tmp.key])
                ppool.put(pm)
                TT(y.ap[p0:128, :], tmp.ap[p0:128, :], u.ap[p0:128, :], ALU.mult, [tmp.key, u.key], [y.key])
                fpool.put(tmp)
                fpool.put(u)
                yield from gn_stages(y, chunk, st.ymix)

            def u_outproj(st, ocs, last):
                assert all(y is not None for y in st.ymix), "ymix incomplete before out-proj"
                psts = []
                for oc in ocs:
                    wt = wo[oc // 4]
                    psts.append(proj(wt, (oc % 4) * 128, 128, st.ymix))
                if last:
                    for y in st.ymix:
                        bpool.put(y)
                yield
                for oc, pst in zip(ocs, psts):
                    resid_add(pst, oc, st.t)

            states = [new_tile_state(t) for t in range(NTILES)]
            run_unit(u_rmsnorm(states[0]))
            for t in range(NTILES):
                st = states[t]
                run_unit(u_A(st, 0))
                run_unit(u_A(st, 1))
                run_unit(u_A(st, 2))
                for blk in range(4):
                    run_unit(u_V(st, blk))
                if t > 0:
                    drain_for = states[t - 1]
                    while any(y is None for y in drain_for.ymix):
                        g = pending.popleft()
                        try:
                            next(g)
                            pending.append(g)
                        except StopIteration:
                            pass
                    run_unit(u_outproj(drain_for, [0, 1, 2], False))
                run_unit(u_B(st, 0))
                if t > 0:
                    run_unit(u_outproj(states[t - 1], [3, 4, 5], False))
                run_unit(u_B(st, 1))
                if t > 0:
                    run_unit(u_outproj(states[t - 1], [6, 7], True))
                run_unit(u_B(st, 2))
                if t + 1 < NTILES:
                    run_unit(u_rmsnorm(states[t + 1]))
                run_unit(u_Afin(st))
                run_unit(u_U(st, 0, False))
                run_unit(u_U(st, 1, False))
                run_unit(u_U(st, 2, True))
            drain()
            run_unit(u_outproj(states[NTILES - 1], [0, 1, 2], False))
            run_unit(u_outproj(states[NTILES - 1], [3, 4, 5], False))
            run_unit(u_outproj(states[NTILES - 1], [6, 7], True))
            drain()

        def attn_phase(li, l, b):
            pk = ("par", li)
            wkv = [wblock(w_xkv_d[li], 512 * i, 512) for i in range(4)]
            mT = [fpool.get() for _ in range(4)]
            assert MEM == 256

            for blk in range(MEM // 128):
                for half in range(2):
                    xt = fpool.get()
                    DMA("sp", xt.ap, mem_d[b, blk * 128:(blk + 1) * 128, half * 512:(half + 1) * 512],
                        "ld%d" % xt.key[1], [], [xt.key])
                    pst = ppool.get()
                    for j in range(4):
                        TR(pst.ap[:, j * 128:(j + 1) * 128], xt.ap[:, j * 128:(j + 1) * 128], [xt.key], [pst.key], inc=(j == 3))
                    fpool.put(xt)
                    for jj in range(2):
                        mt = mT[2 * half + jj]
                        ACT(mt.ap.rearrange("p (c m) -> p c m", c=2)[:, :, blk * 128:(blk + 1) * 128],
                            pst.ap[:, jj * 256:(jj + 1) * 256].rearrange("p (c m) -> p c m", c=2), AF.Identity, [pst.key], [mt.key])
                    ppool.put(pst)
            mn = [bpool.get() for _ in range(4)]
            msrc = [(mT[c // 2].ap[:, (c % 2) * MEM:(c % 2 + 1) * MEM], mT[c // 2].key) for c in range(KC)]
            mdst_ = [(mn[c // 2].ap[:, (c % 2) * MEM:(c % 2 + 1) * MEM], mn[c // 2].key) for c in range(KC)]
            rmsnorm_T(msrc, pcol(li, "norm_mem_g"), pk, mdst_, MEM)
            for m_ in mT:
                fpool.put(m_)
            KT = [bpool.get() for _ in range(4)]
            for dc in range(KC):
                wt = wkv[dc // 4]
                pst = ppool.get()
                for k in range(KC):
                    MM(pst.ap[:, 0:MEM], wt.ap[:, k, (dc % 4) * 128:(dc % 4 + 1) * 128], mdst_[k][0], k == 0, k == KC - 1,
                       [wt.key, mdst_[k][1]], [pst.key])
                kt = KT[dc // 2]
                ACT(kt.ap[:, (dc % 2) * MEM:(dc % 2 + 1) * MEM], pst.ap[:, 0:MEM], AF.Identity, [pst.key], [kt.key])
                ppool.put(pst)
            V = [bpool.get() for _ in range(4)]
            for mb in range(2):
                for half in range(2):
                    wt = wkv[2 + half]
                    pst = ppool.get()
                    for k in range(KC):
                        MM(pst.ap, mdst_[k][0][:, mb * 128:(mb + 1) * 128], wt.ap[:, k, :], k == 0, k == KC - 1,
                           [wt.key, mdst_[k][1]], [pst.key])
                    vt = V[2 * mb + half]
                    ACT(vt.ap, pst.ap, AF.Identity, [pst.key], [vt.key])
                    ppool.put(pst)
            for m_ in mn:
                bpool.put(m_)
            wq = [wblock(w_xq_d[li], 512 * i, 512) for i in range(2)]
            wo = [wblock(w_xo_d[li], 512 * i, 512) for i in range(2)]

            for t in range(NTILES):
                xn = [bpool.get() for _ in range(KC)]
                rmsnorm_T(hsrc(t), pcol(li, "norm_x_g"), pk, [(x.ap, x.key) for x in xn], NT)
                qT = []
                for dc in range(KC):
                    pst = proj(wq[dc // 4], (dc % 4) * 128, 128, xn)
                    q = bpool.get()
                    ACT(q.ap, pst.ap, AF.Identity, [pst.key], [q.key], scale=1.0 / 16.0)
                    ppool.put(pst)
                    qT.append(q)
                for x in xn:
                    bpool.put(x)
                oT = []
                for h in range(4):
                    PT = []
                    for mb in range(2):
                        pst = ppool.get()
                        for dcc in range(2):
                            dc = 2 * h + dcc
                            kt = KT[dc // 2]
                            MM(pst.ap, kt.ap[:, (dc % 2) * MEM + mb * 128:(dc % 2) * MEM + (mb + 1) * 128], qT[dc].ap,
                               dcc == 0, dcc == 1, [kt.key, qT[dc].key], [pst.key])
                        p_ = bpool.get()
                        ACT(p_.ap, pst.ap, AF.Exp, [pst.key], [p_.key])
                        ppool.put(pst)
                        PT.append(p_)
                    pss = ppool.get()
                    for mb in range(2):
                        MM(pss.ap, ones_b[:, :], PT[mb].ap, mb == 0, mb == 1, [PT[mb].key, "ones_b"], [pss.key])
                    rs = fpool.get()
                    RECIP(rs.ap, pss.ap, [pss.key], [rs.key])
                    ppool.put(pss)
                    for dcc in range(2):
                        dc = 2 * h + dcc
                        pst = ppool.get()
                        for mb in range(2):
                            vt = V[2 * mb + dc // 4]
                            MM(pst.ap, vt.ap[:, (dc % 4) * 128:(dc % 4 + 1) * 128], PT[mb].ap, mb == 0, mb == 1,
                               [vt.key, PT[mb].key], [pst.key])
                        o = bpool.get()
                        TT(o.ap, pst.ap, rs.ap, ALU.mult, [pst.key, rs.key], [o.key])
                        ppool.put(pst)
                        oT.append(o)
                    fpool.put(rs)
                    for p_ in PT:
                        bpool.put(p_)
                for q in qT:
                    bpool.put(q)
                for oc in range(KC):
                    pst = proj(wo[oc // 4], (oc % 4) * 128, 128, oT)
                    resid_add(pst, oc, t)
                for o in oT:
                    bpool.put(o)
            for kt in KT:
                bpool.put(kt)
            for vt in V:
                bpool.put(vt)

        lbcnt = [0]
        dgcnt = [0]

        def ffn_phase(li, l, tail_hook=None):
            pk = ("par", li)
            moe = (l % 2 == 1)
            hn = [[None] * NTILES for _ in range(KC)]
            rstds = []
            for t in range(NTILES):
                tl = [bpool.get() for _ in range(KC)]
                r = rmsnorm_T(hsrc(t), pcol(li, "norm_ffn_g"), pk, [(x.ap, x.key) for x in tl], NT, keep_rstd=moe)
                for c in range(KC):
                    hn[c][t] = tl[c]
                if moe:
                    router(li, t, r)
                    fpool.put(r)

            deferred = []

            def slice_compute(wg, wu, wd, nfc, bc, after_tile=None):
                for t in range(NTILES):
                    xn = [hn[c][t] for c in range(KC)]
                    aT = []
                    for fc in range(nfc):
                        pg = proj(wg, fc * 128, 128, xn)
                        pu = proj(wu, fc * 128, 128, xn)
                        sg = fpool.get()
                        ACT(sg.ap, pg.ap, AF.Silu, [pg.key], [sg.key])
                        ppool.put(pg)
                        if bc is not None:
                            TT(sg.ap, sg.ap, bc[t].ap, ALU.mult, [sg.key, bc[t].key], [sg.key])
                        a = bpool.get()
                        TT(a.ap, pu.ap, sg.ap, ALU.mult, [pu.key, sg.key], [a.key])
                        ppool.put(pu)
                        fpool.put(sg)
                        aT.append(a)

                    def down(t=t, aT=aT, wd=wd, nfc=nfc, after_tile=after_tile):
                        for oc in range(KC):
                            pst = ppool.get()
                            for fc in range(nfc):
                                MM(pst.ap, wd.ap[:, fc, oc * 128:(oc + 1) * 128], aT[fc].ap, fc == 0, fc == nfc - 1,
                                   [wd.key, aT[fc].key], [pst.key])
                            resid_add(pst, oc, t)
                        for a in aT:
                            bpool.put(a)
                        if after_tile is not None:
                            after_tile(t)

                    if deferred:
                        deferred.pop()()
                    deferred.append(down)

            if not moe:
                Wg, Wu, Wd = fg_d[0], fu_d[0], fd_d[0]
                f0 = 0
                while f0 < DFF:
                    nf = min(512, DFF - f0)
                    wg = wblock(Wg, f0, nf)
                    wu = wblock(Wu, f0, nf)
                    wd = wblock_down(Wd, f0, nf)
                    slice_compute(wg, wu, wd, nf // 128, None)
                    f0 += nf
            else:
                for e in range(N_EXP):
                    bc = []
                    for t in range(NTILES):
                        pst = ppool.get()
                        for blk in range(NT // 128):
                            gi = (t * (NT // 128) + blk) * 8 + e
                            lb = lbcnt[0] % 2
                            lbcnt[0] += 1
                            sch.op("dve", lambda ee, lb=lb, gi=gi: ee.tensor_copy(
                                out=lbc[:, lb, :], in_=comb_tm[:, gi:gi + 1].to_broadcast([128, 128])), [("comb", t)], [("lbc", lb)])
                            sch.op("pe", lambda ee, pst=pst, lb=lb, blk=blk: ee.matmul(
                                pst.ap[:, blk * 128:(blk + 1) * 128], lbc[:, lb, :], ident[:, :], start=True, stop=True),
                                [("lbc", lb), "ident"], [pst.key])
                        bt = fpool.get()
                        ACT(bt.ap, pst.ap, AF.Identity, [pst.key], [bt.key])
                        ppool.put(pst)
                        bc.append(bt)
                    Wg, Wu, Wd = mg_d[0, e], mu_d[0, e], md_d[0, e]
                    f0 = 0
                    while f0 < DEXP:
                        nf = min(512, DEXP - f0)
                        wg = wblock(Wg, f0, nf)
                        wu = wblock(Wu, f0, nf)
                        wd = wblock_down(Wd, f0, nf)
                        last_slice = (e == N_EXP - 1) and (f0 + nf >= DEXP)
                        if last_slice and tail_hook is not None:
                            def _after(t, bc=bc):
                                if bc[t] is not None:
                                    fpool.put(bc[t])
                                    bc[t] = None
                                tail_hook(t)
                            slice_compute(wg, wu, wd, nf // 128, bc, _after)
                        else:
                            slice_compute(wg, wu, wd, nf // 128, bc)
                        f0 += nf
                    for i_, bt in enumerate(bc):
                        if bt is not None:
                            fpool.put(bt)
                            bc[i_] = None
            if deferred:
                deferred.pop()()
            for c in range(KC):
                for t in range(NTILES):
                    bpool.put(hn[c][t])

        def router(li, t, rstd):
            pk = ("par", li)
            if t == 0:
                for k in range(KC):
                    TS(gw[:, k * 8:(k + 1) * 8], pcol(li, "router", k * 8, 8), pcol(li, "norm_ffn_g", k, 1), None, ALU.mult, None,
                       [pk], ["gw"])
            for blk in range(NT // 128):
                tok = slice(t * NT + blk * 128, t * NT + (blk + 1) * 128)
                pst = ppool.get()
                for k in range(KC):
                    MM(pst.ap[:, 0:8], hT[:, k, tok], gw[:, k * 8:(k + 1) * 8], k == 0, k == KC - 1, [hkey(k, t), "gw"], [pst.key])
                sch.op("pe", lambda e, pst=pst, blk=blk: e.matmul(
                    pst.ap[:, 8:9], rstd.ap[0:1, blk * 128:(blk + 1) * 128], ones_f[0:1, 0:1], start=True, stop=True),
                    [rstd.key, "ones_f"], [pst.key])
                lg = small[:, 8:16]
                TS(lg, pst.ap[:, 0:8], pst.ap[:, 8:9], None, ALU.mult, None, [pst.key], ["small"])
                ppool.put(pst)
                sch.op("dve", lambda e: e.max(out=small[:, 16:24], in_=small[:, 8:16]), ["small"], ["small"])
                TS(small[:, 24:32], lg, small[:, 17:18], None, ALU.is_ge, None, ["small"], ["small"])
                TS(small[:, 32:33], small[:, 16:17], -1.0, None, ALU.mult, None, ["small"], ["small"])
                ACT(small[:, 40:48], lg, AF.Exp, ["small"], ["small"], bias=small[:, 32:33])
                STT(small[:, 48:56], small[:, 40:48], 1.0, small[:, 24:32], ALU.mult, ALU.mult, ["small"], ["small"],
                    accum_out=small[:, 33:34])
                RECIP(small[:, 34:35], small[:, 33:34], ["small"], ["small"])
                gi = (t * (NT // 128) + blk) * 8
                TS(comb_tm[:, gi:gi + 8], small[:, 48:56], small[:, 34:35], None, ALU.mult, None, ["small"], [("comb", t)])

        def final_phase(b, tiles=None):
            for t in (range(NTILES) if tiles is None else tiles):
                rstd = rmsnorm_T(hsrc(t), fing, "fing", None, NT, keep_rstd=True)
                for half in range(2):
                    yT = []
                    for j in range(4):
                        c = 4 * half + j
                        y = fpool.get()
                        STT(y.ap, hT[:, c, t * NT:(t + 1) * NT], fing[:, c:c + 1], rstd.ap, ALU.mult, ALU.mult,
                            [hkey(c, t), rstd.key, "fing"], [y.key])
                        yT.append(y)
                    for blk in range(NT // 128):
                        pst = ppool.get()
                        for j in range(4):
                            TR(pst.ap[:, j * 128:(j + 1) * 128], yT[j].ap[:, blk * 128:(blk + 1) * 128], [yT[j].key], [pst.key], inc=(j == 3))
                        ot = fpool.get()
                        ACT(ot.ap, pst.ap, AF.Identity, [pst.key], [ot.key])
                        ppool.put(pst)
                        r0 = t * NT + blk * 128
                        DMA("sp", out_d[b, r0:r0 + 128, half * 512:(half + 1) * 512], ot.ap, "st%d" % ot.key[1], [ot.key], [])
                        fpool.put(ot)
                    for y in yT:
                        fpool.put(y)
                fpool.put(rstd)

        def hdst(half, blk):
            return (hT[:, 4 * half:4 * half + 4, blk * 128:(blk + 1) * 128],
                    [hkey(4 * half + j, (blk * 128) // NT) for j in range(4)])

        stages = getattr(cfg, "stages", ("mix", "attn", "ffn"))
        n_l = len(cfg.layers)
        tail_ok = ("ffn" in stages) and (cfg.layers[-1] % 2 == 1) and getattr(cfg, "tail_overlap", True)
        preloaded = False
        for b in range(NB):
            if not preloaded:
                load_T(x_d[b], S, hdst)
            preloaded = False

            def tail_hook(t, b=b):
                final_phase(b, [t])
                if b + 1 < NB:
                    load_T(x_d[b + 1], S, hdst, blks=range(t * (NT // 128), (t + 1) * (NT // 128)))

            for li, l in enumerate(cfg.layers):
                if "mix" in stages:
                    mixer_phase(li, l)
                if "attn" in stages:
                    attn_phase(li, l, b)
                if "ffn" in stages:
                    ffn_phase(li, l, tail_hook if (tail_ok and li == n_l - 1) else None)
            if tail_ok:
                preloaded = (b + 1 < NB)
            else:
                final_phase(b)
        sch.barrier_wait("sp", [k for k in sch.cnt if k.startswith("st")])

        sch.check_deadlock()
        with nc.Block() as block:
            def emit(engname, e):
                for (waits, fn, incsem, amt) in sch.ops[engname]:
                    for (s, v) in waits:
                        e.wait_ge(sems[s], v)
                    if fn is not None:
                        ins = fn(e)
                        if incsem is not None:
                            ins.then_inc(sems[incsem], amt)

            @block.tensor
            def _(e):
                emit("pe", e)

            @block.scalar
            def _(e):
                emit("act", e)

            @block.vector
            def _(e):
                emit("dve", e)

            @block.gpsimd
            def _(e):
                emit("pool", e)

            @block.sync
            def _(e):
                emit("sp", e)
    build_nc.last_nops = sch.n_ops
    return nc


def make_in_maps(cfg, inp):
    x = np.asarray(inp["x"], np.float32)
    mem = np.asarray(inp["mem"], np.float32)
    L = len(cfg.layers)
    params = np.stack([_build_params(inp, l) for l in cfg.layers])
    wsT = np.stack([np.ascontiguousarray(np.asarray(inp["gmlp_ws"][l], np.float32).transpose(2, 0, 1)).reshape(128, 5 * 128)
                    for l in cfg.layers])
    ident = _build_consts()
    lay = list(cfg.layers)
    shared = {
        "params": params, "wsT": wsT, "fin_g": _colmajor(inp["norm_final_g"]), "ident": ident,
        "w_in": np.asarray(inp["w_in"], np.float32)[lay], "w_mix_out": np.asarray(inp["w_mix_out"], np.float32)[lay],
        "w_xq": np.asarray(inp["w_xq"], np.float32)[lay], "w_xkv": np.asarray(inp["w_xkv"], np.float32)[lay],
        "w_xo": np.asarray(inp["w_xo"], np.float32)[lay],
    }
    if any(l % 2 == 0 for l in cfg.layers):
        shared["ffn_w_gate"] = np.asarray(inp["ffn_w_gate"], np.float32)
        shared["ffn_w_up"] = np.asarray(inp["ffn_w_up"], np.float32)
        shared["ffn_w_down"] = np.asarray(inp["ffn_w_down"], np.float32)
    if any(l % 2 == 1 for l in cfg.layers):
        shared["moe_w_gate"] = np.asarray(inp["moe_w_gate"], np.float32)
        shared["moe_w_up"] = np.asarray(inp["moe_w_up"], np.float32)
        shared["moe_w_down"] = np.asarray(inp["moe_w_down"], np.float32)
    maps = []
    for c in range(cfg.ncores):
        m = dict(shared)
        m["x"] = np.ascontiguousarray(x[c * cfg.NB:(c + 1) * cfg.NB])
        m["mem"] = np.ascontiguousarray(mem[c * cfg.NB:(c + 1) * cfg.NB])
        maps.append(m)
    return maps


def run(cfg, inp, trace=False):
    nc = build_nc(cfg)
    maps = make_in_maps(cfg, inp)
    res = run_bass_kernel_spmd(nc, maps, core_ids=list(range(cfg.ncores)), trace=trace)
    out = np.concatenate([np.asarray(r["out"]) for r in res.results], axis=0)
    return out.astype(np.float32), res


def kernel(**inputs):
    cfg = Cfg()
    out, _ = run(cfg, inputs)
    return out
```

```python
import numpy as np
import concourse.bass as bass
import concourse.mybir as mybir
from concourse.bass_utils import run_bass_kernel_spmd

F32 = mybir.dt.float32
BF16 = mybir.dt.bfloat16
AF = mybir.ActivationFunctionType
ALU = mybir.AluOpType
AX = mybir.AxisListType

D = 1024
KC = 8
A_W, B_W, C_W = 384, 320, 320
PROJ = 2368
A_K = 31
EPS = 1e-6
NT = 512
N_EXP = 8


class Cfg:
    def __init__(self, S=2048, NB=2, MEM=256, DFF=2816, DEXP=3584, layers=(0, 1), ncores=8,
                 nslot=7, nf32=12, nb16x=8):
        self.S, self.NB, self.MEM, self.DFF, self.DEXP = S, NB, MEM, DFF, DEXP
        self.layers = tuple(layers)
        self.ncores = ncores
        self.nslot, self.nf32, self.nb16x = nslot, nf32, nb16x


def _colmajor(v):
    v = np.asarray(v, np.float32).reshape(-1)
    n = v.shape[0]
    c = (n + 127) // 128
    buf = np.zeros((c * 128,), np.float32)
    buf[:n] = v
    return np.ascontiguousarray(buf.reshape(c, 128).T)


PCOLS = {}


def _param_layout():
    off = 0
    def add(name, n):
        nonlocal off
        PCOLS[name] = (off, n)
        off += n
    add("norm_mix_g", 8); add("norm_x_g", 8); add("norm_mem_g", 8); add("norm_ffn_g", 8)
    add("mix_out_g", 8)
    add("conv_a_w", 3 * A_K); add("conv_a_b", 3); add("ln_a_g", 3); add("ln_a_b", 3)
    add("conv_b_w", 9)
    add("router", 64)
    add("ln_c_g", C_W); add("ln_c_b", C_W)
    add("gmlp_bias", 3 * 128)
    return off


NPCOL = _param_layout()


def _build_params(inp, layer):
    P = np.zeros((128, NPCOL), np.float32)
    def put(name, arr):
        o, n = PCOLS[name]
        assert arr.shape == (128, n), (name, arr.shape, n)
        P[:, o:o + n] = arr
    put("norm_mix_g", _colmajor(inp["norm_mix_g"][layer]))
    put("norm_x_g", _colmajor(inp["norm_x_g"][layer]))
    put("norm_mem_g", _colmajor(inp["norm_mem_g"][layer]))
    put("norm_ffn_g", _colmajor(inp["norm_ffn_g"][layer]))
    put("mix_out_g", _colmajor(inp["mix_out_g"][layer]))
    caw = np.asarray(inp["conv_a_w"][layer], np.float32)
    put("conv_a_w", np.concatenate([caw[:, c * 128:(c + 1) * 128].T for c in range(3)], axis=1))
    put("conv_a_b", _colmajor(inp["conv_a_b"][layer]))
    put("ln_a_g", _colmajor(inp["ln_a_g"][layer]))
    put("ln_a_b", _colmajor(inp["ln_a_b"][layer]))
    cbw = np.asarray(inp["conv_b_w"][layer], np.float32)
    cb = np.zeros((128, 9), np.float32)
    for j in range(3):
        r = min(128, B_W - j * 128)
        cb[:r, j * 3:(j + 1) * 3] = cbw[:, j * 128:j * 128 + r].T
    put("conv_b_w", cb)
    if layer % 2 == 1:
        wr = np.asarray(inp["moe_router"][layer // 2], np.float32)
        put("router", np.ascontiguousarray(wr.reshape(8, 128, 8).transpose(1, 0, 2)).reshape(128, 64))
    put("ln_c_g", np.broadcast_to(np.asarray(inp["ln_c_g"][layer], np.float32)[None, :], (128, C_W)))
    put("ln_c_b", np.broadcast_to(np.asarray(inp["ln_c_b"][layer], np.float32)[None, :], (128, C_W)))
    gb = np.asarray(inp["gmlp_b"][layer], np.float32)
    gbt = np.zeros((128, 3, 128), np.float32)
    gbt[64:128, 0, :] = gb[0][None, :]
    gbt[0:64, 1, :] = gb[1][None, :]
    gbt[64:128, 1, :] = gb[2][None, :]
    gbt[0:64, 2, :] = gb[3][None, :]
    gbt[64:128, 2, :] = gb[4][None, :]
    put("gmlp_bias", gbt.reshape(128, 384))
    return P


def _build_consts():
    ident = np.eye(128, dtype=np.float32)
    return ident


class Sched:
    ENGS = ("pe", "act", "dve", "pool", "sp")

    def __init__(self):
        self.ops = {e: [] for e in self.ENGS}
        self.cnt = {}
        self.seen = {e: {} for e in self.ENGS}
        self.lastw = {}
        self.readers = {}
        self.n_ops = 0

    def _need(self, eng, deps):
        waits = {}
        for (sem, val) in deps:
            if val <= self.seen[eng].get(sem, 0):
                continue
            if val > waits.get(sem, 0):
                waits[sem] = val
        for sem, val in waits.items():
            self.seen[eng][sem] = val
        return list(waits.items())

    def op(self, eng, fn, reads=(), writes=(), inc=True, sem=None, amt=1):
        if sem is None:
            sem = eng
        deps = []
        for k in reads:
            w = self.lastw.get(k)
            if w is not None:
                deps.append(w)
        for k in writes:
            w = self.lastw.get(k)
            if w is not None:
                deps.append(w)
            deps.extend(self.readers.get(k, ()))
        if eng == "pe":
            deps = [d for d in deps if d[0] != "pe"]
        waits = self._need(eng, deps)
        cur = self.cnt.get(sem, 0)
        val = cur + amt
        if inc:
            self.cnt[sem] = val
        for k in reads:
            self.readers.setdefault(k, []).append((sem, val))
        for k in writes:
            self.lastw[k] = (sem, val)
            self.readers[k] = []
        self.ops[eng].append((waits, fn, sem if inc else None, amt))
        self.n_ops += 1

    def check_deadlock(self):
        cnt = {}
        pos = {e: 0 for e in self.ENGS}
        progress = True
        while progress:
            progress = False
            for e in self.ENGS:
                q = self.ops[e]
                while pos[e] < len(q):
                    waits, fn, incsem, amt = q[pos[e]]
                    if any(cnt.get(s_, 0) < v for (s_, v) in waits):
                        break
                    if incsem is not None:
                        cnt[incsem] = cnt.get(incsem, 0) + amt
                    pos[e] += 1
                    progress = True
        stuck = {e: (pos[e], len(self.ops[e])) for e in self.ENGS if pos[e] < len(self.ops[e])}
        if stuck:
            msg = []
            for e, (p_, n_) in stuck.items():
                waits = self.ops[e][p_][0]
                msg.append(f"{e} stuck at {p_}/{n_} waiting {[(s_, v, cnt.get(s_, 0)) for (s_, v) in waits]}")
            raise RuntimeError("DEADLOCK in schedule: " + "; ".join(msg))

    def barrier_wait(self, eng, sems):
        deps = [(s, self.cnt.get(s, 0)) for s in sems]
        waits = self._need(eng, deps)
        if waits:
            self.ops[eng].append((waits, None, None, 0))


class Pool:
    def __init__(self, items):
        self.free_list = list(items)

    def get(self):
        assert self.free_list, "pool exhausted"
        return self.free_list.pop(0)

    def put(self, t):
        self.free_list.append(t)


class Tile:
    def __init__(self, ap, key):
        self.ap = ap
        self.key = key


def build_nc(cfg):
    nc = bass.Bass("TRN2", target_bir_lowering=False)
    S, NB, MEM = cfg.S, cfg.NB, cfg.MEM
    NTILES = S // NT
    L = len(cfg.layers)
    has_dense = any(l % 2 == 0 for l in cfg.layers)
    has_moe = any(l % 2 == 1 for l in cfg.layers)
    DFF, DEXP = cfg.DFF, cfg.DEXP

    def dram(name, shape, kind="ExternalInput"):
        return nc.dram_tensor(name, list(shape), F32, kind=kind).ap()

    x_d = dram("x", [NB, S, D])
    mem_d = dram("mem", [NB, MEM, D])
    par_d = dram("params", [L, 128, NPCOL])
    wst_d = dram("wsT", [L, 128, 5 * 128])
    fin_d = dram("fin_g", [128, 8])
    ident_d = dram("ident", [128, 128])
    w_in_d = dram("w_in", [L, D, PROJ])
    w_mo_d = dram("w_mix_out", [L, D, D])
    w_xq_d = dram("w_xq", [L, D, D])
    w_xkv_d = dram("w_xkv", [L, D, 2 * D])
    w_xo_d = dram("w_xo", [L, D, D])
    if has_dense:
        fg_d = dram("ffn_w_gate", [1, D, DFF])
        fu_d = dram("ffn_w_up", [1, D, DFF])
        fd_d = dram("ffn_w_down", [1, DFF, D])
    if has_moe:
        mg_d = dram("moe_w_gate", [1, N_EXP, D, DEXP])
        mu_d = dram("moe_w_up", [1, N_EXP, D, DEXP])
        md_d = dram("moe_w_down", [1, N_EXP, DEXP, D])
    out_d = dram("out", [NB, S, D], kind="ExternalOutput")

    from contextlib import ExitStack
    es = ExitStack()
    with es:
        def sb(name, shape, dt):
            return es.enter_context(nc.sbuf_tensor(name, list(shape), dt))

        def ps(name):
            return es.enter_context(nc.psum_tensor(name, [128, NT], F32))

        hT = sb("hT", [128, KC, S], F32)
        NB16 = KC * NTILES + cfg.nb16x
        b16 = sb("b16", [128, NB16, NT], BF16)
        f32p = sb("f32p", [128, cfg.nf32, NT], F32)
        ring = sb("ring", [128, cfg.nslot, 4096], BF16)
        abuf = sb("abuf", [128, 3, NT + A_K - 1], BF16)
        NDG = 8
        dg = sb("dg", [128, NDG, 128], BF16)
        ident_b = sb("ident_b", [128, 128], BF16)
        bhist = sb("bhist", [128, 3, 2], F32)
        par = sb("par", [128, L, NPCOL], F32)
        fing = sb("fing", [128, 8], F32)
        wsT = sb("wsTs", [128, 5, 128], BF16)
        ident = sb("identf", [128, 128], F32)
        ones_b = sb("ones_b", [128, 128], BF16)
        bdiag = sb("bdiag", [128, 128], BF16)
        ones_f = sb("ones_f", [128, 128], F32)
        comb_tm = sb("comb_tm", [128, (S // 128) * 8], F32)
        lbc = sb("lbc", [128, 2, 128], F32)
        gw = sb("gw", [128, 64], F32)
        small = sb("small", [128, 64], F32)
        stat6 = sb("stat6", [128, 8], F32)
        stat6v = sb("stat6v", [128, 4, 6], F32)
        banks = [ps(f"bank{i}") for i in range(8)]

        sems = {}
        def sem(name):
            if name not in sems:
                sems[name] = es.enter_context(nc.semaphore(name))
            return sems[name]

        for e in Sched.ENGS:
            sem(e)

        sch = Sched()
        bpool = Pool([Tile(b16[:, i, :], ("b", i)) for i in range(NB16)])
        fpool = Pool([Tile(f32p[:, i, :], ("f", i)) for i in range(cfg.nf32)])
        ppool = Pool([Tile(banks[i][:, :], ("ps", i)) for i in range(8)])

        def ACT(out, in_, func, reads, writes, **kw):
            sch.op("act", lambda e: e.activation(out=out, in_=in_, func=func, **kw), reads, writes)

        def TT(out, in0, in1, op, reads, writes):
            sch.op("dve", lambda e: e.tensor_tensor(out=out, in0=in0, in1=in1, op=op), reads, writes)

        def TS(out, in0, s1, s2, op0, op1, reads, writes, **kw):
            if s2 is None:
                sch.op("dve", lambda e: e.tensor_scalar(out=out, in0=in0, scalar1=s1, scalar2=None, op0=op0, **kw),
                       reads, writes)
            else:
                sch.op("dve", lambda e: e.tensor_scalar(out=out, in0=in0, scalar1=s1, scalar2=s2, op0=op0, op1=op1, **kw),
                       reads, writes)

        def STT(out, in0, scalar, in1, op0, op1, reads, writes, **kw):
            sch.op("dve", lambda e: e.scalar_tensor_tensor(out=out, in0=in0, scalar=scalar, in1=in1, op0=op0, op1=op1, **kw),
                   reads, writes)

        def RECIP(out, in_, reads, writes):
            sch.op("dve", lambda e: e.reciprocal(out=out, in_=in_), reads, writes)

        def MM(out, lhsT, rhs, start, stop, reads, writes, inc_all=False):
            sch.op("pe", lambda e: e.matmul(out, lhsT, rhs, start=start, stop=stop), reads, writes, inc=(stop or inc_all))

        def TR(out, in_, reads, writes, inc=True):
            sch.op("pe", lambda e: e.transpose(out, in_, ident[:, :]), list(reads) + ["ident"], writes, inc=inc)

        def DMA(eng, out, in_, semname, reads, writes, **kw):
            sem(semname)
            sch.op(eng, lambda e: e.dma_start(out=out, in_=in_, **kw), reads, writes, sem=semname, amt=16)

        def hkey(c, t):
            return ("h", c, t)

        def pcol(l, name, c0=0, n=None):
            o, nn = PCOLS[name]
            if n is None:
                n = nn - c0
            return par[:, l, o + c0:o + c0 + n]

        class Ring:
            def __init__(self):
                self.n = cfg.nslot
                self.next = 0
                self.loads = [0] * self.n

            def load(self, src_ap, view):
                i = self.next
                self.next = (self.next + 1) % self.n
                a, b = src_ap.shape[1], src_ap.shape[2]
                if view == "k":
                    dst = ring[:, i, :].rearrange("p (k n) -> p k n", k=8)[:, 0:a, 0:b]
                    full = ring[:, i, :].rearrange("p (k n) -> p k n", k=8)
                else:
                    dst = ring[:, i, :].rearrange("p (k n) -> p k n", k=4)[:, 0:a, 0:b]
                    full = ring[:, i, :].rearrange("p (k n) -> p k n", k=4)
                key = ("w", i)
                DMA("pool", dst, src_ap, f"w{i}", [], [key])
                return Tile(full, key)

        wring = Ring()

        def wblock(W2d, c0, ncols):
            src = W2d[:, c0:c0 + ncols].rearrange("(k p) n -> p k n", p=128)
            return wring.load(src, "k")

        def wblock_down(W2d, f0, nf):
            src = W2d[f0:f0 + nf, :].rearrange("(k p) n -> p k n", p=128)
            return wring.load(src, "d")

        DMA("sp", ident[:, :], ident_d[:, :], "cst", [], ["ident"])
        DMA("sp", fing[:, :], fin_d[:, :], "cst", [], ["fing"])
        for li in range(L):
            DMA("sp", par[:, li, :], par_d[li], "cst", [], [("par", li)])
        for k in ["ident", "fing"] + [("par", li) for li in range(L)]:
            sch.lastw[k] = ("cst", sch.cnt["cst"])
        sch.op("dve", lambda e: e.tensor_copy(out=ident_b[:, :], in_=ident[:, :]), ["ident"], ["ident_b"])
        sch.op("dve", lambda e: e.memset(ones_b[:, :], 1.0), [], ["ones_b"])
        sch.op("dve", lambda e: e.memset(ones_f[:, :], 1.0), [], ["ones_f"])
        sch.op("dve", lambda e: e.memset(bdiag[:, :], 0.0), [], ["bdiag"])
        sch.op("dve", lambda e: e.memset(bdiag[0:64, 0:64], 1.0), [], ["bdiag"])
        sch.op("dve", lambda e: e.memset(bdiag[64:128, 64:128], 1.0), [], ["bdiag"])

        def rmsnorm_T(src, gcol, gkey, dst, n, keep_rstd=False):
            pst = ppool.get()
            for c in range(KC):
                sq = bpool.get()
                ACT(sq.ap[:, :n], src[c][0], AF.Square, [src[c][1]], [sq.key])
                MM(pst.ap[:, :n], ones_b[:, :], sq.ap[:, :n], c == 0, c == KC - 1, [sq.key, "ones_b"], [pst.key], inc_all=True)
                bpool.put(sq)
            rstd = fpool.get()
            ACT(rstd.ap[:, :n], pst.ap[:, :n], AF.Ln, [pst.key], [rstd.key], scale=1.0 / D, bias=EPS)
            ACT(rstd.ap[:, :n], rstd.ap[:, :n], AF.Exp, [rstd.key], [rstd.key], scale=-0.5)
            ppool.put(pst)
            if dst is not None:
                for c in range(KC):
                    STT(dst[c][0], src[c][0], gcol[:, c:c + 1], rstd.ap[:, :n], ALU.mult, ALU.mult,
                        [src[c][1], rstd.key, gkey], [dst[c][1]])
            if keep_rstd:
                return rstd
            fpool.put(rstd)
            return None

        def load_T(src2d, nrows, dst_fn, blks=None):
            for blk in (range(nrows // 128) if blks is None else blks):
                for half in range(2):
                    xt = fpool.get()
                    DMA("sp", xt.ap, src2d[blk * 128:(blk + 1) * 128, half * 512:(half + 1) * 512], "ld%d" % xt.key[1],
                        [], [xt.key])
                    pst = ppool.get()
                    for j in range(4):
                        TR(pst.ap[:, j * 128:(j + 1) * 128], xt.ap[:, j * 128:(j + 1) * 128], [xt.key], [pst.key], inc=(j == 3))
                    fpool.put(xt)
                    dap, dkeys = dst_fn(half, blk)
                    ACT(dap, pst.ap.rearrange("p (j n) -> p j n", j=4), AF.Identity, [pst.key], dkeys)
                    ppool.put(pst)

        def hsrc(t):
            return [(hT[:, c, t * NT:(t + 1) * NT], hkey(c, t)) for c in range(KC)]

        def groupnorm(y, li, chunk):
            sq = bpool.get()
            ACT(sq.ap, y.ap, AF.Square, [y.key], [sq.key])
            pst = ppool.get()
            MM(pst.ap, bdiag[:, :], sq.ap, True, True, [sq.key, "bdiag"], [pst.key])
            bpool.put(sq)
            rstd = fpool.get()
            ACT(rstd.ap, pst.ap, AF.Ln, [pst.key], [rstd.key], scale=1.0 / 64, bias=EPS)
            ACT(rstd.ap, rstd.ap, AF.Exp, [rstd.key], [rstd.key], scale=-0.5)
            ppool.put(pst)
            yn = bpool.get()
            STT(yn.ap, y.ap, pcol(li, "mix_out_g", chunk, 1), rstd.ap, ALU.mult, ALU.mult,
                [y.key, rstd.key, ("par", li)], [yn.key])
            fpool.put(rstd)
            return yn

        def gelu(out_ap, out_key, src_ap, src_key, shape_fn, extra_reads=()):
            t = fpool.get()
            tap = shape_fn(t.ap)
            ACT(out_ap, src_ap, AF.Identity, [src_key], [out_key])
            ACT(tap, src_ap, AF.Square, [src_key], [t.key])
            TS(tap, tap, 0.044715, 1.0, ALU.mult, ALU.add, [t.key], [t.key])
            TT(tap, tap, out_ap, ALU.mult, [t.key, out_key], [t.key])
            ACT(tap, tap, AF.Sigmoid, [t.key], [t.key], scale=1.5957691216057308)
            TT(out_ap, tap, out_ap, ALU.mult, [t.key, out_key] + list(extra_reads), [out_key])
            fpool.put(t)

        def proj(wt, col0, m, xn, prow=0):
            pst = ppool.get()
            for k in range(KC):
                MM(pst.ap[prow:prow + m, :], wt.ap[:, k, col0:col0 + m], xn[k].ap, k == 0, k == KC - 1,
                   [wt.key, xn[k].key], [pst.key])
            return pst

        def resid_add(pst, c, t):
            hs = hT[:, c, t * NT:(t + 1) * NT]
            TT(hs, hs, pst.ap, ALU.add, [hkey(c, t), pst.key], [hkey(c, t)])
            ppool.put(pst)

        def full_barrier():
            allsems = list(sch.cnt.keys())
            for e in Sched.ENGS:
                sch.barrier_wait(e, allsems)

        def mixer_phase(li, l):
            from collections import deque
            W = w_in_d[li]
            wb = [wblock(W, 512 * i, min(512, PROJ - 512 * i)) for i in range(5)]
            wo = [wblock(w_mo_d[li], 512 * i, 512) for i in range(2)]
            DMA("pool", wsT[:, :, :], wst_d[li].rearrange("p (g i) -> p g i", g=5), "wst", [], ["wsT"])
            sch.op("dve", lambda e: e.memset(wsT[64:128, :, 0:64], 0.0), [], ["wsT"])
            sch.op("dve", lambda e: e.memset(abuf[:, :, 0:A_K - 1], 0.0), [], [("abuf", 0), ("abuf", 1), ("abuf", 2)])
            sch.op("dve", lambda e: e.memset(bhist[:, :, :], 0.0), [], ["bhist"])
            pk = ("par", li)

            def wcol(col):
                return wb[col // 512], col % 512

            pending = deque()

            def run_unit(gen):
                olds = list(pending)
                pending.clear()
                try:
                    next(gen)
                    newp = gen
                except StopIteration:
                    newp = None
                for g in olds:
                    try:
                        next(g)
                        pending.append(g)
                    except StopIteration:
                        pass
                if newp is not None:
                    pending.append(newp)

            def drain():
                while pending:
                    g = pending.popleft()
                    try:
                        next(g)
                        pending.append(g)
                    except StopIteration:
                        pass

            def gn_stages(y, chunk, ymix, free_y=True):
                sq = bpool.get()
                ACT(sq.ap, y.ap, AF.Square, [y.key], [sq.key])
                yield
                pst = ppool.get()
                MM(pst.ap, bdiag[:, :], sq.ap, True, True, [sq.key, "bdiag"], [pst.key])
                bpool.put(sq)
                rstd = fpool.get()
                ACT(rstd.ap, pst.ap, AF.Ln, [pst.key], [rstd.key], scale=1.0 / 64, bias=EPS)
                ppool.put(pst)
                ACT(rstd.ap, rstd.ap, AF.Exp, [rstd.key], [rstd.key], scale=-0.5)
                yn = bpool.get()
                STT(yn.ap, y.ap, pcol(li, "mix_out_g", chunk, 1), rstd.ap, ALU.mult, ALU.mult,
                    [y.key, rstd.key, ("par", li)], [yn.key])
                fpool.put(rstd)
                if free_y:
                    fpool.put(y)
                ymix[chunk] = yn

            class TS_:
                pass

            def new_tile_state(t):
                st = TS_()
                st.t = t
                st.xn = None
                st.ymix = [None] * 8
                st.acc = [None] * 3
                st.ybs = [None] * 3
                st.ps1 = st.ps2 = None
                st.astats = 0
                st.y5 = None
                st.vnb = [None] * 4
                st.nproj = 0
                return st

            def u_rmsnorm(st):
                st.xn = [bpool.get() for _ in range(KC)]
                rmsnorm_T(hsrc(st.t), pcol(li, "norm_mix_g"), pk, [(x.ap, x.key) for x in st.xn], NT)
                return
                yield

            def u_A(st, c):
                xn = st.xn
                wt, co = wcol(c * 128)
                pv = proj(wt, co, 128, xn)
                wt, co = wcol(A_W + c * 128)
                pg = proj(wt, co, 128, xn)
                sg = fpool.get()
                ACT(sg.ap, pg.ap, AF.Sigmoid, [pg.key], [sg.key])
                ppool.put(pg)
                ak = ("abuf", c)
                TT(abuf[:, c, A_K - 1:A_K - 1 + NT], pv.ap, sg.ap, ALU.mult, [pv.key, sg.key], [ak])
                ppool.put(pv)
                fpool.put(sg)
                yield
                a = fpool.get()
                cw = pcol(li, "conv_a_w", c * A_K, A_K)
                psc = ppool.get()
                for k in range(A_K):
                    slot = dgcnt[0] % NDG
                    dgcnt[0] += 1
                    TS(dg[:, slot, :], ident_b[:, :], cw[:, k:k + 1], None, ALU.mult, None, ["ident_b", pk], [("dg", slot)])
                    sch.op("pe", lambda e, psc=psc, slot=slot, c=c, k=k: e.matmul(
                        psc.ap, dg[:, slot, :], abuf[:, c, k:k + NT], start=(k == 0), stop=(k == A_K - 1)),
                        [("dg", slot), ak], [psc.key])
                ACT(a.ap, psc.ap, AF.Identity, [psc.key, pk], [a.key], bias=pcol(li, "conv_a_b", c, 1))
                ppool.put(psc)
                ACT(abuf[:, c, 0:A_K - 1], abuf[:, c, NT:NT + A_K - 1], AF.Identity, [ak], [ak])
                yb_ = bpool.get()
                ys_ = bpool.get()
                ACT(yb_.ap, a.ap, AF.Identity, [a.key], [yb_.key])
                ACT(ys_.ap, a.ap, AF.Square, [a.key], [ys_.key])
                st.acc[c] = a
                st.ybs[c] = (yb_, ys_)
                st.astats += 1

            def u_Afin(st):
                assert st.astats == 3, "A conv not complete before A-fin"
                ps1, ps2 = ppool.get(), ppool.get()
                for c in range(3):
                    MM(ps1.ap, ones_b[:, :], st.ybs[c][0].ap, c == 0, c == 2, [st.ybs[c][0].key, "ones_b"], [ps1.key])
                for c in range(3):
                    MM(ps2.ap, ones_b[:, :], st.ybs[c][1].ap, c == 0, c == 2, [st.ybs[c][1].key, "ones_b"], [ps2.key])
                for c in range(3):
                    bpool.put(st.ybs[c][0])
                    bpool.put(st.ybs[c][1])
                mean, msq = fpool.get(), fpool.get()
                ACT(mean.ap, ps1.ap, AF.Identity, [ps1.key], [mean.key], scale=1.0 / A_W)
                ACT(msq.ap, ps1.ap, AF.Square, [ps1.key], [msq.key], scale=1.0 / A_W)
                ppool.put(ps1)
                STT(msq.ap, ps2.ap, 1.0 / A_W, msq.ap, ALU.mult, ALU.subtract, [ps2.key, msq.key], [msq.key])
                ppool.put(ps2)
                yield
                ACT(msq.ap, msq.ap, AF.Ln, [msq.key], [msq.key], bias=EPS)
                ACT(msq.ap, msq.ap, AF.Exp, [msq.key], [msq.key], scale=-0.5)
                yield
                for c in range(3):
                    a = st.acc[c]
                    TT(a.ap, a.ap, mean.ap, ALU.subtract, [a.key, mean.key], [a.key])
                    TT(a.ap, a.ap, msq.ap, ALU.mult, [a.key, msq.key], [a.key])
                fpool.put(mean)
                fpool.put(msq)
                yield
                for c in range(3):
                    a = st.acc[c]
                    ACT(a.ap, a.ap, AF.Silu, [a.key, pk], [a.key], scale=pcol(li, "ln_a_g", c, 1), bias=pcol(li, "ln_a_b", c, 1))
                gens = [gn_stages(st.acc[c], c, st.ymix) for c in range(3)]
                for g in gens:
                    next(g)
                yield
                for g in gens:
                    try:
                        next(g)
                    except StopIteration:
                        pass

            def u_B(st, j):
                xn = st.xn
                r = 128 if j < 2 else 64
                wt, co = wcol(2 * A_W + j * 128)
                pbg = proj(wt, co, r, xn)
                wt, co = wcol(2 * A_W + B_W + j * 128)
                pcg = proj(wt, co, r, xn)
                wt, co = wcol(2 * A_W + 2 * B_W + j * 128)
                pbi = proj(wt, co, r, xn)
                yield
                cg = fpool.get()
                ACT(cg.ap[0:r, :], pcg.ap[0:r, :], AF.Identity, [pcg.key], [cg.key])
                ppool.put(pcg)
                p = fpool.get()
                TT(p.ap[0:r, :], pbi.ap[0:r, :], cg.ap[0:r, :], ALU.mult, [pbi.key, cg.key], [p.key])
                ppool.put(pbi)
                a = cg
                cw = pcol(li, "conv_b_w", j * 3, 3)
                hk = ("bh", j)
                TS(a.ap[0:r, :], p.ap[0:r, :], cw[0:r, 2:3], None, ALU.mult, None, [p.key, pk], [a.key])
                STT(a.ap[0:r, 1:NT], p.ap[0:r, 0:NT - 1], cw[0:r, 1:2], a.ap[0:r, 1:NT], ALU.mult, ALU.add, [p.key, pk, a.key], [a.key])
                STT(a.ap[0:r, 0:1], bhist[0:r, j, 1:2], cw[0:r, 1:2], a.ap[0:r, 0:1], ALU.mult, ALU.add, [hk, "bhist", pk, a.key], [a.key])
                STT(a.ap[0:r, 2:NT], p.ap[0:r, 0:NT - 2], cw[0:r, 0:1], a.ap[0:r, 2:NT], ALU.mult, ALU.add, [p.key, pk, a.key], [a.key])
                STT(a.ap[0:r, 0:2], bhist[0:r, j, 0:2], cw[0:r, 0:1], a.ap[0:r, 0:2], ALU.mult, ALU.add, [hk, "bhist", pk, a.key], [a.key])
                ACT(bhist[0:r, j, 0:2], p.ap[0:r, NT - 2:NT], AF.Identity, [p.key], [hk])
                if j < 2:
                    y = p
                else:
                    st.y5 = fpool.get()
                    y = st.y5
                TT(y.ap[0:r, :], pbg.ap[0:r, :], a.ap[0:r, :], ALU.mult, [pbg.key, a.key], [y.key])
                ppool.put(pbg)
                fpool.put(a)
                if j < 2:
                    yield from gn_stages(y, 3 + j, st.ymix)
                else:
                    fpool.put(p)

            def gelu_stages(out_ap, out_key, src_ap, bank, shape_fn):
                t = fpool.get()
                tap = shape_fn(t.ap)
                ACT(out_ap, src_ap, AF.Identity, [bank.key], [out_key])
                ACT(tap, src_ap, AF.Square, [bank.key], [t.key])
                ppool.put(bank)
                yield
                TS(tap, tap, 0.044715, 1.0, ALU.mult, ALU.add, [t.key], [t.key])
                TT(tap, tap, out_ap, ALU.mult, [t.key, out_key], [t.key])
                yield
                ACT(tap, tap, AF.Sigmoid, [t.key], [t.key], scale=1.5957691216057308)
                yield
                TT(out_ap, tap, out_ap, ALU.mult, [t.key, out_key], [out_key])
                fpool.put(t)

            def u_V(st, blk):
                xn = st.xn
                wt4 = wb[4]
                pst = ppool.get()
                for k in range(KC):
                    MM(pst.ap[:, 0:C_W], xn[k].ap[:, blk * 128:(blk + 1) * 128], wt4.ap[:, k, 0:C_W], k == 0, k == KC - 1,
                       [wt4.key, xn[k].key], [pst.key])
                v = fpool.get()
                yield from gelu_stages(v.ap[:, 0:C_W], v.key, pst.ap[:, 0:C_W], pst, lambda ap: ap[:, 0:C_W])
                sk = ("small_v", blk)
                sm = small[:, 4 * blk:4 * blk + 4]
                s6 = stat6v[:, blk, :]
                sch.op("dve", lambda e, v=v, s6=s6: e.bn_stats(out=s6, in_=v.ap[:, 0:C_W]), [v.key], [sk])
                sch.op("dve", lambda e, sm=sm, s6=s6: e.bn_aggr(out=sm[:, 0:2], in_=s6), [sk], [sk])
                yield
                ACT(sm[:, 2:3], sm[:, 1:2], AF.Sqrt, [sk], [sk], bias=EPS)
                yield
                RECIP(sm[:, 2:3], sm[:, 2:3], [sk], [sk])
                TS(v.ap[:, 0:C_W], v.ap[:, 0:C_W], sm[:, 0:1], sm[:, 2:3], ALU.subtract, ALU.mult, [v.key, sk], [v.key])
                TT(v.ap[:, 0:C_W], v.ap[:, 0:C_W], pcol(li, "ln_c_g"), ALU.mult, [v.key, pk], [v.key])
                vb = bpool.get()
                TT(vb.ap[:, 0:C_W], v.ap[:, 0:C_W], pcol(li, "ln_c_b"), ALU.add, [v.key, pk], [vb.key])
                fpool.put(v)
                st.vnb[blk] = vb

            units_c = [(5, [(0, 64)]), (6, [(1, 0), (2, 64)]), (7, [(3, 0), (4, 64)])]
            ucol = {5: 2 * A_W + 3 * B_W, 6: 2 * A_W + 3 * B_W + 64, 7: 2 * A_W + 3 * B_W + 192}

            def u_U(st, ui, last_proj):
                xn = st.xn
                chunk, groups = units_c[ui]
                p0 = 64 if chunk == 5 else 0
                m = 64 if chunk == 5 else 128
                wt, co = wcol(ucol[chunk])
                pu = proj(wt, co, m, xn, prow=p0)
                if last_proj:
                    for x in xn:
                        bpool.put(x)
                u = fpool.get()
                yield from gelu_stages(u.ap[p0:128, :], u.key, pu.ap[p0:128, :], pu, lambda ap, p0=p0: ap[p0:128, :])
                yield
                vnb = st.vnb
                assert all(v is not None for v in vnb)
                pm = ppool.get()
                ng = len(groups) * (NT // 128)
                i = 0
                for blk in range(NT // 128):
                    for (g, prow) in groups:
                        i += 1
                        sch.op("pe", lambda e, pm=pm, prow=prow, blk=blk, g=g, vb=vnb[blk]: e.matmul(
                            pm.ap[prow:prow + 64, blk * 128:(blk + 1) * 128], vb.ap[:, g * 64:(g + 1) * 64],
                            wsT[:, g, :], start=True, stop=True), [vnb[blk].key, "wsT"], [pm.key], inc=(i == ng))
                if ui == 2:
                    for vb in vnb:
                        bpool.put(vb)
                y = st.y5 if chunk == 5 else fpool.get()
                bias = pcol(li, "gmlp_bias", ui * 128, 128)
                tmp = fpool.get()
                for blk in range(NT // 128):
                    TT(tmp.ap[p0:128, blk * 128:(blk + 1) * 128], pm.ap[p0:128, blk * 128:(blk + 1) * 128], bias[p0:128, :],
                       ALU.add, [pm.key, pk], [tmp.key])
                ppool.put(pm)
                TT(y.ap[p0:128, :], tmp.ap[p0:128, :], u.ap[p0:128, :], ALU.mult, [tmp.key, u.key], [y.key])
                fpool.put(tmp)
                fpool.put(u)
                yield from gn_stages(y, chunk, st.ymix)

            def u_outproj(st, ocs, last):
                assert all(y is not None for y in st.ymix), "ymix incomplete before out-proj"
                psts = []
                for oc in ocs:
                    wt = wo[oc // 4]
                    psts.append(proj(wt, (oc % 4) * 128, 128, st.ymix))
                if last:
                    for y in st.ymix:
                        bpool.put(y)
                yield
                for oc, pst in zip(ocs, psts):
                    resid_add(pst, oc, st.t)

            states = [new_tile_state(t) for t in range(NTILES)]
            run_unit(u_rmsnorm(states[0]))
            for t in range(NTILES):
                st = states[t]
                run_unit(u_A(st, 0))
                run_unit(u_A(st, 1))
                run_unit(u_A(st, 2))
                for blk in range(4):
                    run_unit(u_V(st, blk))
                if t > 0:
                    drain_for = states[t - 1]
                    while any(y is None for y in drain_for.ymix):
                        g = pending.popleft()
                        try:
                            next(g)
                            pending.append(g)
                        except StopIteration:
                            pass
                    run_unit(u_outproj(drain_for, [0, 1, 2], False))
                run_unit(u_B(st, 0))
                if t > 0:
                    run_unit(u_outproj(states[t - 1], [3, 4, 5], False))
                run_unit(u_B(st, 1))
                if t > 0:
                    run_unit(u_outproj(states[t - 1], [6, 7], True))
                run_unit(u_B(st, 2))
                if t + 1 < NTILES:
                    run_unit(u_rmsnorm(states[t + 1]))
                run_unit(u_Afin(st))
                run_unit(u_U(st, 0, False))
                run_unit(u_U(st, 1, False))
                run_unit(u_U(st, 2, True))
            drain()
            run_unit(u_outproj(states[NTILES - 1], [0, 1, 2], False))
            run_unit(u_outproj(states[NTILES - 1], [3, 4, 5], False))
            run_unit(u_outproj(states[NTILES - 1], [6, 7], True))
            drain()

        def attn_phase(li, l, b):
            pk = ("par", li)
            wkv = [wblock(w_xkv_d[li], 512 * i, 512) for i in range(4)]
            mT = [fpool.get() for _ in range(4)]
            assert MEM == 256

            for blk in range(MEM // 128):
                for half in range(2):
                    xt = fpool.get()
                    DMA("sp", xt.ap, mem_d[b, blk * 128:(blk + 1) * 128, half * 512:(half + 1) * 512],
                        "ld%d" % xt.key[1], [], [xt.key])
                    pst = ppool.get()
                    for j in range(4):
                        TR(pst.ap[:, j * 128:(j + 1) * 128], xt.ap[:, j * 128:(j + 1) * 128], [xt.key], [pst.key], inc=(j == 3))
                    fpool.put(xt)
                    for jj in range(2):
                        mt = mT[2 * half + jj]
                        ACT(mt.ap.rearrange("p (c m) -> p c m", c=2)[:, :, blk * 128:(blk + 1) * 128],
                            pst.ap[:, jj * 256:(jj + 1) * 256].rearrange("p (c m) -> p c m", c=2), AF.Identity, [pst.key], [mt.key])
                    ppool.put(pst)
            mn = [bpool.get() for _ in range(4)]
            msrc = [(mT[c // 2].ap[:, (c % 2) * MEM:(c % 2 + 1) * MEM], mT[c // 2].key) for c in range(KC)]
            mdst_ = [(mn[c // 2].ap[:, (c % 2) * MEM:(c % 2 + 1) * MEM], mn[c // 2].key) for c in range(KC)]
            rmsnorm_T(msrc, pcol(li, "norm_mem_g"), pk, mdst_, MEM)
            for m_ in mT:
                fpool.put(m_)
            KT = [bpool.get() for _ in range(4)]
            for dc in range(KC):
                wt = wkv[dc // 4]
                pst = ppool.get()
                for k in range(KC):
                    MM(pst.ap[:, 0:MEM], wt.ap[:, k, (dc % 4) * 128:(dc % 4 + 1) * 128], mdst_[k][0], k == 0, k == KC - 1,
                       [wt.key, mdst_[k][1]], [pst.key])
                kt = KT[dc // 2]
                ACT(kt.ap[:, (dc % 2) * MEM:(dc % 2 + 1) * MEM], pst.ap[:, 0:MEM], AF.Identity, [pst.key], [kt.key])
                ppool.put(pst)
            V = [bpool.get() for _ in range(4)]
            for mb in range(2):
                for half in range(2):
                    wt = wkv[2 + half]
                    pst = ppool.get()
                    for k in range(KC):
                        MM(pst.ap, mdst_[k][0][:, mb * 128:(mb + 1) * 128], wt.ap[:, k, :], k == 0, k == KC - 1,
                           [wt.key, mdst_[k][1]], [pst.key])
                    vt = V[2 * mb + half]
                    ACT(vt.ap, pst.ap, AF.Identity, [pst.key], [vt.key])
                    ppool.put(pst)
            for m_ in mn:
                bpool.put(m_)
            wq = [wblock(w_xq_d[li], 512 * i, 512) for i in range(2)]
            wo = [wblock(w_xo_d[li], 512 * i, 512) for i in range(2)]

            for t in range(NTILES):
                xn = [bpool.get() for _ in range(KC)]
                rmsnorm_T(hsrc(t), pcol(li, "norm_x_g"), pk, [(x.ap, x.key) for x in xn], NT)
                qT = []
                for dc in range(KC):
                    pst = proj(wq[dc // 4], (dc % 4) * 128, 128, xn)
                    q = bpool.get()
                    ACT(q.ap, pst.ap, AF.Identity, [pst.key], [q.key], scale=1.0 / 16.0)
                    ppool.put(pst)
                    qT.append(q)
                for x in xn:
                    bpool.put(x)
                oT = []
                for h in range(4):
                    PT = []
                    for mb in range(2):
                        pst = ppool.get()
                        for dcc in range(2):
                            dc = 2 * h + dcc
                            kt = KT[dc // 2]
                            MM(pst.ap, kt.ap[:, (dc % 2) * MEM + mb * 128:(dc % 2) * MEM + (mb + 1) * 128], qT[dc].ap,
                               dcc == 0, dcc == 1, [kt.key, qT[dc].key], [pst.key])
                        p_ = bpool.get()
                        ACT(p_.ap, pst.ap, AF.Exp, [pst.key], [p_.key])
                        ppool.put(pst)
                        PT.append(p_)
                    pss = ppool.get()
                    for mb in range(2):
                        MM(pss.ap, ones_b[:, :], PT[mb].ap, mb == 0, mb == 1, [PT[mb].key, "ones_b"], [pss.key])
                    rs = fpool.get()
                    RECIP(rs.ap, pss.ap, [pss.key], [rs.key])
                    ppool.put(pss)
                    for dcc in range(2):
                        dc = 2 * h + dcc
                        pst = ppool.get()
                        for mb in range(2):
                            vt = V[2 * mb + dc // 4]
                            MM(pst.ap, vt.ap[:, (dc % 4) * 128:(dc % 4 + 1) * 128], PT[mb].ap, mb == 0, mb == 1,
                               [vt.key, PT[mb].key], [pst.key])
                        o = bpool.get()
                        TT(o.ap, pst.ap, rs.ap, ALU.mult, [pst.key, rs.key], [o.key])
                        ppool.put(pst)
                        oT.append(o)
                    fpool.put(rs)
                    for p_ in PT:
                        bpool.put(p_)
                for q in qT:
                    bpool.put(q)
                for oc in range(KC):
                    pst = proj(wo[oc // 4], (oc % 4) * 128, 128, oT)
                    resid_add(pst, oc, t)
                for o in oT:
                    bpool.put(o)
            for kt in KT:
                bpool.put(kt)
            for vt in V:
                bpool.put(vt)

        lbcnt = [0]
        dgcnt = [0]

        def ffn_phase(li, l, tail_hook=None):
            pk = ("par", li)
            moe = (l % 2 == 1)
            hn = [[None] * NTILES for _ in range(KC)]
            n_pro = [0]

            def prologue():
                t = n_pro[0]
                if t >= NTILES:
                    return
                n_pro[0] += 1
                tl = [bpool.get() for _ in range(KC)]
                r = rmsnorm_T(hsrc(t), pcol(li, "norm_ffn_g"), pk, [(x.ap, x.key) for x in tl], NT, keep_rstd=moe)
                for c in range(KC):
                    hn[c][t] = tl[c]
                if moe:
                    router(li, t, r)
                    fpool.put(r)

            prologue()

            deferred = []

            def slice_compute(wg, wu, wd, nfc, bc, after_tile=None, bc_fn=None):
                for t in range(NTILES):
                    prologue()
                    if bc is not None and bc[t] is None:
                        bc[t] = bc_fn(t)
                    xn = [hn[c][t] for c in range(KC)]
                    aT = []
                    for fc in range(nfc):
                        pg = proj(wg, fc * 128, 128, xn)
                        pu = proj(wu, fc * 128, 128, xn)
                        sg = fpool.get()
                        ACT(sg.ap, pg.ap, AF.Silu, [pg.key], [sg.key])
                        ppool.put(pg)
                        if bc is not None:
                            TT(sg.ap, sg.ap, bc[t].ap, ALU.mult, [sg.key, bc[t].key], [sg.key])
                        a = bpool.get()
                        TT(a.ap, pu.ap, sg.ap, ALU.mult, [pu.key, sg.key], [a.key])
                        ppool.put(pu)
                        fpool.put(sg)
                        aT.append(a)

                    def down(t=t, aT=aT, wd=wd, nfc=nfc, after_tile=after_tile):
                        for oc in range(KC):
                            pst = ppool.get()
                            for fc in range(nfc):
                                MM(pst.ap, wd.ap[:, fc, oc * 128:(oc + 1) * 128], aT[fc].ap, fc == 0, fc == nfc - 1,
                                   [wd.key, aT[fc].key], [pst.key])
                            resid_add(pst, oc, t)
                        for a in aT:
                            bpool.put(a)
                        if after_tile is not None:
                            after_tile(t)

                    if deferred:
                        deferred.pop()()
                    deferred.append(down)

            if not moe:
                Wg, Wu, Wd = fg_d[0], fu_d[0], fd_d[0]
                f0 = 0
                while f0 < DFF:
                    nf = min(512, DFF - f0)
                    wg = wblock(Wg, f0, nf)
                    wu = wblock(Wu, f0, nf)
                    wd = wblock_down(Wd, f0, nf)
                    slice_compute(wg, wu, wd, nf // 128, None)
                    f0 += nf
            else:
                for e in range(N_EXP):
                    bc = [None] * NTILES

                    def make_bc(t, e=e):
                        pst = ppool.get()
                        for blk in range(NT // 128):
                            gi = (t * (NT // 128) + blk) * 8 + e
                            lb = lbcnt[0] % 2
                            lbcnt[0] += 1
                            sch.op("dve", lambda ee, lb=lb, gi=gi: ee.tensor_copy(
                                out=lbc[:, lb, :], in_=comb_tm[:, gi:gi + 1].to_broadcast([128, 128])), [("comb", t)], [("lbc", lb)])
                            sch.op("pe", lambda ee, pst=pst, lb=lb, blk=blk: ee.matmul(
                                pst.ap[:, blk * 128:(blk + 1) * 128], lbc[:, lb, :], ident[:, :], start=True, stop=True),
                                [("lbc", lb), "ident"], [pst.key])
                        bt = fpool.get()
                        ACT(bt.ap, pst.ap, AF.Identity, [pst.key], [bt.key])
                        ppool.put(pst)
                        return bt

                    Wg, Wu, Wd = mg_d[0, e], mu_d[0, e], md_d[0, e]
                    f0 = 0
                    while f0 < DEXP:
                        nf = min(512, DEXP - f0)
                        wg = wblock(Wg, f0, nf)
                        wu = wblock(Wu, f0, nf)
                        wd = wblock_down(Wd, f0, nf)
                        last_slice = (e == N_EXP - 1) and (f0 + nf >= DEXP)
                        if last_slice and tail_hook is not None:
                            def _after(t, bc=bc):
                                if bc[t] is not None:
                                    fpool.put(bc[t])
                                    bc[t] = None
                                tail_hook(t)
                            slice_compute(wg, wu, wd, nf // 128, bc, _after, bc_fn=make_bc)
                        else:
                            slice_compute(wg, wu, wd, nf // 128, bc, bc_fn=make_bc)
                        f0 += nf
                    for i_, bt in enumerate(bc):
                        if bt is not None:
                            fpool.put(bt)
                            bc[i_] = None
            if deferred:
                deferred.pop()()
            for c in range(KC):
                for t in range(NTILES):
                    bpool.put(hn[c][t])

        def router(li, t, rstd):
            pk = ("par", li)
            if t == 0:
                for k in range(KC):
                    TS(gw[:, k * 8:(k + 1) * 8], pcol(li, "router", k * 8, 8), pcol(li, "norm_ffn_g", k, 1), None, ALU.mult, None,
                       [pk], ["gw"])
            for blk in range(NT // 128):
                tok = slice(t * NT + blk * 128, t * NT + (blk + 1) * 128)
                pst = ppool.get()
                for k in range(KC):
                    MM(pst.ap[:, 0:8], hT[:, k, tok], gw[:, k * 8:(k + 1) * 8], k == 0, k == KC - 1, [hkey(k, t), "gw"], [pst.key])
                sch.op("pe", lambda e, pst=pst, blk=blk: e.matmul(
                    pst.ap[:, 8:9], rstd.ap[0:1, blk * 128:(blk + 1) * 128], ones_f[0:1, 0:1], start=True, stop=True),
                    [rstd.key, "ones_f"], [pst.key])
                lg = small[:, 8:16]
                TS(lg, pst.ap[:, 0:8], pst.ap[:, 8:9], None, ALU.mult, None, [pst.key], ["small"])
                ppool.put(pst)
                sch.op("dve", lambda e: e.max(out=small[:, 16:24], in_=small[:, 8:16]), ["small"], ["small"])
                TS(small[:, 24:32], lg, small[:, 17:18], None, ALU.is_ge, None, ["small"], ["small"])
                TS(small[:, 32:33], small[:, 16:17], -1.0, None, ALU.mult, None, ["small"], ["small"])
                ACT(small[:, 40:48], lg, AF.Exp, ["small"], ["small"], bias=small[:, 32:33])
                STT(small[:, 48:56], small[:, 40:48], 1.0, small[:, 24:32], ALU.mult, ALU.mult, ["small"], ["small"],
                    accum_out=small[:, 33:34])
                RECIP(small[:, 34:35], small[:, 33:34], ["small"], ["small"])
                gi = (t * (NT // 128) + blk) * 8
                TS(comb_tm[:, gi:gi + 8], small[:, 48:56], small[:, 34:35], None, ALU.mult, None, ["small"], [("comb", t)])

        def final_phase(b, tiles=None):
            for t in (range(NTILES) if tiles is None else tiles):
                rstd = rmsnorm_T(hsrc(t), fing, "fing", None, NT, keep_rstd=True)
                for half in range(2):
                    yT = []
                    for j in range(4):
                        c = 4 * half + j
                        y = fpool.get()
                        STT(y.ap, hT[:, c, t * NT:(t + 1) * NT], fing[:, c:c + 1], rstd.ap, ALU.mult, ALU.mult,
                            [hkey(c, t), rstd.key, "fing"], [y.key])
                        yT.append(y)
                    for blk in range(NT // 128):
                        pst = ppool.get()
                        for j in range(4):
                            TR(pst.ap[:, j * 128:(j + 1) * 128], yT[j].ap[:, blk * 128:(blk + 1) * 128], [yT[j].key], [pst.key], inc=(j == 3))
                        ot = fpool.get()
                        ACT(ot.ap, pst.ap, AF.Identity, [pst.key], [ot.key])
                        ppool.put(pst)
                        r0 = t * NT + blk * 128
                        DMA("sp", out_d[b, r0:r0 + 128, half * 512:(half + 1) * 512], ot.ap, "st%d" % ot.key[1], [ot.key], [])
                        fpool.put(ot)
                    for y in yT:
                        fpool.put(y)
                fpool.put(rstd)

        def hdst(half, blk):
            return (hT[:, 4 * half:4 * half + 4, blk * 128:(blk + 1) * 128],
                    [hkey(4 * half + j, (blk * 128) // NT) for j in range(4)])

        stages = getattr(cfg, "stages", ("mix", "attn", "ffn"))
        n_l = len(cfg.layers)
        tail_ok = ("ffn" in stages) and (cfg.layers[-1] % 2 == 1) and getattr(cfg, "tail_overlap", True)
        preloaded = False
        for b in range(NB):
            if not preloaded:
                load_T(x_d[b], S, hdst)
            preloaded = False

            def tail_hook(t, b=b):
                final_phase(b, [t])
                if b + 1 < NB:
                    load_T(x_d[b + 1], S, hdst, blks=range(t * (NT // 128), (t + 1) * (NT // 128)))

            for li, l in enumerate(cfg.layers):
                if "mix" in stages:
                    mixer_phase(li, l)
                if "attn" in stages:
                    attn_phase(li, l, b)
                if "ffn" in stages:
                    ffn_phase(li, l, tail_hook if (tail_ok and li == n_l - 1) else None)
            if tail_ok:
                preloaded = (b + 1 < NB)
            else:
                final_phase(b)
        sch.barrier_wait("sp", [k for k in sch.cnt if k.startswith("st")])

        sch.check_deadlock()
        with nc.Block() as block:
            def emit(engname, e):
                for (waits, fn, incsem, amt) in sch.ops[engname]:
                    for (s, v) in waits:
                        e.wait_ge(sems[s], v)
                    if fn is not None:
                        ins = fn(e)
                        if incsem is not None:
                            ins.then_inc(sems[incsem], amt)

            @block.tensor
            def _(e):
                emit("pe", e)

            @block.scalar
            def _(e):
                emit("act", e)

            @block.vector
            def _(e):
                emit("dve", e)

            @block.gpsimd
            def _(e):
                emit("pool", e)

            @block.sync
            def _(e):
                emit("sp", e)
    build_nc.last_nops = sch.n_ops
    return nc


def make_in_maps(cfg, inp):
    x = np.asarray(inp["x"], np.float32)
    mem = np.asarray(inp["mem"], np.float32)
    L = len(cfg.layers)
    params = np.stack([_build_params(inp, l) for l in cfg.layers])
    wsT = np.stack([np.ascontiguousarray(np.asarray(inp["gmlp_ws"][l], np.float32).transpose(2, 0, 1)).reshape(128, 5 * 128)
                    for l in cfg.layers])
    ident = _build_consts()
    lay = list(cfg.layers)
    shared = {
        "params": params, "wsT": wsT, "fin_g": _colmajor(inp["norm_final_g"]), "ident": ident,
        "w_in": np.asarray(inp["w_in"], np.float32)[lay], "w_mix_out": np.asarray(inp["w_mix_out"], np.float32)[lay],
        "w_xq": np.asarray(inp["w_xq"], np.float32)[lay], "w_xkv": np.asarray(inp["w_xkv"], np.float32)[lay],
        "w_xo": np.asarray(inp["w_xo"], np.float32)[lay],
    }
    if any(l % 2 == 0 for l in cfg.layers):
        shared["ffn_w_gate"] = np.asarray(inp["ffn_w_gate"], np.float32)
        shared["ffn_w_up"] = np.asarray(inp["ffn_w_up"], np.float32)
        shared["ffn_w_down"] = np.asarray(inp["ffn_w_down"], np.float32)
    if any(l % 2 == 1 for l in cfg.layers):
        shared["moe_w_gate"] = np.asarray(inp["moe_w_gate"], np.float32)
        shared["moe_w_up"] = np.asarray(inp["moe_w_up"], np.float32)
        shared["moe_w_down"] = np.asarray(inp["moe_w_down"], np.float32)
    maps = []
    for c in range(cfg.ncores):
        m = dict(shared)
        m["x"] = np.ascontiguousarray(x[c * cfg.NB:(c + 1) * cfg.NB])
        m["mem"] = np.ascontiguousarray(mem[c * cfg.NB:(c + 1) * cfg.NB])
        maps.append(m)
    return maps


def run(cfg, inp, trace=False):
    nc = build_nc(cfg)
    maps = make_in_maps(cfg, inp)
    res = run_bass_kernel_spmd(nc, maps, core_ids=list(range(cfg.ncores)), trace=trace)
    out = np.concatenate([np.asarray(r["out"]) for r in res.results], axis=0)
    return out.astype(np.float32), res


def kernel(**inputs):
    cfg = Cfg()
    out, _ = run(cfg, inputs)
    return out
```

```python
import numpy as np
import concourse.bass as bass
import concourse.mybir as mybir
from concourse.bass_utils import run_bass_kernel_spmd

F32 = mybir.dt.float32
BF16 = mybir.dt.bfloat16
AF = mybir.ActivationFunctionType
ALU = mybir.AluOpType
AX = mybir.AxisListType

D = 1024
KC = 8
A_W, B_W, C_W = 384, 320, 320
PROJ = 2368
A_K = 31
EPS = 1e-6
NT = 512
N_EXP = 8


class Cfg:
    def __init__(self, S=2048, NB=2, MEM=256, DFF=2816, DEXP=3584, layers=(0, 1), ncores=8,
                 nslot=7, nf32=12, nb16x=8):
        self.S, self.NB, self.MEM, self.DFF, self.DEXP = S, NB, MEM, DFF, DEXP
        self.layers = tuple(layers)
        self.ncores = ncores
        self.nslot, self.nf32, self.nb16x = nslot, nf32, nb16x


def _colmajor(v):
    v = np.asarray(v, np.float32).reshape(-1)
    n = v.shape[0]
    c = (n + 127) // 128
    buf = np.zeros((c * 128,), np.float32)
    buf[:n] = v
    return np.ascontiguousarray(buf.reshape(c, 128).T)


PCOLS = {}


def _param_layout():
    off = 0
    def add(name, n):
        nonlocal off
        PCOLS[name] = (off, n)
        off += n
    add("norm_mix_g", 8); add("norm_x_g", 8); add("norm_mem_g", 8); add("norm_ffn_g", 8)
    add("mix_out_g", 8)
    add("conv_a_w", 3 * A_K); add("conv_a_b", 3); add("ln_a_g", 3); add("ln_a_b", 3)
    add("conv_b_w", 9)
    add("router", 64)
    add("ln_c_g", C_W); add("ln_c_b", C_W)
    add("gmlp_bias", 3 * 128)
    return off


NPCOL = _param_layout()


def _build_params(inp, layer):
    P = np.zeros((128, NPCOL), np.float32)
    def put(name, arr):
        o, n = PCOLS[name]
        assert arr.shape == (128, n), (name, arr.shape, n)
        P[:, o:o + n] = arr
    put("norm_mix_g", _colmajor(inp["norm_mix_g"][layer]))
    put("norm_x_g", _colmajor(inp["norm_x_g"][layer]))
    put("norm_mem_g", _colmajor(inp["norm_mem_g"][layer]))
    put("norm_ffn_g", _colmajor(inp["norm_ffn_g"][layer]))
    put("mix_out_g", _colmajor(inp["mix_out_g"][layer]))
    caw = np.asarray(inp["conv_a_w"][layer], np.float32)
    put("conv_a_w", np.concatenate([caw[:, c * 128:(c + 1) * 128].T for c in range(3)], axis=1))
    put("conv_a_b", _colmajor(inp["conv_a_b"][layer]))
    put("ln_a_g", _colmajor(inp["ln_a_g"][layer]))
    put("ln_a_b", _colmajor(inp["ln_a_b"][layer]))
    cbw = np.asarray(inp["conv_b_w"][layer], np.float32)
    cb = np.zeros((128, 9), np.float32)
    for j in range(3):
        r = min(128, B_W - j * 128)
        cb[:r, j * 3:(j + 1) * 3] = cbw[:, j * 128:j * 128 + r].T
    put("conv_b_w", cb)
    if layer % 2 == 1:
        wr = np.asarray(inp["moe_router"][layer // 2], np.float32)
        put("router", np.ascontiguousarray(wr.reshape(8, 128, 8).transpose(1, 0, 2)).reshape(128, 64))
    put("ln_c_g", np.broadcast_to(np.asarray(inp["ln_c_g"][layer], np.float32)[None, :], (128, C_W)))
    put("ln_c_b", np.broadcast_to(np.asarray(inp["ln_c_b"][layer], np.float32)[None, :], (128, C_W)))
    gb = np.asarray(inp["gmlp_b"][layer], np.float32)
    gbt = np.zeros((128, 3, 128), np.float32)
    gbt[64:128, 0, :] = gb[0][None, :]
    gbt[0:64, 1, :] = gb[1][None, :]
    gbt[64:128, 1, :] = gb[2][None, :]
    gbt[0:64, 2, :] = gb[3][None, :]
    gbt[64:128, 2, :] = gb[4][None, :]
    put("gmlp_bias", gbt.reshape(128, 384))
    return P


def _build_consts():
    ident = np.eye(128, dtype=np.float32)
    return ident


class Sched:
    ENGS = ("pe", "act", "dve", "pool", "sp")

    def __init__(self):
        self.ops = {e: [] for e in self.ENGS}
        self.cnt = {}
        self.seen = {e: {} for e in self.ENGS}
        self.lastw = {}
        self.readers = {}
        self.n_ops = 0

    def _need(self, eng, deps):
        waits = {}
        for (sem, val) in deps:
            if val <= self.seen[eng].get(sem, 0):
                continue
            if val > waits.get(sem, 0):
                waits[sem] = val
        for sem, val in waits.items():
            self.seen[eng][sem] = val
        return list(waits.items())

    def op(self, eng, fn, reads=(), writes=(), inc=True, sem=None, amt=1):
        if sem is None:
            sem = eng
        deps = []
        for k in reads:
            w = self.lastw.get(k)
            if w is not None:
                deps.append(w)
        for k in writes:
            w = self.lastw.get(k)
            if w is not None:
                deps.append(w)
            deps.extend(self.readers.get(k, ()))
        if eng == "pe":
            deps = [d for d in deps if d[0] != "pe"]
        waits = self._need(eng, deps)
        cur = self.cnt.get(sem, 0)
        val = cur + amt
        if inc:
            self.cnt[sem] = val
        for k in reads:
            self.readers.setdefault(k, []).append((sem, val))
        for k in writes:
            self.lastw[k] = (sem, val)
            self.readers[k] = []
        self.ops[eng].append((waits, fn, sem if inc else None, amt))
        self.n_ops += 1

    def check_deadlock(self):
        cnt = {}
        pos = {e: 0 for e in self.ENGS}
        progress = True
        while progress:
            progress = False
            for e in self.ENGS:
                q = self.ops[e]
                while pos[e] < len(q):
                    waits, fn, incsem, amt = q[pos[e]]
                    if any(cnt.get(s_, 0) < v for (s_, v) in waits):
                        break
                    if incsem is not None:
                        cnt[incsem] = cnt.get(incsem, 0) + amt
                    pos[e] += 1
                    progress = True
        stuck = {e: (pos[e], len(self.ops[e])) for e in self.ENGS if pos[e] < len(self.ops[e])}
        if stuck:
            msg = []
            for e, (p_, n_) in stuck.items():
                waits = self.ops[e][p_][0]
                msg.append(f"{e} stuck at {p_}/{n_} waiting {[(s_, v, cnt.get(s_, 0)) for (s_, v) in waits]}")
            raise RuntimeError("DEADLOCK in schedule: " + "; ".join(msg))

    def barrier_wait(self, eng, sems):
        deps = [(s, self.cnt.get(s, 0)) for s in sems]
        waits = self._need(eng, deps)
        if waits:
            self.ops[eng].append((waits, None, None, 0))


class Pool:
    def __init__(self, items):
        self.free_list = list(items)

    def get(self):
        assert self.free_list, "pool exhausted"
        return self.free_list.pop(0)

    def put(self, t):
        self.free_list.append(t)


class Tile:
    def __init__(self, ap, key):
        self.ap = ap
        self.key = key


def build_nc(cfg):
    nc = bass.Bass("TRN2", target_bir_lowering=False)
    S, NB, MEM = cfg.S, cfg.NB, cfg.MEM
    NTILES = S // NT
    L = len(cfg.layers)
    has_dense = any(l % 2 == 0 for l in cfg.layers)
    has_moe = any(l % 2 == 1 for l in cfg.layers)
    DFF, DEXP = cfg.DFF, cfg.DEXP

    def dram(name, shape, kind="ExternalInput"):
        return nc.dram_tensor(name, list(shape), F32, kind=kind).ap()

    x_d = dram("x", [NB, S, D])
    mem_d = dram("mem", [NB, MEM, D])
    par_d = dram("params", [L, 128, NPCOL])
    wst_d = dram("wsT", [L, 128, 5 * 128])
    fin_d = dram("fin_g", [128, 8])
    ident_d = dram("ident", [128, 128])
    w_in_d = dram("w_in", [L, D, PROJ])
    w_mo_d = dram("w_mix_out", [L, D, D])
    w_xq_d = dram("w_xq", [L, D, D])
    w_xkv_d = dram("w_xkv", [L, D, 2 * D])
    w_xo_d = dram("w_xo", [L, D, D])
    if has_dense:
        fg_d = dram("ffn_w_gate", [1, D, DFF])
        fu_d = dram("ffn_w_up", [1, D, DFF])
        fd_d = dram("ffn_w_down", [1, DFF, D])
    if has_moe:
        mg_d = dram("moe_w_gate", [1, N_EXP, D, DEXP])
        mu_d = dram("moe_w_up", [1, N_EXP, D, DEXP])
        md_d = dram("moe_w_down", [1, N_EXP, DEXP, D])
    out_d = dram("out", [NB, S, D], kind="ExternalOutput")

    from contextlib import ExitStack
    es = ExitStack()
    with es:
        def sb(name, shape, dt):
            return es.enter_context(nc.sbuf_tensor(name, list(shape), dt))

        def ps(name):
            return es.enter_context(nc.psum_tensor(name, [128, NT], F32))

        hT = sb("hT", [128, KC, S], F32)
        NB16 = KC * NTILES + cfg.nb16x
        b16 = sb("b16", [128, NB16, NT], BF16)
        f32p = sb("f32p", [128, cfg.nf32, NT], F32)
        ring = sb("ring", [128, cfg.nslot, 4096], BF16)
        abuf = sb("abuf", [128, 3, NT + A_K - 1], BF16)
        NDG = 8
        dg = sb("dg", [128, NDG, 128], BF16)
        ident_b = sb("ident_b", [128, 128], BF16)
        bhist = sb("bhist", [128, 3, 2], F32)
        par = sb("par", [128, L, NPCOL], F32)
        fing = sb("fing", [128, 8], F32)
        wsT = sb("wsTs", [128, 5, 128], BF16)
        ident = sb("identf", [128, 128], F32)
        ones_b = sb("ones_b", [128, 128], BF16)
        bdiag = sb("bdiag", [128, 128], BF16)
        ones_f = sb("ones_f", [128, 128], F32)
        comb_tm = sb("comb_tm", [128, (S // 128) * 8], F32)
        lbc = sb("lbc", [128, 2, 128], F32)
        gw = sb("gw", [128, 64], F32)
        small = sb("small", [128, 64], F32)
        stat6 = sb("stat6", [128, 8], F32)
        stat6v = sb("stat6v", [128, 4, 6], F32)
        banks = [ps(f"bank{i}") for i in range(8)]

        sems = {}
        def sem(name):
            if name not in sems:
                sems[name] = es.enter_context(nc.semaphore(name))
            return sems[name]

        for e in Sched.ENGS:
            sem(e)

        sch = Sched()
        bpool = Pool([Tile(b16[:, i, :], ("b", i)) for i in range(NB16)])
        fpool = Pool([Tile(f32p[:, i, :], ("f", i)) for i in range(cfg.nf32)])
        ppool = Pool([Tile(banks[i][:, :], ("ps", i)) for i in range(8)])

        def ACT(out, in_, func, reads, writes, **kw):
            sch.op("act", lambda e: e.activation(out=out, in_=in_, func=func, **kw), reads, writes)

        def TT(out, in0, in1, op, reads, writes):
            sch.op("dve", lambda e: e.tensor_tensor(out=out, in0=in0, in1=in1, op=op), reads, writes)

        def TS(out, in0, s1, s2, op0, op1, reads, writes, **kw):
            if s2 is None:
                sch.op("dve", lambda e: e.tensor_scalar(out=out, in0=in0, scalar1=s1, scalar2=None, op0=op0, **kw),
                       reads, writes)
            else:
                sch.op("dve", lambda e: e.tensor_scalar(out=out, in0=in0, scalar1=s1, scalar2=s2, op0=op0, op1=op1, **kw),
                       reads, writes)

        def STT(out, in0, scalar, in1, op0, op1, reads, writes, **kw):
            sch.op("dve", lambda e: e.scalar_tensor_tensor(out=out, in0=in0, scalar=scalar, in1=in1, op0=op0, op1=op1, **kw),
                   reads, writes)

        def RECIP(out, in_, reads, writes):
            sch.op("dve", lambda e: e.reciprocal(out=out, in_=in_), reads, writes)

        def MM(out, lhsT, rhs, start, stop, reads, writes, inc_all=False):
            sch.op("pe", lambda e: e.matmul(out, lhsT, rhs, start=start, stop=stop), reads, writes, inc=(stop or inc_all))

        def TR(out, in_, reads, writes, inc=True):
            sch.op("pe", lambda e: e.transpose(out, in_, ident[:, :]), list(reads) + ["ident"], writes, inc=inc)

        def DMA(eng, out, in_, semname, reads, writes, **kw):
            sem(semname)
            sch.op(eng, lambda e: e.dma_start(out=out, in_=in_, **kw), reads, writes, sem=semname, amt=16)

        def hkey(c, t):
            return ("h", c, t)

        def pcol(l, name, c0=0, n=None):
            o, nn = PCOLS[name]
            if n is None:
                n = nn - c0
            return par[:, l, o + c0:o + c0 + n]

        class Ring:
            def __init__(self):
                self.n = cfg.nslot
                self.next = 0
                self.loads = [0] * self.n

            def load(self, src_ap, view):
                i = self.next
                self.next = (self.next + 1) % self.n
                a, b = src_ap.shape[1], src_ap.shape[2]
                if view == "k":
                    dst = ring[:, i, :].rearrange("p (k n) -> p k n", k=8)[:, 0:a, 0:b]
                    full = ring[:, i, :].rearrange("p (k n) -> p k n", k=8)
                else:
                    dst = ring[:, i, :].rearrange("p (k n) -> p k n", k=4)[:, 0:a, 0:b]
                    full = ring[:, i, :].rearrange("p (k n) -> p k n", k=4)
                key = ("w", i)
                DMA("pool", dst, src_ap, f"w{i}", [], [key])
                return Tile(full, key)

        wring = Ring()

        def wblock(W2d, c0, ncols):
            src = W2d[:, c0:c0 + ncols].rearrange("(k p) n -> p k n", p=128)
            return wring.load(src, "k")

        def wblock_down(W2d, f0, nf):
            src = W2d[f0:f0 + nf, :].rearrange("(k p) n -> p k n", p=128)
            return wring.load(src, "d")

        DMA("sp", ident[:, :], ident_d[:, :], "cst", [], ["ident"])
        DMA("sp", fing[:, :], fin_d[:, :], "cst", [], ["fing"])
        for li in range(L):
            DMA("sp", par[:, li, :], par_d[li], "cst", [], [("par", li)])
        for k in ["ident", "fing"] + [("par", li) for li in range(L)]:
            sch.lastw[k] = ("cst", sch.cnt["cst"])
        sch.op("dve", lambda e: e.tensor_copy(out=ident_b[:, :], in_=ident[:, :]), ["ident"], ["ident_b"])
        sch.op("dve", lambda e: e.memset(ones_b[:, :], 1.0), [], ["ones_b"])
        sch.op("dve", lambda e: e.memset(ones_f[:, :], 1.0), [], ["ones_f"])
        sch.op("dve", lambda e: e.memset(bdiag[:, :], 0.0), [], ["bdiag"])
        sch.op("dve", lambda e: e.memset(bdiag[0:64, 0:64], 1.0), [], ["bdiag"])
        sch.op("dve", lambda e: e.memset(bdiag[64:128, 64:128], 1.0), [], ["bdiag"])

        def rmsnorm_T(src, gcol, gkey, dst, n, keep_rstd=False):
            pst = ppool.get()
            for c in range(KC):
                sq = bpool.get()
                ACT(sq.ap[:, :n], src[c][0], AF.Square, [src[c][1]], [sq.key])
                MM(pst.ap[:, :n], ones_b[:, :], sq.ap[:, :n], c == 0, c == KC - 1, [sq.key, "ones_b"], [pst.key], inc_all=True)
                bpool.put(sq)
            rstd = fpool.get()
            ACT(rstd.ap[:, :n], pst.ap[:, :n], AF.Ln, [pst.key], [rstd.key], scale=1.0 / D, bias=EPS)
            ACT(rstd.ap[:, :n], rstd.ap[:, :n], AF.Exp, [rstd.key], [rstd.key], scale=-0.5)
            ppool.put(pst)
            if dst is not None:
                for c in range(KC):
                    STT(dst[c][0], src[c][0], gcol[:, c:c + 1], rstd.ap[:, :n], ALU.mult, ALU.mult,
                        [src[c][1], rstd.key, gkey], [dst[c][1]])
            if keep_rstd:
                return rstd
            fpool.put(rstd)
            return None

        def load_T(src2d, nrows, dst_fn, blks=None):
            for blk in (range(nrows // 128) if blks is None else blks):
                for half in range(2):
                    xt = fpool.get()
                    DMA("sp", xt.ap, src2d[blk * 128:(blk + 1) * 128, half * 512:(half + 1) * 512], "ld%d" % xt.key[1],
                        [], [xt.key])
                    pst = ppool.get()
                    for j in range(4):
                        TR(pst.ap[:, j * 128:(j + 1) * 128], xt.ap[:, j * 128:(j + 1) * 128], [xt.key], [pst.key], inc=(j == 3))
                    fpool.put(xt)
                    dap, dkeys = dst_fn(half, blk)
                    ACT(dap, pst.ap.rearrange("p (j n) -> p j n", j=4), AF.Identity, [pst.key], dkeys)
                    ppool.put(pst)

        def hsrc(t):
            return [(hT[:, c, t * NT:(t + 1) * NT], hkey(c, t)) for c in range(KC)]

        def groupnorm(y, li, chunk):
            sq = bpool.get()
            ACT(sq.ap, y.ap, AF.Square, [y.key], [sq.key])
            pst = ppool.get()
            MM(pst.ap, bdiag[:, :], sq.ap, True, True, [sq.key, "bdiag"], [pst.key])
            bpool.put(sq)
            rstd = fpool.get()
            ACT(rstd.ap, pst.ap, AF.Ln, [pst.key], [rstd.key], scale=1.0 / 64, bias=EPS)
            ACT(rstd.ap, rstd.ap, AF.Exp, [rstd.key], [rstd.key], scale=-0.5)
            ppool.put(pst)
            yn = bpool.get()
            STT(yn.ap, y.ap, pcol(li, "mix_out_g", chunk, 1), rstd.ap, ALU.mult, ALU.mult,
                [y.key, rstd.key, ("par", li)], [yn.key])
            fpool.put(rstd)
            return yn

        def gelu(out_ap, out_key, src_ap, src_key, shape_fn, extra_reads=()):
            t = fpool.get()
            tap = shape_fn(t.ap)
            ACT(out_ap, src_ap, AF.Identity, [src_key], [out_key])
            ACT(tap, src_ap, AF.Square, [src_key], [t.key])
            TS(tap, tap, 0.044715, 1.0, ALU.mult, ALU.add, [t.key], [t.key])
            TT(tap, tap, out_ap, ALU.mult, [t.key, out_key], [t.key])
            ACT(tap, tap, AF.Sigmoid, [t.key], [t.key], scale=1.5957691216057308)
            TT(out_ap, tap, out_ap, ALU.mult, [t.key, out_key] + list(extra_reads), [out_key])
            fpool.put(t)

        def proj(wt, col0, m, xn, prow=0):
            pst = ppool.get()
            for k in range(KC):
                MM(pst.ap[prow:prow + m, :], wt.ap[:, k, col0:col0 + m], xn[k].ap, k == 0, k == KC - 1,
                   [wt.key, xn[k].key], [pst.key])
            return pst

        def resid_add(pst, c, t):
            hs = hT[:, c, t * NT:(t + 1) * NT]
            TT(hs, hs, pst.ap, ALU.add, [hkey(c, t), pst.key], [hkey(c, t)])
            ppool.put(pst)

        def full_barrier():
            allsems = list(sch.cnt.keys())
            for e in Sched.ENGS:
                sch.barrier_wait(e, allsems)

        def mixer_phase(li, l):
            from collections import deque
            W = w_in_d[li]
            wb = [wblock(W, 512 * i, min(512, PROJ - 512 * i)) for i in range(5)]
            wo = [wblock(w_mo_d[li], 512 * i, 512) for i in range(2)]
            DMA("pool", wsT[:, :, :], wst_d[li].rearrange("p (g i) -> p g i", g=5), "wst", [], ["wsT"])
            sch.op("dve", lambda e: e.memset(wsT[64:128, :, 0:64], 0.0), [], ["wsT"])
            sch.op("dve", lambda e: e.memset(abuf[:, :, 0:A_K - 1], 0.0), [], [("abuf", 0), ("abuf", 1), ("abuf", 2)])
            sch.op("dve", lambda e: e.memset(bhist[:, :, :], 0.0), [], ["bhist"])
            pk = ("par", li)

            def wcol(col):
                return wb[col // 512], col % 512

            pending = deque()

            def run_unit(gen):
                olds = list(pending)
                pending.clear()
                try:
                    next(gen)
                    newp = gen
                except StopIteration:
                    newp = None
                for g in olds:
                    try:
                        next(g)
                        pending.append(g)
                    except StopIteration:
                        pass
                if newp is not None:
                    pending.append(newp)

            def drain():
                while pending:
                    g = pending.popleft()
                    try:
                        next(g)
                        pending.append(g)
                    except StopIteration:
                        pass

            def gn_stages(y, chunk, ymix, free_y=True):
                sq = bpool.get()
                ACT(sq.ap, y.ap, AF.Square, [y.key], [sq.key])
                yield
                pst = ppool.get()
                MM(pst.ap, bdiag[:, :], sq.ap, True, True, [sq.key, "bdiag"], [pst.key])
                bpool.put(sq)
                rstd = fpool.get()
                ACT(rstd.ap, pst.ap, AF.Ln, [pst.key], [rstd.key], scale=1.0 / 64, bias=EPS)
                ppool.put(pst)
                ACT(rstd.ap, rstd.ap, AF.Exp, [rstd.key], [rstd.key], scale=-0.5)
                yn = bpool.get()
                STT(yn.ap, y.ap, pcol(li, "mix_out_g", chunk, 1), rstd.ap, ALU.mult, ALU.mult,
                    [y.key, rstd.key, ("par", li)], [yn.key])
                fpool.put(rstd)
                if free_y:
                    fpool.put(y)
                ymix[chunk] = yn

            class TS_:
                pass

            def new_tile_state(t):
                st = TS_()
                st.t = t
                st.xn = None
                st.ymix = [None] * 8
                st.acc = [None] * 3
                st.ybs = [None] * 3
                st.ps1 = st.ps2 = None
                st.astats = 0
                st.y5 = None
                st.vnb = [None] * 4
                st.nproj = 0
                return st

            def u_rmsnorm(st):
                st.xn = [bpool.get() for _ in range(KC)]
                rmsnorm_T(hsrc(st.t), pcol(li, "norm_mix_g"), pk, [(x.ap, x.key) for x in st.xn], NT)
                return
                yield

            def u_A(st, c):
                xn = st.xn
                wt, co = wcol(c * 128)
                pv = proj(wt, co, 128, xn)
                wt, co = wcol(A_W + c * 128)
                pg = proj(wt, co, 128, xn)
                sg = fpool.get()
                ACT(sg.ap, pg.ap, AF.Sigmoid, [pg.key], [sg.key])
                ppool.put(pg)
                ak = ("abuf", c)
                TT(abuf[:, c, A_K - 1:A_K - 1 + NT], pv.ap, sg.ap, ALU.mult, [pv.key, sg.key], [ak])
                ppool.put(pv)
                fpool.put(sg)
                yield
                a = fpool.get()
                cw = pcol(li, "conv_a_w", c * A_K, A_K)
                psc = ppool.get()
                for k in range(A_K):
                    slot = dgcnt[0] % NDG
                    dgcnt[0] += 1
                    TS(dg[:, slot, :], ident_b[:, :], cw[:, k:k + 1], None, ALU.mult, None, ["ident_b", pk], [("dg", slot)])
                    sch.op("pe", lambda e, psc=psc, slot=slot, c=c, k=k: e.matmul(
                        psc.ap, dg[:, slot, :], abuf[:, c, k:k + NT], start=(k == 0), stop=(k == A_K - 1)),
                        [("dg", slot), ak], [psc.key])
                ACT(a.ap, psc.ap, AF.Identity, [psc.key, pk], [a.key], bias=pcol(li, "conv_a_b", c, 1))
                ppool.put(psc)
                ACT(abuf[:, c, 0:A_K - 1], abuf[:, c, NT:NT + A_K - 1], AF.Identity, [ak], [ak])
                yb_ = bpool.get()
                ys_ = bpool.get()
                ACT(yb_.ap, a.ap, AF.Identity, [a.key], [yb_.key])
                ACT(ys_.ap, a.ap, AF.Square, [a.key], [ys_.key])
                st.acc[c] = a
                st.ybs[c] = (yb_, ys_)
                st.astats += 1

            def u_Afin(st):
                assert st.astats == 3, "A conv not complete before A-fin"
                ps1, ps2 = ppool.get(), ppool.get()
                for c in range(3):
                    MM(ps1.ap, ones_b[:, :], st.ybs[c][0].ap, c == 0, c == 2, [st.ybs[c][0].key, "ones_b"], [ps1.key])
                for c in range(3):
                    MM(ps2.ap, ones_b[:, :], st.ybs[c][1].ap, c == 0, c == 2, [st.ybs[c][1].key, "ones_b"], [ps2.key])
                for c in range(3):
                    bpool.put(st.ybs[c][0])
                    bpool.put(st.ybs[c][1])
                mean, msq = fpool.get(), fpool.get()
                ACT(mean.ap, ps1.ap, AF.Identity, [ps1.key], [mean.key], scale=1.0 / A_W)
                ACT(msq.ap, ps1.ap, AF.Square, [ps1.key], [msq.key], scale=1.0 / A_W)
                ppool.put(ps1)
                STT(msq.ap, ps2.ap, 1.0 / A_W, msq.ap, ALU.mult, ALU.subtract, [ps2.key, msq.key], [msq.key])
                ppool.put(ps2)
                yield
                ACT(msq.ap, msq.ap, AF.Ln, [msq.key], [msq.key], bias=EPS)
                ACT(msq.ap, msq.ap, AF.Exp, [msq.key], [msq.key], scale=-0.5)
                yield
                for c in range(3):
                    a = st.acc[c]
                    TT(a.ap, a.ap, mean.ap, ALU.subtract, [a.key, mean.key], [a.key])
                    TT(a.ap, a.ap, msq.ap, ALU.mult, [a.key, msq.key], [a.key])
                fpool.put(mean)
                fpool.put(msq)
                yield
                for c in range(3):
                    a = st.acc[c]
                    ACT(a.ap, a.ap, AF.Silu, [a.key, pk], [a.key], scale=pcol(li, "ln_a_g", c, 1), bias=pcol(li, "ln_a_b", c, 1))
                gens = [gn_stages(st.acc[c], c, st.ymix) for c in range(3)]
                for g in gens:
                    next(g)
                yield
                for g in gens:
                    try:
                        next(g)
                    except StopIteration:
                        pass

            def u_B(st, j):
                xn = st.xn
                r = 128 if j < 2 else 64
                wt, co = wcol(2 * A_W + j * 128)
                pbg = proj(wt, co, r, xn)
                wt, co = wcol(2 * A_W + B_W + j * 128)
                pcg = proj(wt, co, r, xn)
                wt, co = wcol(2 * A_W + 2 * B_W + j * 128)
                pbi = proj(wt, co, r, xn)
                yield
                cg = fpool.get()
                ACT(cg.ap[0:r, :], pcg.ap[0:r, :], AF.Identity, [pcg.key], [cg.key])
                ppool.put(pcg)
                p = fpool.get()
                TT(p.ap[0:r, :], pbi.ap[0:r, :], cg.ap[0:r, :], ALU.mult, [pbi.key, cg.key], [p.key])
                ppool.put(pbi)
                a = cg
                cw = pcol(li, "conv_b_w", j * 3, 3)
                hk = ("bh", j)
                TS(a.ap[0:r, :], p.ap[0:r, :], cw[0:r, 2:3], None, ALU.mult, None, [p.key, pk], [a.key])
                STT(a.ap[0:r, 1:NT], p.ap[0:r, 0:NT - 1], cw[0:r, 1:2], a.ap[0:r, 1:NT], ALU.mult, ALU.add, [p.key, pk, a.key], [a.key])
                STT(a.ap[0:r, 0:1], bhist[0:r, j, 1:2], cw[0:r, 1:2], a.ap[0:r, 0:1], ALU.mult, ALU.add, [hk, "bhist", pk, a.key], [a.key])
                STT(a.ap[0:r, 2:NT], p.ap[0:r, 0:NT - 2], cw[0:r, 0:1], a.ap[0:r, 2:NT], ALU.mult, ALU.add, [p.key, pk, a.key], [a.key])
                STT(a.ap[0:r, 0:2], bhist[0:r, j, 0:2], cw[0:r, 0:1], a.ap[0:r, 0:2], ALU.mult, ALU.add, [hk, "bhist", pk, a.key], [a.key])
                ACT(bhist[0:r, j, 0:2], p.ap[0:r, NT - 2:NT], AF.Identity, [p.key], [hk])
                if j < 2:
                    y = p
                else:
                    st.y5 = fpool.get()
                    y = st.y5
                TT(y.ap[0:r, :], pbg.ap[0:r, :], a.ap[0:r, :], ALU.mult, [pbg.key, a.key], [y.key])
                ppool.put(pbg)
                fpool.put(a)
                if j < 2:
                    yield from gn_stages(y, 3 + j, st.ymix)
                else:
                    fpool.put(p)

            def gelu_stages(out_ap, out_key, src_ap, bank, shape_fn):
                t = fpool.get()
                tap = shape_fn(t.ap)
                ACT(out_ap, src_ap, AF.Identity, [bank.key], [out_key])
                ACT(tap, src_ap, AF.Square, [bank.key], [t.key])
                ppool.put(bank)
                yield
                TS(tap, tap, 0.044715, 1.0, ALU.mult, ALU.add, [t.key], [t.key])
                TT(tap, tap, out_ap, ALU.mult, [t.key, out_key], [t.key])
                yield
                ACT(tap, tap, AF.Sigmoid, [t.key], [t.key], scale=1.5957691216057308)
                yield
                TT(out_ap, tap, out_ap, ALU.mult, [t.key, out_key], [out_key])
                fpool.put(t)

            def u_V(st, blk):
                xn = st.xn
                wt4 = wb[4]
                pst = ppool.get()
                for k in range(KC):
                    MM(pst.ap[:, 0:C_W], xn[k].ap[:, blk * 128:(blk + 1) * 128], wt4.ap[:, k, 0:C_W], k == 0, k == KC - 1,
                       [wt4.key, xn[k].key], [pst.key])
                v = fpool.get()
                yield from gelu_stages(v.ap[:, 0:C_W], v.key, pst.ap[:, 0:C_W], pst, lambda ap: ap[:, 0:C_W])
                sk = ("small_v", blk)
                sm = small[:, 4 * blk:4 * blk + 4]
                s6 = stat6v[:, blk, :]
                sch.op("dve", lambda e, v=v, s6=s6: e.bn_stats(out=s6, in_=v.ap[:, 0:C_W]), [v.key], [sk])
                sch.op("dve", lambda e, sm=sm, s6=s6: e.bn_aggr(out=sm[:, 0:2], in_=s6), [sk], [sk])
                yield
                ACT(sm[:, 2:3], sm[:, 1:2], AF.Sqrt, [sk], [sk], bias=EPS)
                yield
                RECIP(sm[:, 2:3], sm[:, 2:3], [sk], [sk])
                TS(v.ap[:, 0:C_W], v.ap[:, 0:C_W], sm[:, 0:1], sm[:, 2:3], ALU.subtract, ALU.mult, [v.key, sk], [v.key])
                TT(v.ap[:, 0:C_W], v.ap[:, 0:C_W], pcol(li, "ln_c_g"), ALU.mult, [v.key, pk], [v.key])
                vb = bpool.get()
                TT(vb.ap[:, 0:C_W], v.ap[:, 0:C_W], pcol(li, "ln_c_b"), ALU.add, [v.key, pk], [vb.key])
                fpool.put(v)
                st.vnb[blk] = vb

            units_c = [(5, [(0, 64)]), (6, [(1, 0), (2, 64)]), (7, [(3, 0), (4, 64)])]
            ucol = {5: 2 * A_W + 3 * B_W, 6: 2 * A_W + 3 * B_W + 64, 7: 2 * A_W + 3 * B_W + 192}

            def u_U(st, ui, last_proj):
                xn = st.xn
                chunk, groups = units_c[ui]
                p0 = 64 if chunk == 5 else 0
                m = 64 if chunk == 5 else 128
                wt, co = wcol(ucol[chunk])
                pu = proj(wt, co, m, xn, prow=p0)
                if last_proj:
                    for x in xn:
                        bpool.put(x)
                u = fpool.get()
                yield from gelu_stages(u.ap[p0:128, :], u.key, pu.ap[p0:128, :], pu, lambda ap, p0=p0: ap[p0:128, :])
                yield
                vnb = st.vnb
                assert all(v is not None for v in vnb)
                pm = ppool.get()
                ng = len(groups) * (NT // 128)
                i = 0
                for blk in range(NT // 128):
                    for (g, prow) in groups:
                        i += 1
                        sch.op("pe", lambda e, pm=pm, prow=prow, blk=blk, g=g, vb=vnb[blk]: e.matmul(
                            pm.ap[prow:prow + 64, blk * 128:(blk + 1) * 128], vb.ap[:, g * 64:(g + 1) * 64],
                            wsT[:, g, :], start=True, stop=True), [vnb[blk].key, "wsT"], [pm.key], inc=(i == ng))
                if ui == 2:
                    for vb in vnb:
                        bpool.put(vb)
                y = st.y5 if chunk == 5 else fpool.get()
                bias = pcol(li, "gmlp_bias", ui * 128, 128)
                tmp = fpool.get()
                for blk in range(NT // 128):
                    TT(tmp.ap[p0:128, blk * 128:(blk + 1) * 128], pm.ap[p0:128, blk * 128:(blk + 1) * 128], bias[p0:128, :],
                       ALU.add, [pm.key, pk], [tmp.key])
                ppool.put(pm)
                TT(y.ap[p0:128, :], tmp.ap[p0:128, :], u.ap[p0:128, :], ALU.mult, [tmp.key, u.key], [y.key])
                fpool.put(tmp)
                fpool.put(u)
                yield from gn_stages(y, chunk, st.ymix)

            def u_outproj(st, ocs, last):
                assert all(y is not None for y in st.ymix), "ymix incomplete before out-proj"
                psts = []
                for oc in ocs:
                    wt = wo[oc // 4]
                    psts.append(proj(wt, (oc % 4) * 128, 128, st.ymix))
                if last:
                    for y in st.ymix:
                        bpool.put(y)
                yield
                for oc, pst in zip(ocs, psts):
                    resid_add(pst, oc, st.t)

            states = [new_tile_state(t) for t in range(NTILES)]
            run_unit(u_rmsnorm(states[0]))
            for t in range(NTILES):
                st = states[t]
                run_unit(u_A(st, 0))
                run_unit(u_A(st, 1))
                run_unit(u_A(st, 2))
                for blk in range(4):
                    run_unit(u_V(st, blk))
                if t > 0:
                    drain_for = states[t - 1]
                    while any(y is None for y in drain_for.ymix):
                        g = pending.popleft()
                        try:
                            next(g)
                            pending.append(g)
                        except StopIteration:
                            pass
                    run_unit(u_outproj(drain_for, [0, 1, 2], False))
                run_unit(u_B(st, 0))
                if t > 0:
                    run_unit(u_outproj(states[t - 1], [3, 4, 5], False))
                run_unit(u_B(st, 1))
                if t > 0:
                    run_unit(u_outproj(states[t - 1], [6, 7], True))
                run_unit(u_B(st, 2))
                if t + 1 < NTILES:
                    run_unit(u_rmsnorm(states[t + 1]))
                run_unit(u_Afin(st))
                run_unit(u_U(st, 0, False))
                run_unit(u_U(st, 1, False))
                run_unit(u_U(st, 2, True))
            drain()
            run_unit(u_outproj(states[NTILES - 1], [0, 1, 2], False))
            run_unit(u_outproj(states[NTILES - 1], [3, 4, 5], False))
            run_unit(u_outproj(states[NTILES - 1], [6, 7], True))
            drain()

        def attn_phase(li, l, b):
            pk = ("par", li)
            wkv = [wblock(w_xkv_d[li], 512 * i, 512) for i in range(4)]
            mT = [fpool.get() for _ in range(4)]
            assert MEM == 256

            for blk in range(MEM // 128):
                for half in range(2):
                    xt = fpool.get()
                    DMA("sp", xt.ap, mem_d[b, blk * 128:(blk + 1) * 128, half * 512:(half + 1) * 512],
                        "ld%d" % xt.key[1], [], [xt.key])
                    pst = ppool.get()
                    for j in range(4):
                        TR(pst.ap[:, j * 128:(j + 1) * 128], xt.ap[:, j * 128:(j + 1) * 128], [xt.key], [pst.key], inc=(j == 3))
                    fpool.put(xt)
                    for jj in range(2):
                        mt = mT[2 * half + jj]
                        ACT(mt.ap.rearrange("p (c m) -> p c m", c=2)[:, :, blk * 128:(blk + 1) * 128],
                            pst.ap[:, jj * 256:(jj + 1) * 256].rearrange("p (c m) -> p c m", c=2), AF.Identity, [pst.key], [mt.key])
                    ppool.put(pst)
            mn = [bpool.get() for _ in range(4)]
            msrc = [(mT[c // 2].ap[:, (c % 2) * MEM:(c % 2 + 1) * MEM], mT[c // 2].key) for c in range(KC)]
            mdst_ = [(mn[c // 2].ap[:, (c % 2) * MEM:(c % 2 + 1) * MEM], mn[c // 2].key) for c in range(KC)]
            rmsnorm_T(msrc, pcol(li, "norm_mem_g"), pk, mdst_, MEM)
            for m_ in mT:
                fpool.put(m_)
            KT = [bpool.get() for _ in range(4)]
            for dc in range(KC):
                wt = wkv[dc // 4]
                pst = ppool.get()
                for k in range(KC):
                    MM(pst.ap[:, 0:MEM], wt.ap[:, k, (dc % 4) * 128:(dc % 4 + 1) * 128], mdst_[k][0], k == 0, k == KC - 1,
                       [wt.key, mdst_[k][1]], [pst.key])
                kt = KT[dc // 2]
                ACT(kt.ap[:, (dc % 2) * MEM:(dc % 2 + 1) * MEM], pst.ap[:, 0:MEM], AF.Identity, [pst.key], [kt.key])
                ppool.put(pst)
            V = [bpool.get() for _ in range(4)]
            for mb in range(2):
                for half in range(2):
                    wt = wkv[2 + half]
                    pst = ppool.get()
                    for k in range(KC):
                        MM(pst.ap, mdst_[k][0][:, mb * 128:(mb + 1) * 128], wt.ap[:, k, :], k == 0, k == KC - 1,
                           [wt.key, mdst_[k][1]], [pst.key])
                    vt = V[2 * mb + half]
                    ACT(vt.ap, pst.ap, AF.Identity, [pst.key], [vt.key])
                    ppool.put(pst)
            for m_ in mn:
                bpool.put(m_)
            wq = [wblock(w_xq_d[li], 512 * i, 512) for i in range(2)]
            wo = [wblock(w_xo_d[li], 512 * i, 512) for i in range(2)]

            def rnq(t):
                xn = [bpool.get() for _ in range(KC)]
                rmsnorm_T(hsrc(t), pcol(li, "norm_x_g"), pk, [(x.ap, x.key) for x in xn], NT)
                qT = []
                for dc in range(KC):
                    pst = proj(wq[dc // 4], (dc % 4) * 128, 128, xn)
                    q = bpool.get()
                    ACT(q.ap, pst.ap, AF.Identity, [pst.key], [q.key], scale=1.0 / 16.0)
                    ppool.put(pst)
                    qT.append(q)
                for x in xn:
                    bpool.put(x)
                return qT

            def head_front(h, qT):
                PT = []
                for mb in range(2):
                    pst = ppool.get()
                    for dcc in range(2):
                        dc = 2 * h + dcc
                        kt = KT[dc // 2]
                        MM(pst.ap, kt.ap[:, (dc % 2) * MEM + mb * 128:(dc % 2) * MEM + (mb + 1) * 128], qT[dc].ap,
                           dcc == 0, dcc == 1, [kt.key, qT[dc].key], [pst.key])
                    p_ = bpool.get()
                    ACT(p_.ap, pst.ap, AF.Exp, [pst.key], [p_.key])
                    ppool.put(pst)
                    PT.append(p_)
                return PT

            def head_back(h, PT, oT):
                pss = ppool.get()
                for mb in range(2):
                    MM(pss.ap, ones_b[:, :], PT[mb].ap, mb == 0, mb == 1, [PT[mb].key, "ones_b"], [pss.key])
                rs = fpool.get()
                RECIP(rs.ap, pss.ap, [pss.key], [rs.key])
                ppool.put(pss)
                for dcc in range(2):
                    dc = 2 * h + dcc
                    pst = ppool.get()
                    for mb in range(2):
                        vt = V[2 * mb + dc // 4]
                        MM(pst.ap, vt.ap[:, (dc % 4) * 128:(dc % 4 + 1) * 128], PT[mb].ap, mb == 0, mb == 1,
                           [vt.key, PT[mb].key], [pst.key])
                    o = bpool.get()
                    TT(o.ap, pst.ap, rs.ap, ALU.mult, [pst.key, rs.key], [o.key])
                    ppool.put(pst)
                    oT.append(o)
                fpool.put(rs)
                for p_ in PT:
                    bpool.put(p_)

            qT_next = rnq(0)
            for t in range(NTILES):
                qT = qT_next
                oT = []
                pend = None
                for h in range(4):
                    PT = head_front(h, qT)
                    if pend is not None:
                        head_back(pend[0], pend[1], oT)
                    pend = (h, PT)
                for q in qT:
                    bpool.put(q)
                if t + 1 < NTILES:
                    qT_next = rnq(t + 1)
                head_back(pend[0], pend[1], oT)
                for oc in range(KC):
                    pst = proj(wo[oc // 4], (oc % 4) * 128, 128, oT)
                    resid_add(pst, oc, t)
                for o in oT:
                    bpool.put(o)
            for kt in KT:
                bpool.put(kt)
            for vt in V:
                bpool.put(vt)

        lbcnt = [0]
        dgcnt = [0]

        def ffn_phase(li, l, tail_hook=None):
            pk = ("par", li)
            moe = (l % 2 == 1)
            hn = [[None] * NTILES for _ in range(KC)]
            rstds = []
            for t in range(NTILES):
                tl = [bpool.get() for _ in range(KC)]
                r = rmsnorm_T(hsrc(t), pcol(li, "norm_ffn_g"), pk, [(x.ap, x.key) for x in tl], NT, keep_rstd=moe)
                for c in range(KC):
                    hn[c][t] = tl[c]
                if moe:
                    router(li, t, r)
                    fpool.put(r)

            deferred = []

            def slice_compute(wg, wu, wd, nfc, bc, after_tile=None):
                for t in range(NTILES):
                    xn = [hn[c][t] for c in range(KC)]
                    aT = []
                    for fc in range(nfc):
                        pg = proj(wg, fc * 128, 128, xn)
                        pu = proj(wu, fc * 128, 128, xn)
                        sg = fpool.get()
                        ACT(sg.ap, pg.ap, AF.Silu, [pg.key], [sg.key])
                        ppool.put(pg)
                        if bc is not None:
                            TT(sg.ap, sg.ap, bc[t].ap, ALU.mult, [sg.key, bc[t].key], [sg.key])
                        a = bpool.get()
                        TT(a.ap, pu.ap, sg.ap, ALU.mult, [pu.key, sg.key], [a.key])
                        ppool.put(pu)
                        fpool.put(sg)
                        aT.append(a)

                    def down(t=t, aT=aT, wd=wd, nfc=nfc, after_tile=after_tile):
                        for oc in range(KC):
                            pst = ppool.get()
                            for fc in range(nfc):
                                MM(pst.ap, wd.ap[:, fc, oc * 128:(oc + 1) * 128], aT[fc].ap, fc == 0, fc == nfc - 1,
                                   [wd.key, aT[fc].key], [pst.key])
                            resid_add(pst, oc, t)
                        for a in aT:
                            bpool.put(a)
                        if after_tile is not None:
                            after_tile(t)

                    if deferred:
                        deferred.pop()()
                    deferred.append(down)

            if not moe:
                Wg, Wu, Wd = fg_d[0], fu_d[0], fd_d[0]
                f0 = 0
                while f0 < DFF:
                    nf = min(512, DFF - f0)
                    wg = wblock(Wg, f0, nf)
                    wu = wblock(Wu, f0, nf)
                    wd = wblock_down(Wd, f0, nf)
                    slice_compute(wg, wu, wd, nf // 128, None)
                    f0 += nf
            else:
                for e in range(N_EXP):
                    bc = []
                    for t in range(NTILES):
                        pst = ppool.get()
                        for blk in range(NT // 128):
                            gi = (t * (NT // 128) + blk) * 8 + e
                            lb = lbcnt[0] % 2
                            lbcnt[0] += 1
                            sch.op("dve", lambda ee, lb=lb, gi=gi: ee.tensor_copy(
                                out=lbc[:, lb, :], in_=comb_tm[:, gi:gi + 1].to_broadcast([128, 128])), [("comb", t)], [("lbc", lb)])
                            sch.op("pe", lambda ee, pst=pst, lb=lb, blk=blk: ee.matmul(
                                pst.ap[:, blk * 128:(blk + 1) * 128], lbc[:, lb, :], ident[:, :], start=True, stop=True),
                                [("lbc", lb), "ident"], [pst.key])
                        bt = fpool.get()
                        ACT(bt.ap, pst.ap, AF.Identity, [pst.key], [bt.key])
                        ppool.put(pst)
                        bc.append(bt)
                    Wg, Wu, Wd = mg_d[0, e], mu_d[0, e], md_d[0, e]
                    f0 = 0
                    while f0 < DEXP:
                        nf = min(512, DEXP - f0)
                        wg = wblock(Wg, f0, nf)
                        wu = wblock(Wu, f0, nf)
                        wd = wblock_down(Wd, f0, nf)
                        last_slice = (e == N_EXP - 1) and (f0 + nf >= DEXP)
                        if last_slice and tail_hook is not None:
                            def _after(t, bc=bc):
                                if bc[t] is not None:
                                    fpool.put(bc[t])
                                    bc[t] = None
                                tail_hook(t)
                            slice_compute(wg, wu, wd, nf // 128, bc, _after)
                        else:
                            slice_compute(wg, wu, wd, nf // 128, bc)
                        f0 += nf
                    for i_, bt in enumerate(bc):
                        if bt is not None:
                            fpool.put(bt)
                            bc[i_] = None
            if deferred:
                deferred.pop()()
            for c in range(KC):
                for t in range(NTILES):
                    bpool.put(hn[c][t])

        def router(li, t, rstd):
            pk = ("par", li)
            if t == 0:
                for k in range(KC):
                    TS(gw[:, k * 8:(k + 1) * 8], pcol(li, "router", k * 8, 8), pcol(li, "norm_ffn_g", k, 1), None, ALU.mult, None,
                       [pk], ["gw"])
            for blk in range(NT // 128):
                tok = slice(t * NT + blk * 128, t * NT + (blk + 1) * 128)
                pst = ppool.get()
                for k in range(KC):
                    MM(pst.ap[:, 0:8], hT[:, k, tok], gw[:, k * 8:(k + 1) * 8], k == 0, k == KC - 1, [hkey(k, t), "gw"], [pst.key])
                sch.op("pe", lambda e, pst=pst, blk=blk: e.matmul(
                    pst.ap[:, 8:9], rstd.ap[0:1, blk * 128:(blk + 1) * 128], ones_f[0:1, 0:1], start=True, stop=True),
                    [rstd.key, "ones_f"], [pst.key])
                lg = small[:, 8:16]
                TS(lg, pst.ap[:, 0:8], pst.ap[:, 8:9], None, ALU.mult, None, [pst.key], ["small"])
                ppool.put(pst)
                sch.op("dve", lambda e: e.max(out=small[:, 16:24], in_=small[:, 8:16]), ["small"], ["small"])
                TS(small[:, 24:32], lg, small[:, 17:18], None, ALU.is_ge, None, ["small"], ["small"])
                TS(small[:, 32:33], small[:, 16:17], -1.0, None, ALU.mult, None, ["small"], ["small"])
                ACT(small[:, 40:48], lg, AF.Exp, ["small"], ["small"], bias=small[:, 32:33])
                STT(small[:, 48:56], small[:, 40:48], 1.0, small[:, 24:32], ALU.mult, ALU.mult, ["small"], ["small"],
                    accum_out=small[:, 33:34])
                RECIP(small[:, 34:35], small[:, 33:34], ["small"], ["small"])
                gi = (t * (NT // 128) + blk) * 8
                TS(comb_tm[:, gi:gi + 8], small[:, 48:56], small[:, 34:35], None, ALU.mult, None, ["small"], [("comb", t)])

        def final_phase(b, tiles=None):
            for t in (range(NTILES) if tiles is None else tiles):
                rstd = rmsnorm_T(hsrc(t), fing, "fing", None, NT, keep_rstd=True)
                for half in range(2):
                    yT = []
                    for j in range(4):
                        c = 4 * half + j
                        y = fpool.get()
                        STT(y.ap, hT[:, c, t * NT:(t + 1) * NT], fing[:, c:c + 1], rstd.ap, ALU.mult, ALU.mult,
                            [hkey(c, t), rstd.key, "fing"], [y.key])
                        yT.append(y)
                    for blk in range(NT // 128):
                        pst = ppool.get()
                        for j in range(4):
                            TR(pst.ap[:, j * 128:(j + 1) * 128], yT[j].ap[:, blk * 128:(blk + 1) * 128], [yT[j].key], [pst.key], inc=(j == 3))
                        ot = fpool.get()
                        ACT(ot.ap, pst.ap, AF.Identity, [pst.key], [ot.key])
                        ppool.put(pst)
                        r0 = t * NT + blk * 128
                        DMA("sp", out_d[b, r0:r0 + 128, half * 512:(half + 1) * 512], ot.ap, "st%d" % ot.key[1], [ot.key], [])
                        fpool.put(ot)
                    for y in yT:
                        fpool.put(y)
                fpool.put(rstd)

        def hdst(half, blk):
            return (hT[:, 4 * half:4 * half + 4, blk * 128:(blk + 1) * 128],
                    [hkey(4 * half + j, (blk * 128) // NT) for j in range(4)])

        stages = getattr(cfg, "stages", ("mix", "attn", "ffn"))
        n_l = len(cfg.layers)
        tail_ok = ("ffn" in stages) and (cfg.layers[-1] % 2 == 1) and getattr(cfg, "tail_overlap", True)
        preloaded = False
        for b in range(NB):
            if not preloaded:
                load_T(x_d[b], S, hdst)
            preloaded = False

            def tail_hook(t, b=b):
                final_phase(b, [t])
                if b + 1 < NB:
                    load_T(x_d[b + 1], S, hdst, blks=range(t * (NT // 128), (t + 1) * (NT // 128)))

            for li, l in enumerate(cfg.layers):
                if "mix" in stages:
                    mixer_phase(li, l)
                if "attn" in stages:
                    attn_phase(li, l, b)
                if "ffn" in stages:
                    ffn_phase(li, l, tail_hook if (tail_ok and li == n_l - 1) else None)
            if tail_ok:
                preloaded = (b + 1 < NB)
            else:
                final_phase(b)
        sch.barrier_wait("sp", [k for k in sch.cnt if k.startswith("st")])

        sch.check_deadlock()
        with nc.Block() as block:
            def emit(engname, e):
                for (waits, fn, incsem, amt) in sch.ops[engname]:
                    for (s, v) in waits:
                        e.wait_ge(sems[s], v)
                    if fn is not None:
                        ins = fn(e)
                        if incsem is not None:
                            ins.then_inc(sems[incsem], amt)

            @block.tensor
            def _(e):
                emit("pe", e)

            @block.scalar
            def _(e):
                emit("act", e)

            @block.vector
            def _(e):
                emit("dve", e)

            @block.gpsimd
            def _(e):
                emit("pool", e)

            @block.sync
            def _(e):
                emit("sp", e)
    build_nc.last_nops = sch.n_ops
    return nc


def make_in_maps(cfg, inp):
    x = np.asarray(inp["x"], np.float32)
    mem = np.asarray(inp["mem"], np.float32)
    L = len(cfg.layers)
    params = np.stack([_build_params(inp, l) for l in cfg.layers])
    wsT = np.stack([np.ascontiguousarray(np.asarray(inp["gmlp_ws"][l], np.float32).transpose(2, 0, 1)).reshape(128, 5 * 128)
                    for l in cfg.layers])
    ident = _build_consts()
    lay = list(cfg.layers)
    shared = {
        "params": params, "wsT": wsT, "fin_g": _colmajor(inp["norm_final_g"]), "ident": ident,
        "w_in": np.asarray(inp["w_in"], np.float32)[lay], "w_mix_out": np.asarray(inp["w_mix_out"], np.float32)[lay],
        "w_xq": np.asarray(inp["w_xq"], np.float32)[lay], "w_xkv": np.asarray(inp["w_xkv"], np.float32)[lay],
        "w_xo": np.asarray(inp["w_xo"], np.float32)[lay],
    }
    if any(l % 2 == 0 for l in cfg.layers):
        shared["ffn_w_gate"] = np.asarray(inp["ffn_w_gate"], np.float32)
        shared["ffn_w_up"] = np.asarray(inp["ffn_w_up"], np.float32)
        shared["ffn_w_down"] = np.asarray(inp["ffn_w_down"], np.float32)
    if any(l % 2 == 1 for l in cfg.layers):
        shared["moe_w_gate"] = np.asarray(inp["moe_w_gate"], np.float32)
        shared["moe_w_up"] = np.asarray(inp["moe_w_up"], np.float32)
        shared["moe_w_down"] = np.asarray(inp["moe_w_down"], np.float32)
    maps = []
    for c in range(cfg.ncores):
        m = dict(shared)
        m["x"] = np.ascontiguousarray(x[c * cfg.NB:(c + 1) * cfg.NB])
        m["mem"] = np.ascontiguousarray(mem[c * cfg.NB:(c + 1) * cfg.NB])
        maps.append(m)
    return maps


def run(cfg, inp, trace=False):
    nc = build_nc(cfg)
    maps = make_in_maps(cfg, inp)
    res = run_bass_kernel_spmd(nc, maps, core_ids=list(range(cfg.ncores)), trace=trace)
    out = np.concatenate([np.asarray(r["out"]) for r in res.results], axis=0)
    return out.astype(np.float32), res


def kernel(**inputs):
    cfg = Cfg()
    out, _ = run(cfg, inputs)
    return out
```

```python
import numpy as np
import concourse.bass as bass
import concourse.mybir as mybir
from concourse.bass_utils import run_bass_kernel_spmd

F32 = mybir.dt.float32
BF16 = mybir.dt.bfloat16
AF = mybir.ActivationFunctionType
ALU = mybir.AluOpType
AX = mybir.AxisListType

D = 1024
KC = 8
A_W, B_W, C_W = 384, 320, 320
PROJ = 2368
A_K = 31
EPS = 1e-6
NT = 512
N_EXP = 8


class Cfg:
    def __init__(self, S=2048, NB=2, MEM=256, DFF=2816, DEXP=3584, layers=(0, 1), ncores=8,
                 nslot=7, nf32=12, nb16x=8):
        self.S, self.NB, self.MEM, self.DFF, self.DEXP = S, NB, MEM, DFF, DEXP
        self.layers = tuple(layers)
        self.ncores = ncores
        self.nslot, self.nf32, self.nb16x = nslot, nf32, nb16x


def _colmajor(v):
    v = np.asarray(v, np.float32).reshape(-1)
    n = v.shape[0]
    c = (n + 127) // 128
    buf = np.zeros((c * 128,), np.float32)
    buf[:n] = v
    return np.ascontiguousarray(buf.reshape(c, 128).T)


PCOLS = {}


def _param_layout():
    off = 0
    def add(name, n):
        nonlocal off
        PCOLS[name] = (off, n)
        off += n
    add("norm_mix_g", 8); add("norm_x_g", 8); add("norm_mem_g", 8); add("norm_ffn_g", 8)
    add("mix_out_g", 8)
    add("conv_a_w", 3 * A_K); add("conv_a_b", 3); add("ln_a_g", 3); add("ln_a_b", 3)
    add("conv_b_w", 9)
    add("router", 64)
    add("ln_c_g", C_W); add("ln_c_b", C_W)
    add("gmlp_bias", 3 * 128)
    return off


NPCOL = _param_layout()


def _build_params(inp, layer):
    P = np.zeros((128, NPCOL), np.float32)
    def put(name, arr):
        o, n = PCOLS[name]
        assert arr.shape == (128, n), (name, arr.shape, n)
        P[:, o:o + n] = arr
    put("norm_mix_g", _colmajor(inp["norm_mix_g"][layer]))
    put("norm_x_g", _colmajor(inp["norm_x_g"][layer]))
    put("norm_mem_g", _colmajor(inp["norm_mem_g"][layer]))
    put("norm_ffn_g", _colmajor(inp["norm_ffn_g"][layer]))
    put("mix_out_g", _colmajor(inp["mix_out_g"][layer]))
    caw = np.asarray(inp["conv_a_w"][layer], np.float32)
    put("conv_a_w", np.concatenate([caw[:, c * 128:(c + 1) * 128].T for c in range(3)], axis=1))
    put("conv_a_b", _colmajor(inp["conv_a_b"][layer]))
    put("ln_a_g", _colmajor(inp["ln_a_g"][layer]))
    put("ln_a_b", _colmajor(inp["ln_a_b"][layer]))
    cbw = np.asarray(inp["conv_b_w"][layer], np.float32)
    cb = np.zeros((128, 9), np.float32)
    for j in range(3):
        r = min(128, B_W - j * 128)
        cb[:r, j * 3:(j + 1) * 3] = cbw[:, j * 128:j * 128 + r].T
    put("conv_b_w", cb)
    if layer % 2 == 1:
        wr = np.asarray(inp["moe_router"][layer // 2], np.float32)
        put("router", np.ascontiguousarray(wr.reshape(8, 128, 8).transpose(1, 0, 2)).reshape(128, 64))
    put("ln_c_g", np.broadcast_to(np.asarray(inp["ln_c_g"][layer], np.float32)[None, :], (128, C_W)))
    put("ln_c_b", np.broadcast_to(np.asarray(inp["ln_c_b"][layer], np.float32)[None, :], (128, C_W)))
    gb = np.asarray(inp["gmlp_b"][layer], np.float32)
    gbt = np.zeros((128, 3, 128), np.float32)
    gbt[64:128, 0, :] = gb[0][None, :]
    gbt[0:64, 1, :] = gb[1][None, :]
    gbt[64:128, 1, :] = gb[2][None, :]
    gbt[0:64, 2, :] = gb[3][None, :]
    gbt[64:128, 2, :] = gb[4][None, :]
    put("gmlp_bias", gbt.reshape(128, 384))
    return P


def _build_consts():
    ident = np.eye(128, dtype=np.float32)
    return ident


class Sched:
    ENGS = ("pe", "act", "dve", "pool", "sp")

    def __init__(self):
        self.ops = {e: [] for e in self.ENGS}
        self.cnt = {}
        self.seen = {e: {} for e in self.ENGS}
        self.lastw = {}
        self.readers = {}
        self.n_ops = 0

    def _need(self, eng, deps):
        waits = {}
        for (sem, val) in deps:
            if val <= self.seen[eng].get(sem, 0):
                continue
            if val > waits.get(sem, 0):
                waits[sem] = val
        for sem, val in waits.items():
            self.seen[eng][sem] = val
        return list(waits.items())

    def op(self, eng, fn, reads=(), writes=(), inc=True, sem=None, amt=1):
        if sem is None:
            sem = eng
        deps = []
        for k in reads:
            w = self.lastw.get(k)
            if w is not None:
                deps.append(w)
        for k in writes:
            w = self.lastw.get(k)
            if w is not None:
                deps.append(w)
            deps.extend(self.readers.get(k, ()))
        if eng == "pe":
            deps = [d for d in deps if d[0] != "pe"]
        waits = self._need(eng, deps)
        cur = self.cnt.get(sem, 0)
        val = cur + amt
        if inc:
            self.cnt[sem] = val
        for k in reads:
            self.readers.setdefault(k, []).append((sem, val))
        for k in writes:
            self.lastw[k] = (sem, val)
            self.readers[k] = []
        self.ops[eng].append((waits, fn, sem if inc else None, amt))
        self.n_ops += 1

    def check_deadlock(self):
        cnt = {}
        pos = {e: 0 for e in self.ENGS}
        progress = True
        while progress:
            progress = False
            for e in self.ENGS:
                q = self.ops[e]
                while pos[e] < len(q):
                    waits, fn, incsem, amt = q[pos[e]]
                    if any(cnt.get(s_, 0) < v for (s_, v) in waits):
                        break
                    if incsem is not None:
                        cnt[incsem] = cnt.get(incsem, 0) + amt
                    pos[e] += 1
                    progress = True
        stuck = {e: (pos[e], len(self.ops[e])) for e in self.ENGS if pos[e] < len(self.ops[e])}
        if stuck:
            msg = []
            for e, (p_, n_) in stuck.items():
                waits = self.ops[e][p_][0]
                msg.append(f"{e} stuck at {p_}/{n_} waiting {[(s_, v, cnt.get(s_, 0)) for (s_, v) in waits]}")
            raise RuntimeError("DEADLOCK in schedule: " + "; ".join(msg))

    def barrier_wait(self, eng, sems):
        deps = [(s, self.cnt.get(s, 0)) for s in sems]
        waits = self._need(eng, deps)
        if waits:
            self.ops[eng].append((waits, None, None, 0))


class Pool:
    def __init__(self, items):
        self.free_list = list(items)

    def get(self):
        assert self.free_list, "pool exhausted"
        return self.free_list.pop(0)

    def put(self, t):
        self.free_list.append(t)


class Tile:
    def __init__(self, ap, key):
        self.ap = ap
        self.key = key


def build_nc(cfg):
    nc = bass.Bass("TRN2", target_bir_lowering=False)
    S, NB, MEM = cfg.S, cfg.NB, cfg.MEM
    NTILES = S // NT
    L = len(cfg.layers)
    has_dense = any(l % 2 == 0 for l in cfg.layers)
    has_moe = any(l % 2 == 1 for l in cfg.layers)
    DFF, DEXP = cfg.DFF, cfg.DEXP

    def dram(name, shape, kind="ExternalInput"):
        return nc.dram_tensor(name, list(shape), F32, kind=kind).ap()

    x_d = dram("x", [NB, S, D])
    mem_d = dram("mem", [NB, MEM, D])
    par_d = dram("params", [L, 128, NPCOL])
    wst_d = dram("wsT", [L, 128, 5 * 128])
    fin_d = dram("fin_g", [128, 8])
    ident_d = dram("ident", [128, 128])
    w_in_d = dram("w_in", [L, D, PROJ])
    w_mo_d = dram("w_mix_out", [L, D, D])
    w_xq_d = dram("w_xq", [L, D, D])
    w_xkv_d = dram("w_xkv", [L, D, 2 * D])
    w_xo_d = dram("w_xo", [L, D, D])
    if has_dense:
        fg_d = dram("ffn_w_gate", [1, D, DFF])
        fu_d = dram("ffn_w_up", [1, D, DFF])
        fd_d = dram("ffn_w_down", [1, DFF, D])
    if has_moe:
        mg_d = dram("moe_w_gate", [1, N_EXP, D, DEXP])
        mu_d = dram("moe_w_up", [1, N_EXP, D, DEXP])
        md_d = dram("moe_w_down", [1, N_EXP, DEXP, D])
    out_d = dram("out", [NB, S, D], kind="ExternalOutput")

    from contextlib import ExitStack
    es = ExitStack()
    with es:
        def sb(name, shape, dt):
            return es.enter_context(nc.sbuf_tensor(name, list(shape), dt))

        def ps(name):
            return es.enter_context(nc.psum_tensor(name, [128, NT], F32))

        hT = sb("hT", [128, KC, S], F32)
        NB16 = KC * NTILES + cfg.nb16x
        b16 = sb("b16", [128, NB16, NT], BF16)
        f32p = sb("f32p", [128, cfg.nf32, NT], F32)
        ring = sb("ring", [128, cfg.nslot, 4096], BF16)
        abuf = sb("abuf", [128, 3, NT + A_K - 1], BF16)
        NDG = 8
        dg = sb("dg", [128, NDG, 128], BF16)
        ident_b = sb("ident_b", [128, 128], BF16)
        bhist = sb("bhist", [128, 3, 2], F32)
        par = sb("par", [128, L, NPCOL], F32)
        fing = sb("fing", [128, 8], F32)
        wsT = sb("wsTs", [128, 5, 128], BF16)
        ident = sb("identf", [128, 128], F32)
        ones_b = sb("ones_b", [128, 128], BF16)
        bdiag = sb("bdiag", [128, 128], BF16)
        ones_f = sb("ones_f", [128, 128], F32)
        comb_tm = sb("comb_tm", [128, (S // 128) * 8], F32)
        lbc = sb("lbc", [128, 2, 128], F32)
        gw = sb("gw", [128, 64], F32)
        small = sb("small", [128, 64], F32)
        stat6 = sb("stat6", [128, 8], F32)
        stat6v = sb("stat6v", [128, 4, 6], F32)
        banks = [ps(f"bank{i}") for i in range(8)]

        sems = {}
        def sem(name):
            if name not in sems:
                sems[name] = es.enter_context(nc.semaphore(name))
            return sems[name]

        for e in Sched.ENGS:
            sem(e)

        sch = Sched()
        bpool = Pool([Tile(b16[:, i, :], ("b", i)) for i in range(NB16)])
        fpool = Pool([Tile(f32p[:, i, :], ("f", i)) for i in range(cfg.nf32)])
        ppool = Pool([Tile(banks[i][:, :], ("ps", i)) for i in range(8)])

        def ACT(out, in_, func, reads, writes, **kw):
            sch.op("act", lambda e: e.activation(out=out, in_=in_, func=func, **kw), reads, writes)

        def TT(out, in0, in1, op, reads, writes):
            sch.op("dve", lambda e: e.tensor_tensor(out=out, in0=in0, in1=in1, op=op), reads, writes)

        def TS(out, in0, s1, s2, op0, op1, reads, writes, **kw):
            if s2 is None:
                sch.op("dve", lambda e: e.tensor_scalar(out=out, in0=in0, scalar1=s1, scalar2=None, op0=op0, **kw),
                       reads, writes)
            else:
                sch.op("dve", lambda e: e.tensor_scalar(out=out, in0=in0, scalar1=s1, scalar2=s2, op0=op0, op1=op1, **kw),
                       reads, writes)

        def STT(out, in0, scalar, in1, op0, op1, reads, writes, **kw):
            sch.op("dve", lambda e: e.scalar_tensor_tensor(out=out, in0=in0, scalar=scalar, in1=in1, op0=op0, op1=op1, **kw),
                   reads, writes)

        def RECIP(out, in_, reads, writes):
            sch.op("dve", lambda e: e.reciprocal(out=out, in_=in_), reads, writes)

        def MM(out, lhsT, rhs, start, stop, reads, writes, inc_all=False):
            sch.op("pe", lambda e: e.matmul(out, lhsT, rhs, start=start, stop=stop), reads, writes, inc=(stop or inc_all))

        def TR(out, in_, reads, writes, inc=True):
            sch.op("pe", lambda e: e.transpose(out, in_, ident[:, :]), list(reads) + ["ident"], writes, inc=inc)

        def DMA(eng, out, in_, semname, reads, writes, **kw):
            sem(semname)
            sch.op(eng, lambda e: e.dma_start(out=out, in_=in_, **kw), reads, writes, sem=semname, amt=16)

        def hkey(c, t):
            return ("h", c, t)

        def pcol(l, name, c0=0, n=None):
            o, nn = PCOLS[name]
            if n is None:
                n = nn - c0
            return par[:, l, o + c0:o + c0 + n]

        class Ring:
            def __init__(self):
                self.n = cfg.nslot
                self.next = 0
                self.loads = [0] * self.n

            def load(self, src_ap, view):
                i = self.next
                self.next = (self.next + 1) % self.n
                a, b = src_ap.shape[1], src_ap.shape[2]
                if view == "k":
                    dst = ring[:, i, :].rearrange("p (k n) -> p k n", k=8)[:, 0:a, 0:b]
                    full = ring[:, i, :].rearrange("p (k n) -> p k n", k=8)
                else:
                    dst = ring[:, i, :].rearrange("p (k n) -> p k n", k=4)[:, 0:a, 0:b]
                    full = ring[:, i, :].rearrange("p (k n) -> p k n", k=4)
                key = ("w", i)
                DMA("pool", dst, src_ap, f"w{i}", [], [key])
                return Tile(full, key)

        wring = Ring()

        def wblock(W2d, c0, ncols):
            src = W2d[:, c0:c0 + ncols].rearrange("(k p) n -> p k n", p=128)
            return wring.load(src, "k")

        def wblock_down(W2d, f0, nf):
            src = W2d[f0:f0 + nf, :].rearrange("(k p) n -> p k n", p=128)
            return wring.load(src, "d")

        DMA("sp", ident[:, :], ident_d[:, :], "cst", [], ["ident"])
        DMA("sp", fing[:, :], fin_d[:, :], "cst", [], ["fing"])
        for li in range(L):
            DMA("sp", par[:, li, :], par_d[li], "cst", [], [("par", li)])
        for k in ["ident", "fing"] + [("par", li) for li in range(L)]:
            sch.lastw[k] = ("cst", sch.cnt["cst"])
        sch.op("dve", lambda e: e.tensor_copy(out=ident_b[:, :], in_=ident[:, :]), ["ident"], ["ident_b"])
        sch.op("dve", lambda e: e.memset(ones_b[:, :], 1.0), [], ["ones_b"])
        sch.op("dve", lambda e: e.memset(ones_f[:, :], 1.0), [], ["ones_f"])
        sch.op("dve", lambda e: e.memset(bdiag[:, :], 0.0), [], ["bdiag"])
        sch.op("dve", lambda e: e.memset(bdiag[0:64, 0:64], 1.0), [], ["bdiag"])
        sch.op("dve", lambda e: e.memset(bdiag[64:128, 64:128], 1.0), [], ["bdiag"])

        def rmsnorm_T(src, gcol, gkey, dst, n, keep_rstd=False):
            pst = ppool.get()
            for c in range(KC):
                sq = bpool.get()
                ACT(sq.ap[:, :n], src[c][0], AF.Square, [src[c][1]], [sq.key])
                MM(pst.ap[:, :n], ones_b[:, :], sq.ap[:, :n], c == 0, c == KC - 1, [sq.key, "ones_b"], [pst.key], inc_all=True)
                bpool.put(sq)
            rstd = fpool.get()
            ACT(rstd.ap[:, :n], pst.ap[:, :n], AF.Ln, [pst.key], [rstd.key], scale=1.0 / D, bias=EPS)
            ACT(rstd.ap[:, :n], rstd.ap[:, :n], AF.Exp, [rstd.key], [rstd.key], scale=-0.5)
            ppool.put(pst)
            if dst is not None:
                for c in range(KC):
                    STT(dst[c][0], src[c][0], gcol[:, c:c + 1], rstd.ap[:, :n], ALU.mult, ALU.mult,
                        [src[c][1], rstd.key, gkey], [dst[c][1]])
            if keep_rstd:
                return rstd
            fpool.put(rstd)
            return None

        def load_T(src2d, nrows, dst_fn, blks=None):
            for blk in (range(nrows // 128) if blks is None else blks):
                for half in range(2):
                    xt = fpool.get()
                    DMA("sp", xt.ap, src2d[blk * 128:(blk + 1) * 128, half * 512:(half + 1) * 512], "ld%d" % xt.key[1],
                        [], [xt.key])
                    pst = ppool.get()
                    for j in range(4):
                        TR(pst.ap[:, j * 128:(j + 1) * 128], xt.ap[:, j * 128:(j + 1) * 128], [xt.key], [pst.key], inc=(j == 3))
                    fpool.put(xt)
                    dap, dkeys = dst_fn(half, blk)
                    ACT(dap, pst.ap.rearrange("p (j n) -> p j n", j=4), AF.Identity, [pst.key], dkeys)
                    ppool.put(pst)

        def hsrc(t):
            return [(hT[:, c, t * NT:(t + 1) * NT], hkey(c, t)) for c in range(KC)]

        def groupnorm(y, li, chunk):
            sq = bpool.get()
            ACT(sq.ap, y.ap, AF.Square, [y.key], [sq.key])
            pst = ppool.get()
            MM(pst.ap, bdiag[:, :], sq.ap, True, True, [sq.key, "bdiag"], [pst.key])
            bpool.put(sq)
            rstd = fpool.get()
            ACT(rstd.ap, pst.ap, AF.Ln, [pst.key], [rstd.key], scale=1.0 / 64, bias=EPS)
            ACT(rstd.ap, rstd.ap, AF.Exp, [rstd.key], [rstd.key], scale=-0.5)
            ppool.put(pst)
            yn = bpool.get()
            STT(yn.ap, y.ap, pcol(li, "mix_out_g", chunk, 1), rstd.ap, ALU.mult, ALU.mult,
                [y.key, rstd.key, ("par", li)], [yn.key])
            fpool.put(rstd)
            return yn

        def gelu(out_ap, out_key, src_ap, src_key, shape_fn, extra_reads=()):
            t = fpool.get()
            tap = shape_fn(t.ap)
            ACT(out_ap, src_ap, AF.Identity, [src_key], [out_key])
            ACT(tap, src_ap, AF.Square, [src_key], [t.key])
            TS(tap, tap, 0.044715, 1.0, ALU.mult, ALU.add, [t.key], [t.key])
            TT(tap, tap, out_ap, ALU.mult, [t.key, out_key], [t.key])
            ACT(tap, tap, AF.Sigmoid, [t.key], [t.key], scale=1.5957691216057308)
            TT(out_ap, tap, out_ap, ALU.mult, [t.key, out_key] + list(extra_reads), [out_key])
            fpool.put(t)

        def proj(wt, col0, m, xn, prow=0):
            pst = ppool.get()
            for k in range(KC):
                MM(pst.ap[prow:prow + m, :], wt.ap[:, k, col0:col0 + m], xn[k].ap, k == 0, k == KC - 1,
                   [wt.key, xn[k].key], [pst.key])
            return pst

        def resid_add(pst, c, t):
            hs = hT[:, c, t * NT:(t + 1) * NT]
            TT(hs, hs, pst.ap, ALU.add, [hkey(c, t), pst.key], [hkey(c, t)])
            ppool.put(pst)

        def full_barrier():
            allsems = list(sch.cnt.keys())
            for e in Sched.ENGS:
                sch.barrier_wait(e, allsems)

        def mixer_phase(li, l):
            from collections import deque
            W = w_in_d[li]
            wb = [wblock(W, 512 * i, min(512, PROJ - 512 * i)) for i in range(5)]
            wo = [wblock(w_mo_d[li], 512 * i, 512) for i in range(2)]
            DMA("pool", wsT[:, :, :], wst_d[li].rearrange("p (g i) -> p g i", g=5), "wst", [], ["wsT"])
            sch.op("dve", lambda e: e.memset(wsT[64:128, :, 0:64], 0.0), [], ["wsT"])
            sch.op("dve", lambda e: e.memset(abuf[:, :, 0:A_K - 1], 0.0), [], [("abuf", 0), ("abuf", 1), ("abuf", 2)])
            sch.op("dve", lambda e: e.memset(bhist[:, :, :], 0.0), [], ["bhist"])
            pk = ("par", li)

            def wcol(col):
                return wb[col // 512], col % 512

            pending = deque()

            def run_unit(gen):
                olds = list(pending)
                pending.clear()
                try:
                    next(gen)
                    newp = gen
                except StopIteration:
                    newp = None
                for g in olds:
                    try:
                        next(g)
                        pending.append(g)
                    except StopIteration:
                        pass
                if newp is not None:
                    pending.append(newp)

            def drain():
                while pending:
                    g = pending.popleft()
                    try:
                        next(g)
                        pending.append(g)
                    except StopIteration:
                        pass

            def gn_stages(y, chunk, ymix, free_y=True):
                sq = bpool.get()
                ACT(sq.ap, y.ap, AF.Square, [y.key], [sq.key])
                yield
                pst = ppool.get()
                MM(pst.ap, bdiag[:, :], sq.ap, True, True, [sq.key, "bdiag"], [pst.key])
                bpool.put(sq)
                rstd = fpool.get()
                ACT(rstd.ap, pst.ap, AF.Ln, [pst.key], [rstd.key], scale=1.0 / 64, bias=EPS)
                ppool.put(pst)
                ACT(rstd.ap, rstd.ap, AF.Exp, [rstd.key], [rstd.key], scale=-0.5)
                yn = bpool.get()
                STT(yn.ap, y.ap, pcol(li, "mix_out_g", chunk, 1), rstd.ap, ALU.mult, ALU.mult,
                    [y.key, rstd.key, ("par", li)], [yn.key])
                fpool.put(rstd)
                if free_y:
                    fpool.put(y)
                ymix[chunk] = yn

            class TS_:
                pass

            def new_tile_state(t):
                st = TS_()
                st.t = t
                st.xn = None
                st.ymix = [None] * 8
                st.acc = [None] * 3
                st.ybs = [None] * 3
                st.ps1 = st.ps2 = None
                st.astats = 0
                st.y5 = None
                st.vnb = [None] * 4
                st.nproj = 0
                return st

            def u_rmsnorm(st):
                st.xn = [bpool.get() for _ in range(KC)]
                rmsnorm_T(hsrc(st.t), pcol(li, "norm_mix_g"), pk, [(x.ap, x.key) for x in st.xn], NT)
                return
                yield

            def u_A(st, c):
                xn = st.xn
                wt, co = wcol(c * 128)
                pv = proj(wt, co, 128, xn)
                wt, co = wcol(A_W + c * 128)
                pg = proj(wt, co, 128, xn)
                sg = fpool.get()
                ACT(sg.ap, pg.ap, AF.Sigmoid, [pg.key], [sg.key])
                ppool.put(pg)
                ak = ("abuf", c)
                TT(abuf[:, c, A_K - 1:A_K - 1 + NT], pv.ap, sg.ap, ALU.mult, [pv.key, sg.key], [ak])
                ppool.put(pv)
                fpool.put(sg)
                yield
                a = fpool.get()
                cw = pcol(li, "conv_a_w", c * A_K, A_K)
                psc = ppool.get()
                for k in range(A_K):
                    slot = dgcnt[0] % NDG
                    dgcnt[0] += 1
                    TS(dg[:, slot, :], ident_b[:, :], cw[:, k:k + 1], None, ALU.mult, None, ["ident_b", pk], [("dg", slot)])
                    sch.op("pe", lambda e, psc=psc, slot=slot, c=c, k=k: e.matmul(
                        psc.ap, dg[:, slot, :], abuf[:, c, k:k + NT], start=(k == 0), stop=(k == A_K - 1)),
                        [("dg", slot), ak], [psc.key])
                ACT(a.ap, psc.ap, AF.Identity, [psc.key, pk], [a.key], bias=pcol(li, "conv_a_b", c, 1))
                ppool.put(psc)
                ACT(abuf[:, c, 0:A_K - 1], abuf[:, c, NT:NT + A_K - 1], AF.Identity, [ak], [ak])
                yb_ = bpool.get()
                ys_ = bpool.get()
                ACT(yb_.ap, a.ap, AF.Identity, [a.key], [yb_.key])
                ACT(ys_.ap, a.ap, AF.Square, [a.key], [ys_.key])
                st.acc[c] = a
                st.ybs[c] = (yb_, ys_)
                st.astats += 1

            def u_Afin(st):
                assert st.astats == 3, "A conv not complete before A-fin"
                ps1, ps2 = ppool.get(), ppool.get()
                for c in range(3):
                    MM(ps1.ap, ones_b[:, :], st.ybs[c][0].ap, c == 0, c == 2, [st.ybs[c][0].key, "ones_b"], [ps1.key])
                for c in range(3):
                    MM(ps2.ap, ones_b[:, :], st.ybs[c][1].ap, c == 0, c == 2, [st.ybs[c][1].key, "ones_b"], [ps2.key])
                for c in range(3):
                    bpool.put(st.ybs[c][0])
                    bpool.put(st.ybs[c][1])
                mean, msq = fpool.get(), fpool.get()
                ACT(mean.ap, ps1.ap, AF.Identity, [ps1.key], [mean.key], scale=1.0 / A_W)
                ACT(msq.ap, ps1.ap, AF.Square, [ps1.key], [msq.key], scale=1.0 / A_W)
                ppool.put(ps1)
                STT(msq.ap, ps2.ap, 1.0 / A_W, msq.ap, ALU.mult, ALU.subtract, [ps2.key, msq.key], [msq.key])
                ppool.put(ps2)
                yield
                ACT(msq.ap, msq.ap, AF.Ln, [msq.key], [msq.key], bias=EPS)
                ACT(msq.ap, msq.ap, AF.Exp, [msq.key], [msq.key], scale=-0.5)
                yield
                for c in range(3):
                    a = st.acc[c]
                    TT(a.ap, a.ap, mean.ap, ALU.subtract, [a.key, mean.key], [a.key])
                    TT(a.ap, a.ap, msq.ap, ALU.mult, [a.key, msq.key], [a.key])
                fpool.put(mean)
                fpool.put(msq)
                yield
                for c in range(3):
                    a = st.acc[c]
                    ACT(a.ap, a.ap, AF.Silu, [a.key, pk], [a.key], scale=pcol(li, "ln_a_g", c, 1), bias=pcol(li, "ln_a_b", c, 1))
                gens = [gn_stages(st.acc[c], c, st.ymix) for c in range(3)]
                for g in gens:
                    next(g)
                yield
                for g in gens:
                    try:
                        next(g)
                    except StopIteration:
                        pass

            def u_B(st, j):
                xn = st.xn
                r = 128 if j < 2 else 64
                wt, co = wcol(2 * A_W + j * 128)
                pbg = proj(wt, co, r, xn)
                wt, co = wcol(2 * A_W + B_W + j * 128)
                pcg = proj(wt, co, r, xn)
                wt, co = wcol(2 * A_W + 2 * B_W + j * 128)
                pbi = proj(wt, co, r, xn)
                yield
                cg = fpool.get()
                ACT(cg.ap[0:r, :], pcg.ap[0:r, :], AF.Identity, [pcg.key], [cg.key])
                ppool.put(pcg)
                p = fpool.get()
                TT(p.ap[0:r, :], pbi.ap[0:r, :], cg.ap[0:r, :], ALU.mult, [pbi.key, cg.key], [p.key])
                ppool.put(pbi)
                a = cg
                cw = pcol(li, "conv_b_w", j * 3, 3)
                hk = ("bh", j)
                TS(a.ap[0:r, :], p.ap[0:r, :], cw[0:r, 2:3], None, ALU.mult, None, [p.key, pk], [a.key])
                STT(a.ap[0:r, 1:NT], p.ap[0:r, 0:NT - 1], cw[0:r, 1:2], a.ap[0:r, 1:NT], ALU.mult, ALU.add, [p.key, pk, a.key], [a.key])
                STT(a.ap[0:r, 0:1], bhist[0:r, j, 1:2], cw[0:r, 1:2], a.ap[0:r, 0:1], ALU.mult, ALU.add, [hk, "bhist", pk, a.key], [a.key])
                STT(a.ap[0:r, 2:NT], p.ap[0:r, 0:NT - 2], cw[0:r, 0:1], a.ap[0:r, 2:NT], ALU.mult, ALU.add, [p.key, pk, a.key], [a.key])
                STT(a.ap[0:r, 0:2], bhist[0:r, j, 0:2], cw[0:r, 0:1], a.ap[0:r, 0:2], ALU.mult, ALU.add, [hk, "bhist", pk, a.key], [a.key])
                ACT(bhist[0:r, j, 0:2], p.ap[0:r, NT - 2:NT], AF.Identity, [p.key], [hk])
                if j < 2:
                    y = p
                else:
                    st.y5 = fpool.get()
                    y = st.y5
                TT(y.ap[0:r, :], pbg.ap[0:r, :], a.ap[0:r, :], ALU.mult, [pbg.key, a.key], [y.key])
                ppool.put(pbg)
                fpool.put(a)
                if j < 2:
                    yield from gn_stages(y, 3 + j, st.ymix)
                else:
                    fpool.put(p)

            def gelu_stages(out_ap, out_key, src_ap, bank, shape_fn):
                t = fpool.get()
                tap = shape_fn(t.ap)
                ACT(out_ap, src_ap, AF.Identity, [bank.key], [out_key])
                ACT(tap, src_ap, AF.Square, [bank.key], [t.key])
                ppool.put(bank)
                yield
                TS(tap, tap, 0.044715, 1.0, ALU.mult, ALU.add, [t.key], [t.key])
                TT(tap, tap, out_ap, ALU.mult, [t.key, out_key], [t.key])
                yield
                ACT(tap, tap, AF.Sigmoid, [t.key], [t.key], scale=1.5957691216057308)
                yield
                TT(out_ap, tap, out_ap, ALU.mult, [t.key, out_key], [out_key])
                fpool.put(t)

            def u_V(st, blk):
                xn = st.xn
                wt4 = wb[4]
                pst = ppool.get()
                for k in range(KC):
                    MM(pst.ap[:, 0:C_W], xn[k].ap[:, blk * 128:(blk + 1) * 128], wt4.ap[:, k, 0:C_W], k == 0, k == KC - 1,
                       [wt4.key, xn[k].key], [pst.key])
                v = fpool.get()
                yield from gelu_stages(v.ap[:, 0:C_W], v.key, pst.ap[:, 0:C_W], pst, lambda ap: ap[:, 0:C_W])
                sk = ("small_v", blk)
                sm = small[:, 4 * blk:4 * blk + 4]
                s6 = stat6v[:, blk, :]
                sch.op("dve", lambda e, v=v, s6=s6: e.bn_stats(out=s6, in_=v.ap[:, 0:C_W]), [v.key], [sk])
                sch.op("dve", lambda e, sm=sm, s6=s6: e.bn_aggr(out=sm[:, 0:2], in_=s6), [sk], [sk])
                yield
                ACT(sm[:, 2:3], sm[:, 1:2], AF.Sqrt, [sk], [sk], bias=EPS)
                yield
                RECIP(sm[:, 2:3], sm[:, 2:3], [sk], [sk])
                TS(v.ap[:, 0:C_W], v.ap[:, 0:C_W], sm[:, 0:1], sm[:, 2:3], ALU.subtract, ALU.mult, [v.key, sk], [v.key])
                TT(v.ap[:, 0:C_W], v.ap[:, 0:C_W], pcol(li, "ln_c_g"), ALU.mult, [v.key, pk], [v.key])
                vb = bpool.get()
                TT(vb.ap[:, 0:C_W], v.ap[:, 0:C_W], pcol(li, "ln_c_b"), ALU.add, [v.key, pk], [vb.key])
                fpool.put(v)
                st.vnb[blk] = vb

            units_c = [(5, [(0, 64)]), (6, [(1, 0), (2, 64)]), (7, [(3, 0), (4, 64)])]
            ucol = {5: 2 * A_W + 3 * B_W, 6: 2 * A_W + 3 * B_W + 64, 7: 2 * A_W + 3 * B_W + 192}

            def u_U(st, ui, last_proj):
                xn = st.xn
                chunk, groups = units_c[ui]
                p0 = 64 if chunk == 5 else 0
                m = 64 if chunk == 5 else 128
                wt, co = wcol(ucol[chunk])
                pu = proj(wt, co, m, xn, prow=p0)
                if last_proj:
                    for x in xn:
                        bpool.put(x)
                u = fpool.get()
                yield from gelu_stages(u.ap[p0:128, :], u.key, pu.ap[p0:128, :], pu, lambda ap, p0=p0: ap[p0:128, :])
                yield
                vnb = st.vnb
                assert all(v is not None for v in vnb)
                pm = ppool.get()
                ng = len(groups) * (NT // 128)
                i = 0
                for blk in range(NT // 128):
                    for (g, prow) in groups:
                        i += 1
                        sch.op("pe", lambda e, pm=pm, prow=prow, blk=blk, g=g, vb=vnb[blk]: e.matmul(
                            pm.ap[prow:prow + 64, blk * 128:(blk + 1) * 128], vb.ap[:, g * 64:(g + 1) * 64],
                            wsT[:, g, :], start=True, stop=True), [vnb[blk].key, "wsT"], [pm.key], inc=(i == ng))
                if ui == 2:
                    for vb in vnb:
                        bpool.put(vb)
                y = st.y5 if chunk == 5 else fpool.get()
                bias = pcol(li, "gmlp_bias", ui * 128, 128)
                tmp = fpool.get()
                for blk in range(NT // 128):
                    TT(tmp.ap[p0:128, blk * 128:(blk + 1) * 128], pm.ap[p0:128, blk * 128:(blk + 1) * 128], bias[p0:128, :],
                       ALU.add, [pm.key, pk], [tmp.key])
                ppool.put(pm)
                TT(y.ap[p0:128, :], tmp.ap[p0:128, :], u.ap[p0:128, :], ALU.mult, [tmp.key, u.key], [y.key])
                fpool.put(tmp)
                fpool.put(u)
                yield from gn_stages(y, chunk, st.ymix)

            def u_outproj(st, ocs, last):
                assert all(y is not None for y in st.ymix), "ymix incomplete before out-proj"
                psts = []
                for oc in ocs:
                    wt = wo[oc // 4]
                    psts.append(proj(wt, (oc % 4) * 128, 128, st.ymix))
                if last:
                    for y in st.ymix:
                        bpool.put(y)
                yield
                for oc, pst in zip(ocs, psts):
                    resid_add(pst, oc, st.t)

            states = [new_tile_state(t) for t in range(NTILES)]
            run_unit(u_rmsnorm(states[0]))
            for t in range(NTILES):
                st = states[t]
                run_unit(u_A(st, 0))
                run_unit(u_A(st, 1))
                run_unit(u_A(st, 2))
                run_unit(u_V(st, 0))
                run_unit(u_B(st, 0))
                run_unit(u_V(st, 1))
                if t > 0:
                    drain_for = states[t - 1]
                    while any(y is None for y in drain_for.ymix):
                        g = pending.popleft()
                        try:
                            next(g)
                            pending.append(g)
                        except StopIteration:
                            pass
                    run_unit(u_outproj(drain_for, [0, 1, 2], False))
                run_unit(u_V(st, 2))
                run_unit(u_B(st, 1))
                if t > 0:
                    run_unit(u_outproj(states[t - 1], [3, 4, 5], False))
                run_unit(u_V(st, 3))
                if t + 1 < NTILES:
                    run_unit(u_rmsnorm(states[t + 1]))
                run_unit(u_B(st, 2))
                if t > 0:
                    run_unit(u_outproj(states[t - 1], [6, 7], True))
                run_unit(u_Afin(st))
                run_unit(u_U(st, 0, False))
                run_unit(u_U(st, 1, False))
                run_unit(u_U(st, 2, True))
            drain()
            run_unit(u_outproj(states[NTILES - 1], [0, 1, 2], False))
            run_unit(u_outproj(states[NTILES - 1], [3, 4, 5], False))
            run_unit(u_outproj(states[NTILES - 1], [6, 7], True))
            drain()

        def attn_phase(li, l, b):
            pk = ("par", li)
            wkv = [wblock(w_xkv_d[li], 512 * i, 512) for i in range(4)]
            mT = [fpool.get() for _ in range(4)]
            assert MEM == 256

            for blk in range(MEM // 128):
                for half in range(2):
                    xt = fpool.get()
                    DMA("sp", xt.ap, mem_d[b, blk * 128:(blk + 1) * 128, half * 512:(half + 1) * 512],
                        "ld%d" % xt.key[1], [], [xt.key])
                    pst = ppool.get()
                    for j in range(4):
                        TR(pst.ap[:, j * 128:(j + 1) * 128], xt.ap[:, j * 128:(j + 1) * 128], [xt.key], [pst.key], inc=(j == 3))
                    fpool.put(xt)
                    for jj in range(2):
                        mt = mT[2 * half + jj]
                        ACT(mt.ap.rearrange("p (c m) -> p c m", c=2)[:, :, blk * 128:(blk + 1) * 128],
                            pst.ap[:, jj * 256:(jj + 1) * 256].rearrange("p (c m) -> p c m", c=2), AF.Identity, [pst.key], [mt.key])
                    ppool.put(pst)
            mn = [bpool.get() for _ in range(4)]
            msrc = [(mT[c // 2].ap[:, (c % 2) * MEM:(c % 2 + 1) * MEM], mT[c // 2].key) for c in range(KC)]
            mdst_ = [(mn[c // 2].ap[:, (c % 2) * MEM:(c % 2 + 1) * MEM], mn[c // 2].key) for c in range(KC)]
            rmsnorm_T(msrc, pcol(li, "norm_mem_g"), pk, mdst_, MEM)
            for m_ in mT:
                fpool.put(m_)
            KT = [bpool.get() for _ in range(4)]
            for dc in range(KC):
                wt = wkv[dc // 4]
                pst = ppool.get()
                for k in range(KC):
                    MM(pst.ap[:, 0:MEM], wt.ap[:, k, (dc % 4) * 128:(dc % 4 + 1) * 128], mdst_[k][0], k == 0, k == KC - 1,
                       [wt.key, mdst_[k][1]], [pst.key])
                kt = KT[dc // 2]
                ACT(kt.ap[:, (dc % 2) * MEM:(dc % 2 + 1) * MEM], pst.ap[:, 0:MEM], AF.Identity, [pst.key], [kt.key])
                ppool.put(pst)
            V = [bpool.get() for _ in range(4)]
            for mb in range(2):
                for half in range(2):
                    wt = wkv[2 + half]
                    pst = ppool.get()
                    for k in range(KC):
                        MM(pst.ap, mdst_[k][0][:, mb * 128:(mb + 1) * 128], wt.ap[:, k, :], k == 0, k == KC - 1,
                           [wt.key, mdst_[k][1]], [pst.key])
                    vt = V[2 * mb + half]
                    ACT(vt.ap, pst.ap, AF.Identity, [pst.key], [vt.key])
                    ppool.put(pst)
            for m_ in mn:
                bpool.put(m_)
            wq = [wblock(w_xq_d[li], 512 * i, 512) for i in range(2)]
            wo = [wblock(w_xo_d[li], 512 * i, 512) for i in range(2)]

            def rnq(t):
                xn = [bpool.get() for _ in range(KC)]
                rmsnorm_T(hsrc(t), pcol(li, "norm_x_g"), pk, [(x.ap, x.key) for x in xn], NT)
                qT = []
                for dc in range(KC):
                    pst = proj(wq[dc // 4], (dc % 4) * 128, 128, xn)
                    q = bpool.get()
                    ACT(q.ap, pst.ap, AF.Identity, [pst.key], [q.key], scale=1.0 / 16.0)
                    ppool.put(pst)
                    qT.append(q)
                for x in xn:
                    bpool.put(x)
                return qT

            def head_front(h, qT):
                PT = []
                for mb in range(2):
                    pst = ppool.get()
                    for dcc in range(2):
                        dc = 2 * h + dcc
                        kt = KT[dc // 2]
                        MM(pst.ap, kt.ap[:, (dc % 2) * MEM + mb * 128:(dc % 2) * MEM + (mb + 1) * 128], qT[dc].ap,
                           dcc == 0, dcc == 1, [kt.key, qT[dc].key], [pst.key])
                    p_ = bpool.get()
                    ACT(p_.ap, pst.ap, AF.Exp, [pst.key], [p_.key])
                    ppool.put(pst)
                    PT.append(p_)
                return PT

            def head_back(h, PT, oT):
                pss = ppool.get()
                for mb in range(2):
                    MM(pss.ap, ones_b[:, :], PT[mb].ap, mb == 0, mb == 1, [PT[mb].key, "ones_b"], [pss.key])
                rs = fpool.get()
                RECIP(rs.ap, pss.ap, [pss.key], [rs.key])
                ppool.put(pss)
                for dcc in range(2):
                    dc = 2 * h + dcc
                    pst = ppool.get()
                    for mb in range(2):
                        vt = V[2 * mb + dc // 4]
                        MM(pst.ap, vt.ap[:, (dc % 4) * 128:(dc % 4 + 1) * 128], PT[mb].ap, mb == 0, mb == 1,
                           [vt.key, PT[mb].key], [pst.key])
                    o = bpool.get()
                    TT(o.ap, pst.ap, rs.ap, ALU.mult, [pst.key, rs.key], [o.key])
                    ppool.put(pst)
                    oT.append(o)
                fpool.put(rs)
                for p_ in PT:
                    bpool.put(p_)

            qT_next = rnq(0)
            for t in range(NTILES):
                qT = qT_next
                oT = []
                pend = None
                for h in range(4):
                    PT = head_front(h, qT)
                    if pend is not None:
                        head_back(pend[0], pend[1], oT)
                    pend = (h, PT)
                for q in qT:
                    bpool.put(q)
                if t + 1 < NTILES:
                    qT_next = rnq(t + 1)
                head_back(pend[0], pend[1], oT)
                for oc in range(KC):
                    pst = proj(wo[oc // 4], (oc % 4) * 128, 128, oT)
                    resid_add(pst, oc, t)
                for o in oT:
                    bpool.put(o)
            for kt in KT:
                bpool.put(kt)
            for vt in V:
                bpool.put(vt)

        lbcnt = [0]
        dgcnt = [0]

        def ffn_phase(li, l, tail_hook=None):
            pk = ("par", li)
            moe = (l % 2 == 1)
            hn = [[None] * NTILES for _ in range(KC)]
            rstds = []
            for t in range(NTILES):
                tl = [bpool.get() for _ in range(KC)]
                r = rmsnorm_T(hsrc(t), pcol(li, "norm_ffn_g"), pk, [(x.ap, x.key) for x in tl], NT, keep_rstd=moe)
                for c in range(KC):
                    hn[c][t] = tl[c]
                if moe:
                    router(li, t, r)
                    fpool.put(r)

            deferred = []

            def slice_compute(wg, wu, wd, nfc, bc, after_tile=None):
                for t in range(NTILES):
                    xn = [hn[c][t] for c in range(KC)]
                    aT = []
                    for fc in range(nfc):
                        pg = proj(wg, fc * 128, 128, xn)
                        pu = proj(wu, fc * 128, 128, xn)
                        sg = fpool.get()
                        ACT(sg.ap, pg.ap, AF.Silu, [pg.key], [sg.key])
                        ppool.put(pg)
                        if bc is not None:
                            TT(sg.ap, sg.ap, bc[t].ap, ALU.mult, [sg.key, bc[t].key], [sg.key])
                        a = bpool.get()
                        TT(a.ap, pu.ap, sg.ap, ALU.mult, [pu.key, sg.key], [a.key])
                        ppool.put(pu)
                        fpool.put(sg)
                        aT.append(a)

                    def down(t=t, aT=aT, wd=wd, nfc=nfc, after_tile=after_tile):
                        for oc in range(KC):
                            pst = ppool.get()
                            for fc in range(nfc):
                                MM(pst.ap, wd.ap[:, fc, oc * 128:(oc + 1) * 128], aT[fc].ap, fc == 0, fc == nfc - 1,
                                   [wd.key, aT[fc].key], [pst.key])
                            resid_add(pst, oc, t)
                        for a in aT:
                            bpool.put(a)
                        if after_tile is not None:
                            after_tile(t)

                    if deferred:
                        deferred.pop()()
                    deferred.append(down)

            if not moe:
                Wg, Wu, Wd = fg_d[0], fu_d[0], fd_d[0]
                f0 = 0
                while f0 < DFF:
                    nf = min(512, DFF - f0)
                    wg = wblock(Wg, f0, nf)
                    wu = wblock(Wu, f0, nf)
                    wd = wblock_down(Wd, f0, nf)
                    slice_compute(wg, wu, wd, nf // 128, None)
                    f0 += nf
            else:
                for e in range(N_EXP):
                    bc = []
                    for t in range(NTILES):
                        pst = ppool.get()
                        for blk in range(NT // 128):
                            gi = (t * (NT // 128) + blk) * 8 + e
                            lb = lbcnt[0] % 2
                            lbcnt[0] += 1
                            sch.op("dve", lambda ee, lb=lb, gi=gi: ee.tensor_copy(
                                out=lbc[:, lb, :], in_=comb_tm[:, gi:gi + 1].to_broadcast([128, 128])), [("comb", t)], [("lbc", lb)])
                            sch.op("pe", lambda ee, pst=pst, lb=lb, blk=blk: ee.matmul(
                                pst.ap[:, blk * 128:(blk + 1) * 128], lbc[:, lb, :], ident[:, :], start=True, stop=True),
                                [("lbc", lb), "ident"], [pst.key])
                        bt = fpool.get()
                        ACT(bt.ap, pst.ap, AF.Identity, [pst.key], [bt.key])
                        ppool.put(pst)
                        bc.append(bt)
                    Wg, Wu, Wd = mg_d[0, e], mu_d[0, e], md_d[0, e]
                    f0 = 0
                    while f0 < DEXP:
                        nf = min(512, DEXP - f0)
                        wg = wblock(Wg, f0, nf)
                        wu = wblock(Wu, f0, nf)
                        wd = wblock_down(Wd, f0, nf)
                        last_slice = (e == N_EXP - 1) and (f0 + nf >= DEXP)
                        if last_slice and tail_hook is not None:
                            def _after(t, bc=bc):
                                if bc[t] is not None:
                                    fpool.put(bc[t])
                                    bc[t] = None
                                tail_hook(t)
                            slice_compute(wg, wu, wd, nf // 128, bc, _after)
                        else:
                            slice_compute(wg, wu, wd, nf // 128, bc)
                        f0 += nf
                    for i_, bt in enumerate(bc):
                        if bt is not None:
                            fpool.put(bt)
                            bc[i_] = None
            if deferred:
                deferred.pop()()
            for c in range(KC):
                for t in range(NTILES):
                    bpool.put(hn[c][t])

        def router(li, t, rstd):
            pk = ("par", li)
            if t == 0:
                for k in range(KC):
                    TS(gw[:, k * 8:(k + 1) * 8], pcol(li, "router", k * 8, 8), pcol(li, "norm_ffn_g", k, 1), None, ALU.mult, None,
                       [pk], ["gw"])
            for blk in range(NT // 128):
                tok = slice(t * NT + blk * 128, t * NT + (blk + 1) * 128)
                pst = ppool.get()
                for k in range(KC):
                    MM(pst.ap[:, 0:8], hT[:, k, tok], gw[:, k * 8:(k + 1) * 8], k == 0, k == KC - 1, [hkey(k, t), "gw"], [pst.key])
                sch.op("pe", lambda e, pst=pst, blk=blk: e.matmul(
                    pst.ap[:, 8:9], rstd.ap[0:1, blk * 128:(blk + 1) * 128], ones_f[0:1, 0:1], start=True, stop=True),
                    [rstd.key, "ones_f"], [pst.key])
                lg = small[:, 8:16]
                TS(lg, pst.ap[:, 0:8], pst.ap[:, 8:9], None, ALU.mult, None, [pst.key], ["small"])
                ppool.put(pst)
                sch.op("dve", lambda e: e.max(out=small[:, 16:24], in_=small[:, 8:16]), ["small"], ["small"])
                TS(small[:, 24:32], lg, small[:, 17:18], None, ALU.is_ge, None, ["small"], ["small"])
                TS(small[:, 32:33], small[:, 16:17], -1.0, None, ALU.mult, None, ["small"], ["small"])
                ACT(small[:, 40:48], lg, AF.Exp, ["small"], ["small"], bias=small[:, 32:33])
                STT(small[:, 48:56], small[:, 40:48], 1.0, small[:, 24:32], ALU.mult, ALU.mult, ["small"], ["small"],
                    accum_out=small[:, 33:34])
                RECIP(small[:, 34:35], small[:, 33:34], ["small"], ["small"])
                gi = (t * (NT // 128) + blk) * 8
                TS(comb_tm[:, gi:gi + 8], small[:, 48:56], small[:, 34:35], None, ALU.mult, None, ["small"], [("comb", t)])

        def final_phase(b, tiles=None):
            for t in (range(NTILES) if tiles is None else tiles):
                rstd = rmsnorm_T(hsrc(t), fing, "fing", None, NT, keep_rstd=True)
                for half in range(2):
                    yT = []
                    for j in range(4):
                        c = 4 * half + j
                        y = fpool.get()
                        STT(y.ap, hT[:, c, t * NT:(t + 1) * NT], fing[:, c:c + 1], rstd.ap, ALU.mult, ALU.mult,
                            [hkey(c, t), rstd.key, "fing"], [y.key])
                        yT.append(y)
                    for blk in range(NT // 128):
                        pst = ppool.get()
                        for j in range(4):
                            TR(pst.ap[:, j * 128:(j + 1) * 128], yT[j].ap[:, blk * 128:(blk + 1) * 128], [yT[j].key], [pst.key], inc=(j == 3))
                        ot = fpool.get()
                        ACT(ot.ap, pst.ap, AF.Identity, [pst.key], [ot.key])
                        ppool.put(pst)
                        r0 = t * NT + blk * 128
                        DMA("sp", out_d[b, r0:r0 + 128, half * 512:(half + 1) * 512], ot.ap, "st%d" % ot.key[1], [ot.key], [])
                        fpool.put(ot)
                    for y in yT:
                        fpool.put(y)
                fpool.put(rstd)

        def hdst(half, blk):
            return (hT[:, 4 * half:4 * half + 4, blk * 128:(blk + 1) * 128],
                    [hkey(4 * half + j, (blk * 128) // NT) for j in range(4)])

        stages = getattr(cfg, "stages", ("mix", "attn", "ffn"))
        n_l = len(cfg.layers)
        tail_ok = ("ffn" in stages) and (cfg.layers[-1] % 2 == 1) and getattr(cfg, "tail_overlap", True)
        preloaded = False
        for b in range(NB):
            if not preloaded:
                load_T(x_d[b], S, hdst)
            preloaded = False

            def tail_hook(t, b=b):
                final_phase(b, [t])
                if b + 1 < NB:
                    load_T(x_d[b + 1], S, hdst, blks=range(t * (NT // 128), (t + 1) * (NT // 128)))

            for li, l in enumerate(cfg.layers):
                if "mix" in stages:
                    mixer_phase(li, l)
                if "attn" in stages:
                    attn_phase(li, l, b)
                if "ffn" in stages:
                    ffn_phase(li, l, tail_hook if (tail_ok and li == n_l - 1) else None)
            if tail_ok:
                preloaded = (b + 1 < NB)
            else:
                final_phase(b)
        sch.barrier_wait("sp", [k for k in sch.cnt if k.startswith("st")])

        sch.check_deadlock()
        with nc.Block() as block:
            def emit(engname, e):
                for (waits, fn, incsem, amt) in sch.ops[engname]:
                    for (s, v) in waits:
                        e.wait_ge(sems[s], v)
                    if fn is not None:
                        ins = fn(e)
                        if incsem is not None:
                            ins.then_inc(sems[incsem], amt)

            @block.tensor
            def _(e):
                emit("pe", e)

            @block.scalar
            def _(e):
                emit("act", e)

            @block.vector
            def _(e):
                emit("dve", e)

            @block.gpsimd
            def _(e):
                emit("pool", e)

            @block.sync
            def _(e):
                emit("sp", e)
    build_nc.last_nops = sch.n_ops
    return nc


def make_in_maps(cfg, inp):
    x = np.asarray(inp["x"], np.float32)
    mem = np.asarray(inp["mem"], np.float32)
    L = len(cfg.layers)
    params = np.stack([_build_params(inp, l) for l in cfg.layers])
    wsT = np.stack([np.ascontiguousarray(np.asarray(inp["gmlp_ws"][l], np.float32).transpose(2, 0, 1)).reshape(128, 5 * 128)
                    for l in cfg.layers])
    ident = _build_consts()
    lay = list(cfg.layers)
    shared = {
        "params": params, "wsT": wsT, "fin_g": _colmajor(inp["norm_final_g"]), "ident": ident,
        "w_in": np.asarray(inp["w_in"], np.float32)[lay], "w_mix_out": np.asarray(inp["w_mix_out"], np.float32)[lay],
        "w_xq": np.asarray(inp["w_xq"], np.float32)[lay], "w_xkv": np.asarray(inp["w_xkv"], np.float32)[lay],
        "w_xo": np.asarray(inp["w_xo"], np.float32)[lay],
    }
    if any(l % 2 == 0 for l in cfg.layers):
        shared["ffn_w_gate"] = np.asarray(inp["ffn_w_gate"], np.float32)
        shared["ffn_w_up"] = np.asarray(inp["ffn_w_up"], np.float32)
        shared["ffn_w_down"] = np.asarray(inp["ffn_w_down"], np.float32)
    if any(l % 2 == 1 for l in cfg.layers):
        shared["moe_w_gate"] = np.asarray(inp["moe_w_gate"], np.float32)
        shared["moe_w_up"] = np.asarray(inp["moe_w_up"], np.float32)
        shared["moe_w_down"] = np.asarray(inp["moe_w_down"], np.float32)
    maps = []
    for c in range(cfg.ncores):
        m = dict(shared)
        m["x"] = np.ascontiguousarray(x[c * cfg.NB:(c + 1) * cfg.NB])
        m["mem"] = np.ascontiguousarray(mem[c * cfg.NB:(c + 1) * cfg.NB])
        maps.append(m)
    return maps


def run(cfg, inp, trace=False):
    nc = build_nc(cfg)
    maps = make_in_maps(cfg, inp)
    res = run_bass_kernel_spmd(nc, maps, core_ids=list(range(cfg.ncores)), trace=trace)
    out = np.concatenate([np.asarray(r["out"]) for r in res.results], axis=0)
    return out.astype(np.float32), res


def kernel(**inputs):
    cfg = Cfg()
    out, _ = run(cfg, inputs)
    return out
```
